# Optimizing a Trainium2 kernel written in Bass

```python
import math
import jax, jax.numpy as jnp
from jax import lax
import numpy as np

D_MODEL = 1024
BATCH = 4
SEQ = 8192
DEPTH = 2

HEAD_DIM = 64
ROPE_THETA = 500000.0
ROT_DIM = HEAD_DIM // 4
BLOCK = 128
EPS = 1e-6
NEG = -1e30

DILATED_PATTERNS = ((128, 1), (512, 4), (2048, 16))
A_HEADS_PER_GROUP = 4
A_HEADS = A_HEADS_PER_GROUP * len(DILATED_PATTERNS)
A_WIDTH = A_HEADS * HEAD_DIM
A_OUT = A_HEADS_PER_GROUP * HEAD_DIM

B_HEADS = 4
B_QK_WIDTH = B_HEADS * 2 * HEAD_DIM
B_V_DIM = 2 * HEAD_DIM
B_V_WIDTH = B_HEADS * B_V_DIM

POOL_WINDOWS = (2, 4, 8, 16)
C_GROUPS = len(POOL_WINDOWS)
C_GROUP_DIM = 128
C_WIDTH = C_GROUPS * C_GROUP_DIM

N_BRANCH = 3

OFF_QA = 0
OFF_KA = OFF_QA + A_WIDTH
OFF_VA = OFF_KA + A_WIDTH
OFF_QB = OFF_VA + A_WIDTH
OFF_KB = OFF_QB + B_QK_WIDTH
OFF_VB = OFF_KB + B_QK_WIDTH
OFF_C = OFF_VB + B_V_WIDTH
OFF_G = OFF_C + C_WIDTH
IN_COLS = OFF_G + N_BRANCH * D_MODEL

D_FF = 3584
N_EXPERTS = 8
TOP_K = 2
N_DENSE = (DEPTH + 1) // 2
N_MOE = DEPTH // 2

kernel_name = "hybrid_dilated_diff_pool_moe_block"


def rmsnorm(x, g):
    xf = x.astype(jnp.float32)
    y = xf * lax.rsqrt(jnp.mean(xf * xf, axis=-1, keepdims=True) + EPS)
    return (y * g.astype(jnp.float32)).astype(x.dtype)


def rope_tables(positions):
    inv_freq = ROPE_THETA ** (-jnp.arange(0, ROT_DIM, 2, dtype=jnp.float32) / ROT_DIM)
    ang = positions.astype(jnp.float32)[..., None] * inv_freq
    return jnp.cos(ang)[:, :, None, :], jnp.sin(ang)[:, :, None, :]


def apply_partial_rope(x, cos, sin):
    half = ROT_DIM // 2
    c = cos.astype(x.dtype)
    s = sin.astype(x.dtype)
    x1 = x[..., :half]
    x2 = x[..., half:ROT_DIM]
    return jnp.concatenate([x1 * c - x2 * s, x2 * c + x1 * s, x[..., ROT_DIM:]], axis=-1)


def dilated_window_attn(q, k, v, dil, span):
    assert span <= BLOCK
    B, S, H, Dh = q.shape
    L = S // dil
    nblk = -(-L // BLOCK)
    Lp = nblk * BLOCK

    def strided(t):
        t = t.astype(jnp.float32).reshape(B, L, dil, H, Dh).transpose(0, 2, 3, 1, 4)
        return jnp.pad(t, ((0, 0), (0, 0), (0, 0), (0, Lp - L), (0, 0)))

    qs, ks, vs = strided(q), strided(k), strided(v)

    def band(t):
        front = jnp.pad(t, ((0, 0), (0, 0), (0, 0), (BLOCK, 0), (0, 0)))
        prev = front[:, :, :, :Lp].reshape(B, dil, H, nblk, BLOCK, Dh)
        cur = t.reshape(B, dil, H, nblk, BLOCK, Dh)
        return jnp.concatenate([prev, cur], axis=-2)

    qb = qs.reshape(B, dil, H, nblk, BLOCK, Dh)
    kb, vb = band(ks), band(vs)
    s = jnp.einsum('brhnqd,brhnkd->brhnqk', qb, kb) * (Dh ** -0.5)

    a = jnp.arange(BLOCK)[:, None]
    j = jnp.arange(2 * BLOCK)[None, :]
    dist = a + BLOCK - j
    valid = (dist >= 0) & (dist <= span)
    blk = jnp.arange(nblk)[:, None, None]
    mask = valid[None] & ((blk > 0) | (j[None] >= BLOCK))
    s = jnp.where(mask, s, NEG)

    m = jnp.max(s, axis=-1, keepdims=True)
    p = jnp.exp(s - m)
    l = jnp.sum(p, axis=-1, keepdims=True)
    o = jnp.einsum('brhnqk,brhnkd->brhnqd', p, vb) / l
    lse = (m + jnp.log(l))[..., 0]

    o = o.reshape(B, dil, H, Lp, Dh)[:, :, :, :L].transpose(0, 3, 1, 2, 4).reshape(B, S, H, Dh)
    lse = lse.reshape(B, dil, H, Lp)[:, :, :, :L].transpose(0, 3, 1, 2).reshape(B, S, H)
    return o, lse


def dilated_mixture(q, k, v):
    B, S = q.shape[:2]
    outs, lses = [], []
    for g, (window, dil) in enumerate(DILATED_PATTERNS):
        sl = slice(g * A_HEADS_PER_GROUP, (g + 1) * A_HEADS_PER_GROUP)
        o, lse = dilated_window_attn(q[:, :, sl], k[:, :, sl], v[:, :, sl], dil, window // dil)
        outs.append(o)
        lses.append(lse)
    o = jnp.stack(outs, axis=0)
    w = jax.nn.softmax(jnp.stack(lses, axis=0), axis=0)
    out = jnp.sum(w[..., None] * o, axis=0)
    return out.reshape(B, S, A_OUT).astype(q.dtype)


def diff_attention(q, k, v, lam, subln_g, lambda_init):
    B, S, H, _, Dh = q.shape
    nq = S // BLOCK
    qb = q.reshape(B, nq, BLOCK, H, 2, Dh).transpose(1, 0, 2, 3, 4, 5)
    kf = k.astype(jnp.float32)
    vf = v.astype(jnp.float32)
    k_pos = jnp.arange(S)
    scale = Dh ** -0.5

    def one_block(args):
        qblk, i = args
        s = jnp.einsum('bqhcd,bkhcd->bhcqk', qblk.astype(jnp.float32), kf) * scale
        q_pos = i * BLOCK + jnp.arange(BLOCK)
        s = jnp.where(k_pos[None, :] <= q_pos[:, None], s, NEG)
        p = jax.nn.softmax(s, axis=-1)
        a = p[:, :, 0] - lam * p[:, :, 1]
        return jnp.einsum('bhqk,bkhe->bqhe', a, vf)

    o = lax.map(one_block, (qb, jnp.arange(nq)))
    o = o.transpose(1, 0, 2, 3, 4).reshape(B, S, H, 2 * Dh)
    o = rmsnorm(o, subln_g) * (1.0 - lambda_init)
    return o.reshape(B, S, H * 2 * Dh).astype(q.dtype)


def multiscale_pool(c, pool_w, pool_scale):
    B, S, _ = c.shape
    cg = c.astype(jnp.float32).reshape(B, S, C_GROUPS, C_GROUP_DIM)
    t = jnp.arange(S)
    outs = []
    for g, w in enumerate(POOL_WINDOWS):
        xg = cg[:, :, g]
        cs = jnp.cumsum(xg, axis=1)
        cs_shift = jnp.pad(cs, ((0, 0), (w, 0), (0, 0)))[:, :S]
        cnt = jnp.minimum(t + 1, w).astype(jnp.float32)[None, :, None]
        outs.append((cs - cs_shift) / cnt - xg)
    d = jnp.stack(outs, axis=2)
    y = jnp.einsum('bsgc,gce->bsge', d, pool_w.astype(jnp.float32)).reshape(B, S, C_WIDTH)
    return (y * pool_scale.astype(jnp.float32)).astype(c.dtype)


def swiglu(h, wg, wu, wd):
    return (jax.nn.silu(h @ wg) * (h @ wu)) @ wd


def moe_ffn(h, w_router, wg, wu, wd):
    logits = (h @ w_router).astype(jnp.float32)
    topv, topi = lax.top_k(logits, TOP_K)
    probs = jax.nn.softmax(topv, axis=-1)
    gate = jnp.sum(jax.nn.one_hot(topi, N_EXPERTS, dtype=jnp.float32) * probs[..., None], axis=-2)
    y = jnp.zeros_like(h)
    for e in range(N_EXPERTS):
        y = y + gate[..., e:e + 1].astype(h.dtype) * swiglu(h, wg[e], wu[e], wd[e])
    return y


def setup_inputs(seed: int = 0) -> dict:
    key = jax.random.key(seed)
    ks = jax.random.split(key, 24)
    f32 = jnp.float32
    nrm = lambda k, shape, fan: jax.random.normal(k, shape, f32) * (fan ** -0.5)
    x = jax.random.normal(ks[0], (BATCH, SEQ, D_MODEL), f32)
    offset = jax.random.randint(ks[1], (BATCH, 1), 0, 1024, dtype=jnp.int32)
    positions = offset + jnp.arange(SEQ, dtype=jnp.int32)[None, :]
    return {
        "x": x,
        "positions": positions,
        "norm1_g": 1.0 + 0.02 * jax.random.normal(ks[2], (DEPTH, D_MODEL), f32),
        "w_in": nrm(ks[3], (DEPTH, D_MODEL, IN_COLS), D_MODEL),
        "b_gate": 0.02 * jax.random.normal(ks[4], (DEPTH, N_BRANCH, D_MODEL), f32),
        "diff_lambda": 0.1 * jax.random.normal(ks[5], (DEPTH, 4, HEAD_DIM), f32),
        "diff_subln_g": 1.0 + 0.02 * jax.random.normal(ks[6], (DEPTH, B_V_DIM), f32),
        "pool_w": nrm(ks[7], (DEPTH, C_GROUPS, C_GROUP_DIM, C_GROUP_DIM), C_GROUP_DIM),
        "pool_scale": 1.0 + 0.02 * jax.random.normal(ks[8], (DEPTH, C_WIDTH), f32),
        "w_proj_a": nrm(ks[9], (DEPTH, A_OUT, D_MODEL), A_OUT),
        "w_proj_b": nrm(ks[10], (DEPTH, B_V_WIDTH, D_MODEL), B_V_WIDTH),
        "w_proj_c": nrm(ks[11], (DEPTH, C_WIDTH, D_MODEL), C_WIDTH),
        "w_out": nrm(ks[12], (DEPTH, D_MODEL, D_MODEL), D_MODEL),
        "norm2_g": 1.0 + 0.02 * jax.random.normal(ks[13], (DEPTH, D_MODEL), f32),
        "ffn_w_gate": nrm(ks[14], (N_DENSE, D_MODEL, D_FF), D_MODEL),
        "ffn_w_up": nrm(ks[15], (N_DENSE, D_MODEL, D_FF), D_MODEL),
        "ffn_w_down": nrm(ks[16], (N_DENSE, D_FF, D_MODEL), D_FF),
        "moe_router": nrm(ks[17], (N_MOE, D_MODEL, N_EXPERTS), D_MODEL),
        "moe_w_gate": nrm(ks[18], (N_MOE, N_EXPERTS, D_MODEL, D_FF), D_MODEL),
        "moe_w_up": nrm(ks[19], (N_MOE, N_EXPERTS, D_MODEL, D_FF), D_MODEL),
        "moe_w_down": nrm(ks[20], (N_MOE, N_EXPERTS, D_FF, D_MODEL), D_FF),
        "final_norm_g": 1.0 + 0.02 * jax.random.normal(ks[21], (D_MODEL,), f32),
    }


def reference(x, positions, norm1_g, w_in, b_gate, diff_lambda, diff_subln_g, pool_w,
              pool_scale, w_proj_a, w_proj_b, w_proj_c, w_out, norm2_g, ffn_w_gate,
              ffn_w_up, ffn_w_down, moe_router, moe_w_gate, moe_w_up, moe_w_down,
              final_norm_g):
    B, S, D = x.shape
    cos, sin = rope_tables(positions)
    for l in range(DEPTH):
        h = rmsnorm(x, norm1_g[l])
        z = h @ w_in[l]

        qa = apply_partial_rope(z[..., OFF_QA:OFF_KA].reshape(B, S, A_HEADS, HEAD_DIM), cos, sin)
        ka = apply_partial_rope(z[..., OFF_KA:OFF_VA].reshape(B, S, A_HEADS, HEAD_DIM), cos, sin)
        va = z[..., OFF_VA:OFF_QB].reshape(B, S, A_HEADS, HEAD_DIM)
        out_a = dilated_mixture(qa, ka, va)

        qb = apply_partial_rope(z[..., OFF_QB:OFF_KB].reshape(B, S, 2 * B_HEADS, HEAD_DIM), cos, sin)
        kb = apply_partial_rope(z[..., OFF_KB:OFF_VB].reshape(B, S, 2 * B_HEADS, HEAD_DIM), cos, sin)
        qb = qb.reshape(B, S, B_HEADS, 2, HEAD_DIM)
        kb = kb.reshape(B, S, B_HEADS, 2, HEAD_DIM)
        vb = z[..., OFF_VB:OFF_C].reshape(B, S, B_HEADS, B_V_DIM)
        lambda_init = 0.8 - 0.6 * math.exp(-0.3 * l)
        lp = diff_lambda[l].astype(jnp.float32)
        lam = jnp.exp(jnp.sum(lp[0] * lp[1])) - jnp.exp(jnp.sum(lp[2] * lp[3])) + lambda_init
        out_b = diff_attention(qb, kb, vb, lam, diff_subln_g[l], lambda_init)

        out_c = multiscale_pool(z[..., OFF_C:OFF_G], pool_w[l], pool_scale[l])

        gates = jax.nn.sigmoid(z[..., OFF_G:IN_COLS].reshape(B, S, N_BRANCH, D) + b_gate[l])
        mixed = (gates[:, :, 0] * (out_a @ w_proj_a[l])
                 + gates[:, :, 1] * (out_b @ w_proj_b[l])
                 + gates[:, :, 2] * (out_c @ w_proj_c[l]))
        x = x + mixed @ w_out[l]

        h2 = rmsnorm(x, norm2_g[l])
        if l % 2 == 0:
            i = l // 2
            x = x + swiglu(h2, ffn_w_gate[i], ffn_w_up[i], ffn_w_down[i])
        else:
            i = l // 2
            x = x + moe_ffn(h2, moe_router[i], moe_w_gate[i], moe_w_up[i], moe_w_down[i])
    return rmsnorm(x, final_norm_g)
```

```python
import math
from contextlib import ExitStack

import numpy as np
import concourse.bass as bass
import concourse.mybir as mybir
from concourse.bass_utils import run_bass_kernel_spmd

F32 = mybir.dt.float32
BF16 = mybir.dt.bfloat16
I32 = mybir.dt.int32
AF = mybir.ActivationFunctionType
ALU = mybir.AluOpType

SEM_BLOCK = 16384
N_ENG_SEMS = 12
N_DMA_SLOTS = 24
SAME_ENGINE_SYNC = True

D = 1024
T = 8192
TO = 4096
NEGM = -30000.0
EPS = 1e-6
DFF = 3584
NF = DFF // 128
NE = 8
TWO_PI = 6.28318
WARM_B = 12
WARM_P2B = 0


class Buf:
    __slots__ = ("name", "w", "r", "rd")

    def __init__(self, name=""):
        self.name = name
        self.w = None
        self.r = {}
        self.rd = []


class Op:
    __slots__ = ("idx", "eng", "fn", "deps", "needs_inc", "dma", "cnt", "slot", "sval")

    def __init__(self, idx, eng, fn, dma):
        self.idx = idx
        self.eng = eng
        self.fn = fn
        self.deps = []
        self.needs_inc = False
        self.dma = dma
        self.cnt = None
        self.slot = None
        self.sval = None


class Sched:
    ENGS = ("pe", "act", "dve", "pool", "sp")

    def __init__(self):
        self.ops = []
        self.last = {e: None for e in self.ENGS}
        self.dmas = {e: [] for e in self.ENGS}

    def emit(self, eng, fn, reads=(), writes=(), dma=False, extra=(), bg=False):
        op = Op(len(self.ops), eng, fn, dma)
        deps = {}
        for b in reads:
            if b.w is not None:
                deps[b.w.idx] = b.w
        for b in writes:
            if b.w is not None:
                deps[b.w.idx] = b.w
            for o in b.r.values():
                deps[o.idx] = o
            for o in b.rd:
                deps[o.idx] = o
        for o in extra:
            if o is not None:
                deps[o.idx] = o
        for d in deps.values():
            if (not d.dma) and d.eng == eng and (eng == "pe" or not SAME_ENGINE_SYNC):
                continue
            op.deps.append(d)
            d.needs_inc = True
        for b in reads:
            if dma:
                b.rd.append(op)
            else:
                b.r[eng] = op
        for b in writes:
            b.w = op
            b.r = {}
            b.rd = []
        self.ops.append(op)
        if dma:
            if not bg:
                self.dmas[eng].append(op)
        else:
            self.last[eng] = op
        return op

    def dma(self, out, in_, reads=(), writes=(), q="sp", bg=False):
        return self.emit(q, lambda e, o=out, i=in_: e.dma_start(out=o, in_=i), reads, writes, dma=True, bg=bg)

    def finalize(self):
        cnt = {e: 0 for e in self.ENGS}
        ndma = {e: 0 for e in self.ENGS}
        self.dma_ops = {e: [] for e in self.ENGS}
        for op in self.ops:
            if op.dma:
                i = ndma[op.eng]
                ndma[op.eng] += 1
                op.slot = i % N_DMA_SLOTS
                op.sval = 16 * (i // N_DMA_SLOTS + 1)
                if i >= N_DMA_SLOTS:
                    op.deps.append(self.dma_ops[op.eng][i - N_DMA_SLOTS])
                self.dma_ops[op.eng].append(op)
            elif op.needs_inc:
                cnt[op.eng] += 1
                op.cnt = cnt[op.eng]
        self.cnt = cnt
        self.ndma = ndma

    def run(self, nc):
        self.finalize()
        with ExitStack() as st:
            esems = {}
            for e in self.ENGS:
                nblk = (self.cnt[e] + SEM_BLOCK - 1) // SEM_BLOCK
                assert nblk <= N_ENG_SEMS, (e, self.cnt[e])
                esems[e] = [st.enter_context(nc.semaphore(f"s_{e}{k}")) for k in range(max(nblk, 1))]
            dsems = {}
            for e in self.ENGS:
                if self.ndma[e]:
                    dsems[e] = [st.enter_context(nc.semaphore(f"d_{e}{k}")) for k in range(N_DMA_SLOTS)]
            block = st.enter_context(nc.Block())

            def target(d):
                if d.dma:
                    return ("d", d.eng, d.slot), dsems[d.eng][d.slot], d.sval, d.sval
                blk = (d.cnt - 1) // SEM_BLOCK
                return ("c", d.eng), esems[d.eng][blk], (d.cnt - 1) % SEM_BLOCK + 1, d.cnt

            by_eng = {e: [o for o in self.ops if o.eng == e] for e in self.ENGS}

            def body(ename):
                def f(eng):
                    known = {}
                    for op in by_eng[ename]:
                        for d in op.deps:
                            key, sem, val, gval = target(d)
                            if known.get(key, 0) >= gval:
                                continue
                            eng.wait_ge(sem, val)
                            known[key] = gval
                        inst = op.fn(eng)
                        if op.dma:
                            inst.then_inc(dsems[ename][op.slot], 16)
                        elif op.needs_inc:
                            blk = (op.cnt - 1) // SEM_BLOCK
                            inst.then_inc(esems[ename][blk], 1)
                    if ename == "sp":
                        for q in self.ENGS:
                            for o in self.dma_ops[q][-N_DMA_SLOTS:]:
                                key, sem, val, gval = target(o)
                                if known.get(key, 0) >= gval:
                                    continue
                                eng.wait_ge(sem, val)
                                known[key] = gval
                return f

            block.tensor(body("pe"))
            block.scalar(body("act"))
            block.vector(body("dve"))
            block.gpsimd(body("pool"))
            block.sync(body("sp"))


class Arena:
    def __init__(self, ap, n):
        self.ap = ap
        self.n = n
        self.off = 0

    def reset(self):
        self.off = 0

    def alloc(self, shape):
        n = 1
        for s in shape:
            n *= s
        assert self.off + n <= self.n, ("arena overflow", self.off, n, self.n)
        a = self.ap[:, self.off:self.off + n]
        self.off += n
        if len(shape) == 2:
            a = a.rearrange("p (a b) -> p a b", b=shape[1])
        elif len(shape) == 3:
            a = a.rearrange("p (a b c) -> p a b c", b=shape[1], c=shape[2])
        return a


class B:
    def __init__(self, nc, st):
        self.nc = nc
        self.S = Sched()
        self.st = st
        self.nbuf = 0

    def buf(self, name="b"):
        self.nbuf += 1
        return Buf(f"{name}{self.nbuf}")

    def mm(self, out, lhsT, rhs, start, stop, reads, writes):
        return self.S.emit("pe", lambda e, o=out, l=lhsT, r=rhs, a=start, b=stop:
                           e.matmul(o, l, r, start=a, stop=b), reads, writes)

    def act(self, out, in_, func, reads, writes, scale=1.0, bias=None, eng="act"):
        if bias is None:
            fn = lambda e, o=out, i=in_, f=func, s=scale: e.activation(out=o, in_=i, func=f, scale=s)
        else:
            fn = lambda e, o=out, i=in_, f=func, s=scale, b=bias: e.activation(out=o, in_=i, func=f, scale=s, bias=b)
        return self.S.emit(eng, fn, reads, writes)

    def tt(self, out, in0, in1, op, reads, writes, eng="dve"):
        return self.S.emit(eng, lambda e, o=out, a=in0, b=in1, p=op: e.tensor_tensor(out=o, in0=a, in1=b, op=p),
                           reads, writes)

    def ts(self, out, in0, s1, s2, op0, op1, reads, writes, eng="dve"):
        if s2 is None:
            fn = lambda e, o=out, a=in0, x=s1, p=op0: e.tensor_scalar(out=o, in0=a, scalar1=x, scalar2=None, op0=p)
        else:
            fn = lambda e, o=out, a=in0, x=s1, y=s2, p=op0, q=op1: e.tensor_scalar(
                out=o, in0=a, scalar1=x, scalar2=y, op0=p, op1=q)
        return self.S.emit(eng, fn, reads, writes)

    def stt(self, out, in0, scalar, in1, op0, op1, reads, writes, eng="dve"):
        return self.S.emit(eng, lambda e, o=out, a=in0, s=scalar, b=in1, p=op0, q=op1:
                           e.scalar_tensor_tensor(out=o, in0=a, scalar=s, in1=b, op0=p, op1=q), reads, writes)

    def copy(self, out, in_, reads, writes, eng="dve"):
        return self.S.emit(eng, lambda e, o=out, i=in_: e.tensor_copy(out=o, in_=i), reads, writes)

    def recip(self, out, in_, reads, writes):
        return self.S.emit("dve", lambda e, o=out, i=in_: e.reciprocal(out=o, in_=i), reads, writes)

    def memset(self, ap, val, writes, eng="dve"):
        return self.S.emit(eng, lambda e, a=ap, v=val: e.memset(a, v), (), writes)

    def barrier(self):
        S = self.S
        deps = [S.last[e] for e in S.ENGS]
        for q in S.ENGS:
            deps += S.dmas[q][-N_DMA_SLOTS:]
        z = self.bar_t
        S.emit("act", lambda e: e.memset(z[:, 0:1], 0.0) if hasattr(e, "memset") else e.activation(
            out=z[:, 0:1], in_=z[:, 0:1], func=AF.Copy), (), (), extra=deps)
        S.emit("dve", lambda e: e.memset(z[:, 1:2], 0.0), (), (), extra=deps)
        S.emit("pool", lambda e: e.memset(z[:, 2:3], 0.0), (), (), extra=deps)
        S.emit("pe", lambda e: e.matmul(self.bar_ps[0:1, 0:1], self.ones[0:1, 0:1], self.ones[0:1, 0:1],
                                        start=True, stop=True), (), (), extra=deps)
        S.emit("sp", lambda e: e.dma_start(out=self.bar_d[0:1, 0:8], in_=self.bar_d[1:2, 0:8]), (), (),
               dma=True, extra=deps)
        self.abf.reset()
        self.af.reset()


def build_program(debug=False, n_layers=2, dense_moe=True):
    nc = bass.Bass("TRN2", target_bir_lowering=False)
    dt_in = lambda n, s, d=F32: nc.dram_tensor(n, list(s), d, kind="ExternalInput").ap()
    dt_sc = lambda n, s, d=BF16: nc.dram_tensor(n, list(s), d, kind="Internal").ap()

    xT = dt_in("xT", [D, T])
    pos = dt_in("pos", [1, T], I32)
    invcnt = dt_in("invcnt", [4, T])
    percore = dt_in("percore", [128, 2])
    consts = dt_in("consts", [128, 128 * 3 + 256 + 2048 + 2])
    vecs = dt_in("vecs", [2, 128, 64])
    lam_in = dt_in("lam_in", [2, 1, 256])
    fin_g = dt_in("fin_g", [128, 8])
    w_in = dt_in("w_in", [2, D, 7424])
    pool_w = dt_in("pool_w", [2, 4, 128, 128])
    w_pa = dt_in("w_pa", [2, 256, D])
    w_pb = dt_in("w_pb", [2, 512, D])
    w_pc = dt_in("w_pc", [2, 512, D])
    w_out = dt_in("w_out", [2, D, D])
    ffn_g = dt_in("ffn_g", [D, DFF])
    ffn_u = dt_in("ffn_u", [D, DFF])
    ffn_d = dt_in("ffn_d", [DFF, D])
    router = dt_in("router", [128, 8, NE])
    moe_g = dt_in("moe_g", [NE, D, DFF])
    moe_u = dt_in("moe_u", [NE, D, DFF])
    moe_d = dt_in("moe_d", [NE, DFF, D])
    outT = nc.dram_tensor("outT", [D, TO], F32, kind="ExternalOutput").ap()
    dbg = nc.dram_tensor("dbg", [D, T], F32, kind="ExternalOutput").ap() if debug else None

    xs = dt_sc("xs", [D, T], F32)
    hT = dt_sc("hT", [D, T])
    QAt = dt_sc("QAt", [768, T]); KAt = dt_sc("KAt", [768, T]); VA = dt_sc("VA", [T, 768])
    QBt = dt_sc("QBt", [512, T]); KBt = dt_sc("KBt", [512, T]); VB = dt_sc("VB", [T, 512])
    Ct = dt_sc("Ct", [512, T])
    OAt = dt_sc("OAt", [256, T]); OBt = dt_sc("OBt", [512, T]); OCt = dt_sc("OCt", [512, T])
    bar_d = dt_sc("bar_d", [2, 8], F32)
    Wg_b = dt_sc("Wg_b", [9, 14, 128, 2048]); Wu_b = dt_sc("Wu_b", [9, 14, 128, 2048])
    Wd_b = dt_sc("Wd_b", [9, 8, 128, 3584])

    with ExitStack() as st:
        b = B(nc, st)
        S = b.S
        sbt = lambda n, s, d: st.enter_context(nc.sbuf_tensor(n, list(s), d))
        NBF = 63600
        NF32 = 16128
        abf_t = sbt("abf", [128, NBF], BF16)
        af_t = sbt("af32", [128, NF32], F32)
        b.abf = Arena(abf_t, NBF)
        b.af = Arena(af_t, NF32)
        cst = sbt("cst", [128, 128 * 3 + 256 + 2048], BF16)
        cf = sbt("cf", [128, 8], F32)
        ropeC = dt_sc("ropeC", [128, T])
        ropeS = dt_sc("ropeS", [128, T])
        rtab = sbt("rtab", [128, 2, 2048], BF16)
        brt = Buf("rtab")
        vec_sb = sbt("vec_sb", [128, 2, 64], F32)
        lam_sb = sbt("lam_sb", [128, 2, 8], F32)
        fing_sb = sbt("fing_sb", [128, 8], F32)
        b.bar_t = sbt("bar_t", [128, 4], F32)
        b.bar_d = bar_d
        ps = [st.enter_context(nc.psum_tensor(f"ps{i}", [128, 512], F32))[:, :] for i in range(8)]
        pb = [Buf(f"ps{i}") for i in range(8)]
        b.bar_ps = st.enter_context(nc.psum_tensor("barps", [128, 16], F32)) if False else ps[7]
        ident = cst[:, 0:128]
        ones = cst[:, 128:256]
        Rblk = cst[:, 256:384]
        maskA = cst[:, 384:640]
        maskB = cst[:, 640:2688].rearrange("p (i q) -> p i q", q=512)
        b.ones = ones
        freq = cf[:, 0:1]; sgn = cf[:, 1:2]; pbias = cf[:, 2:3]; pflag = cf[:, 3:4]; epsc = cf[:, 4:5]
        bc = Buf("consts")

        S.dma(cst[:, :], consts[:, 0:2688], writes=[bc], q="pool")
        S.dma(cf[:, 0:2], consts[:, 2688:2690], writes=[bc], q="sp")
        S.dma(cf[:, 2:4], percore[:, :], writes=[bc], q="sp")
        b.memset(cf[:, 4:5], EPS, [bc])
        S.dma(vec_sb[:, 0, :], vecs[0], writes=[bc], q="sp")
        S.dma(vec_sb[:, 1, :], vecs[1], writes=[bc], q="sp")
        S.dma(fing_sb[:, :], fin_g[:, :], writes=[bc], q="sp")
        for k in range(8):
            S.dma(xs[k * 128:(k + 1) * 128, :], xT[k * 128:(k + 1) * 128, :], q="sp")
        lraw = b.af.alloc([2, 256])
        blr = Buf("lraw")
        for l in range(2):
            S.dma(lraw[:, l, :], lam_in[l].broadcast_to([128, 256]), writes=[blr], q="sp")
        for l in range(2):
            linit = 0.8 - 0.6 * math.exp(-0.3 * l)
            pr = b.af.alloc([128])
            b.tt(pr[:, 0:64], lraw[:, l, 0:64], lraw[:, l, 64:128], ALU.mult, [blr], [blr])
            b.tt(pr[:, 64:128], lraw[:, l, 128:192], lraw[:, l, 192:256], ALU.mult, [blr], [blr])
            S.emit("dve", lambda e, o=lam_sb[:, l, 0:1], i=pr[:, 0:64]: e.reduce_sum(out=o, in_=i, axis=mybir.AxisListType.X), [blr], [bc])
            S.emit("dve", lambda e, o=lam_sb[:, l, 1:2], i=pr[:, 64:128]: e.reduce_sum(out=o, in_=i, axis=mybir.AxisListType.X), [blr], [bc])
            b.act(lam_sb[:, l, 2:4], lam_sb[:, l, 0:2], AF.Exp, [bc], [bc])
            b.stt(lam_sb[:, l, 4:5], lam_sb[:, l, 3:4], -linit, lam_sb[:, l, 2:3], ALU.add, ALU.subtract, [bc], [bc])
            b.ts(vec_sb[:, l, 40:41], vec_sb[:, l, 40:41], 1.0 - linit, None, ALU.mult, None, [bc], [bc])
        posi = b.af.alloc([2048]).bitcast(I32)
        bpi = Buf("posi")
        u = b.af.alloc([2048]); kf = b.af.alloc([2048]); fr = b.af.alloc([2048])
        bu = Buf("u")
        for ch in range(4):
            sl = slice(ch * 2048, (ch + 1) * 2048)
            S.dma(posi[:, :], pos[0:1, sl].broadcast_to([128, 2048]), writes=[bpi], q="sp")
            b.copy(u, posi[:, :], [bpi], [bu])
            b.ts(u, u, freq, None, ALU.mult, None, [bu, bc], [bu])
            for which, tab in ((0, ropeS), (1, ropeC)):
                if which == 1:
                    b.ts(u, u, 0.25, None, ALU.add, None, [bu], [bu])
                b.copy(posi[:, :], u, [bu], [bpi])
                b.copy(kf, posi[:, :], [bpi], [bu])
                b.tt(fr, u, kf, ALU.subtract, [bu], [bu])
                b.act(fr, fr, AF.Sin, [bu], [bu], scale=TWO_PI)
                if which == 0:
                    b.ts(rtab[:, 0, :], fr, sgn, None, ALU.mult, None, [bu, bc], [brt])
                else:
                    b.copy(rtab[:, 1, :], fr, [bu], [brt])
                S.dma(tab[:, sl], rtab[:, which, :], reads=[brt], q="sp")

        def load_w(dst, src, k_chunks, q="pool"):
            for k in range(k_chunks):
                S.dma(dst[:, k, :], src[k * 128:(k + 1) * 128, :], writes=[bw], q=q)

        def rms_tile(xt, bxt, gcol, h, bh, psb, psbuf, scr, bscr, n=512, kch=8, dim=1024.0, bsq=None):
            sq = scr["sq"]
            if bsq is None:
                bsq = bscr
            for k in range(kch):
                b.act(sq[:, k, :], xt[:, k, :], AF.Square, [bxt], [bsq])
            for k in range(kch):
                b.mm(psb[:, 0:n], ones, sq[:, k, :], k == 0, k == kch - 1, [bsq, bc], [psbuf])
            b.act(scr["ln"], psb[:, 0:n], AF.Ln, [psbuf, bc], [bscr], scale=1.0 / dim, bias=epsc)
            b.act(scr["rstd"], scr["ln"], AF.Exp, [bscr], [bscr], scale=-0.5)
            for k in range(kch):
                b.stt(h[:, k, :], xt[:, k, :], gcol[:, k:k + 1], scr["rstd"], ALU.mult, ALU.mult,
                      [bxt, bscr, bc], [bh])

        bw = Buf("w")

        def phase_p1(l, tiles_full, tiles_kvc):
            nonlocal bw
            b.barrier()
            bw = Buf("w")
            W = b.abf.alloc([8, 4352])
            load_w(W, w_in[l][:, 0:4352], 8)
            xts = [b.af.alloc([8, 512]) for _ in range(2)]
            bxts = [Buf("xt") for _ in range(2)]
            scrs = [{"sq": b.abf.alloc([8, 512]), "ln": b.af.alloc([512]), "rstd": b.af.alloc([512])} for _ in range(2)]
            bscrs = [Buf("scr") for _ in range(2)]
            hs_ = [b.abf.alloc([8, 512]) for _ in range(2)]; bhs = [Buf("h") for _ in range(2)]
            qs = [b.abf.alloc([512]) for _ in range(3)]; bqs = [Buf("q") for _ in range(3)]
            t1 = [b.af.alloc([512]) for _ in range(3)]; bt1 = [Buf("t1") for _ in range(3)]
            qo = [b.abf.alloc([512]) for _ in range(3)]; bqo = [Buf("qo") for _ in range(3)]
            vsb = b.abf.alloc([4, 1280]); bvs = Buf("vsb")
            g1 = vec_sb[:, l, 0:8]
            rc = [b.abf.alloc([512]) for _ in range(2)]; rs_ = [b.abf.alloc([512]) for _ in range(2)]
            brc = [Buf("rc") for _ in range(2)]
            nt = 0
            alltiles = sorted(set(tiles_full) | set(tiles_kvc))
            it = 0
            for ti in alltiles:
                t0 = ti * 512
                full = ti in tiles_full
                rj = nt % 2; nt += 1
                xt = xts[rj]; bxt = bxts[rj]
                h = hs_[rj]; bh = bhs[rj]; scr = scrs[rj]; bscr = bscrs[rj]

                def p1_load(tix_, jj):
                    tt0 = tix_ * 512
                    S.dma(xts[jj], xs.rearrange("(k p) t -> p k t", p=128)[:, :, tt0:tt0 + 512], writes=[bxts[jj]], q="sp")
                    S.dma(rc[jj], ropeC[:, tt0:tt0 + 512], writes=[brc[jj]], q="sp")
                    S.dma(rs_[jj], ropeS[:, tt0:tt0 + 512], writes=[brc[jj]], q="sp")
                if nt == 1:
                    p1_load(ti, rj)
                if nt < len(alltiles):
                    p1_load(alltiles[nt], nt % 2)
                rms_tile(xt, bxt, g1, h, bh, ps[0], pb[0], scr, bscr)
                if full:
                    S.dma(hT.rearrange("(k p) t -> p k t", p=128)[:, :, t0:t0 + 512], h, reads=[bh], q="sp")
                chunks = []
                if full:
                    chunks += [(0 + i * 128, QAt, i * 128, True) for i in range(6)]
                chunks += [(768 + i * 128, KAt, i * 128, True) for i in range(6)]
                if full:
                    chunks += [(2304 + i * 128, QBt, i * 128, True) for i in range(4)]
                chunks += [(2816 + i * 128, KBt, i * 128, True) for i in range(4)]
                chunks += [(3840 + i * 128, Ct, i * 128, False) for i in range(4)]
                def p1_main(ci):
                    co, dst, dr, rope = chunks[ci]
                    pi = 1 + (ci % 3)
                    for k in range(8):
                        b.mm(ps[pi], W[:, k, co:co + 128], h[:, k, :], k == 0, k == 7, [bw, bh], [pb[pi]])
                    j = ci % 3
                    if not rope:
                        b.act(qo[j], ps[pi], AF.Copy, [pb[pi]], [bqo[j]])
                    else:
                        b.act(qs[j], ps[pi], AF.Copy, [pb[pi]], [bqs[j]])

                def p1_post(ci):
                    co, dst, dr, rope = chunks[ci]
                    j = ci % 3
                    if rope:
                        pr = 4 + (ci % 2)
                        b.mm(ps[pr], Rblk, qs[j], True, True, [bc, bqs[j]], [pb[pr]])
                        b.tt(t1[j], qs[j], rc[rj], ALU.mult, [bqs[j], brc[rj]], [bt1[j]], eng="pool")
                        b.tt(qo[j], ps[pr], rs_[rj], ALU.mult, [pb[pr], brc[rj]], [bqo[j]])
                        b.tt(qo[j], qo[j], t1[j], ALU.add, [bqo[j], bt1[j]], [bqo[j]])
                    S.dma(dst[dr:dr + 128, t0:t0 + 512], qo[j], reads=[bqo[j]], q="sp")

                for ci in range(len(chunks)):
                    p1_main(ci)
                    if ci >= 1:
                        p1_post(ci - 1)
                p1_post(len(chunks) - 1)
                it += 1
                for s in range(4):
                    for (co, n, vo) in ((1536, 512, 0), (2048, 256, 512), (3328, 512, 768)):
                        pi = 1 + (it % 3); it += 1
                        for k in range(8):
                            b.mm(ps[pi][:, 0:n], h[:, k, s * 128:(s + 1) * 128], W[:, k, co:co + n], k == 0, k == 7,
                                 [bw, bh], [pb[pi]])
                        b.act(vsb[:, s, vo:vo + n], ps[pi][:, 0:n], AF.Copy, [pb[pi]], [bvs])
                S.dma(VA[t0:t0 + 512, :].rearrange("(s p) c -> p s c", p=128), vsb[:, :, 0:768], reads=[bvs], q="sp")
                S.dma(VB[t0:t0 + 512, :].rearrange("(s p) c -> p s c", p=128), vsb[:, :, 768:1280], reads=[bvs], q="sp")

        bconv = [Buf(f"conv{i}") for i in range(9)]

        def emit_conv(idx, g_src, u_src, d_src):
            for gi in range(14):
                S.dma(Wg_b[idx, gi].rearrange("p (k j) -> p k j", j=256),
                      g_src[:, gi * 256:(gi + 1) * 256].rearrange("(k p) j -> p k j", p=128),
                      writes=[bconv[idx]], q="pool", bg=True)
                S.dma(Wu_b[idx, gi].rearrange("p (k j) -> p k j", j=256),
                      u_src[:, gi * 256:(gi + 1) * 256].rearrange("(k p) j -> p k j", p=128),
                      writes=[bconv[idx]], q="pool", bg=True)
            for c in range(8):
                S.dma(Wd_b[idx, c].rearrange("p (f j) -> p f j", j=128),
                      d_src[:, c * 128:(c + 1) * 128].rearrange("(f p) j -> p f j", p=128),
                      writes=[bconv[idx]], q="pool", bg=True)

        def phase_p2b(l, qtiles):
            b.barrier()
            if l == 0:
                emit_conv(0, ffn_g, ffn_u, ffn_d)
                if n_layers > 1:
                    for e in range(NE):
                        emit_conv(1 + e, moe_g[e], moe_u[e], moe_d[e])
            nkb_max = (max(qtiles) + 1) * 4
            kts = [b.abf.alloc([T]) for _ in range(1)]; bkt = [Buf("kt") for _ in range(1)]
            vts = [b.abf.alloc([64, 128]) for _ in range(1)]; bvt = [Buf("vt") for _ in range(1)]
            qzs = [[b.abf.alloc([512]) for _ in range(2)] for _ in range(2)]; bqt = [Buf("qt") for _ in range(2)]
            for par in range(2):
                for c in range(2):
                    b.memset(qzs[par][c], 0.0, [bqt[par]])
            pts = [b.abf.alloc([512]) for _ in range(4)]; bpt = [Buf("pt") for _ in range(4)]
            rd = b.af.alloc([512]); o1 = b.af.alloc([512]); o2 = b.af.alloc([512]); lnb = b.af.alloc([512])
            sqb = b.abf.alloc([512]); ob = b.abf.alloc([512])
            be = Buf("epi"); bob = Buf("ob")
            accD = [b.af.alloc([512]) for _ in range(2)]; bacc = [Buf("accD") for _ in range(2)]
            dh = b.abf.alloc([512]); dl = b.abf.alloc([512]); bdh = Buf("dh")
            pending = [None]
            neglam = lam_sb[:, l, 4:5]
            gsub = vec_sb[:, l, 40:41]
            nq = 0
            for hh in range(4):
                kt = kts[0]; vt = vts[0]
                S.dma(kt[:, 0:nkb_max * 128], KBt[hh * 128:(hh + 1) * 128, 0:nkb_max * 128], writes=[bkt[0]], q="sp")
                for c8 in range(0, nkb_max, 8):
                    S.dma(vt[:, c8:c8 + 8, :],
                          VB[c8 * 128:(c8 + 8) * 128, hh * 128:(hh + 1) * 128].rearrange("(n p) c -> p n c", p=128),
                          writes=[bvt[0]], q="sp")
                for qi in qtiles:
                    q0 = qi * 512
                    own = q0 >= TO
                    qz = qzs[nq % 2]; bq = bqt[nq % 2]; nq += 1
                    S.dma(qz[0][0:64, :], QBt[hh * 128:hh * 128 + 64, q0:q0 + 512], writes=[bq], q="sp")
                    S.dma(qz[1][64:128, :], QBt[hh * 128 + 64:(hh + 1) * 128, q0:q0 + 512], writes=[bq], q="sp")
                    nkb = (qi + 1) * 4
                    if own:
                        kbs = list(range(0, nkb))
                    else:
                        kbs = list(range(0, nkb))
                    items = [(c, kb) for kb in kbs for c in (0, 1)]

                    def qk(i):
                        c, kb = items[i]
                        sb_i = i % 4
                        diag = kb >= qi * 4
                        rows = slice(64 * c, 64 * c + 64)
                        b.mm(ps[sb_i], kt[:, kb * 128:(kb + 1) * 128], qz[c], True, not diag,
                             [bkt[0], bq], [pb[sb_i]])
                        if diag:
                            b.mm(ps[sb_i], ident, maskB[:, kb - qi * 4, :], False, True, [bc], [pb[sb_i]])

                    LA = 3
                    n_it = len(items)
                    for _w in range(WARM_P2B if qi != qtiles[0] else WARM_B):
                        b.mm(ps[7], ones, maskB[:, 0, :], True, True, [bc], [pb[7]])
                    for i0 in range(min(LA, n_it)):
                        qk(i0)
                    for i, (c, kb) in enumerate(items):
                        if i + LA < n_it:
                            qk(i + LA)
                        sb_i = i % 4
                        pt = pts[i % 4]; bp = bpt[i % 4]
                        pref = own and kb < TO // 128
                        b.act(pt, ps[sb_i], AF.Exp, [pb[sb_i], bc], [bp], scale=0.125, bias=pbias if pref else None)
                        first = kb == kbs[0]; last = kb == kbs[-1]
                        b.mm(ps[4 + c], vt[:, kb, :], pt, first, last, [bvt[0], bp], [pb[4 + c]])
                        if first:
                            b.copy(accD[c], pt, [bp], [bacc[c]])
                        else:
                            b.tt(accD[c], accD[c], pt, ALU.add, [bp, bacc[c]], [bacc[c]])
                        if i == 6 and pending[0] is not None:
                            pending[0]()
                            pending[0] = None
                    if pending[0] is not None:
                        pending[0]()
                        pending[0] = None
                    for c in (0, 1):
                        b.copy(dh, accD[c], [bacc[c]], [bdh])
                        b.tt(dl, accD[c], dh, ALU.subtract, [bacc[c], bdh], [bdh])
                        b.mm(ps[6 + c], ones, dh, True, False, [bc, bdh], [pb[6 + c]])
                        b.mm(ps[6 + c], ones, dl, False, True, [bc, bdh], [pb[6 + c]])
                        b.act(rd, ps[6 + c], AF.Ln, [pb[6 + c]], [be])
                        b.act(rd, rd, AF.Exp, [be], [be], scale=-1.0)
                        b.tt(o1 if c == 0 else o2, ps[4 + c], rd, ALU.mult, [pb[4 + c], be], [be])
                    b.stt(o1, o2, neglam, o1, ALU.mult, ALU.add, [be, bc], [be])

                    def part2(hh=hh, q0=q0):
                        b.act(sqb, o1, AF.Square, [be], [be])
                        b.mm(ps[6], ones, sqb, True, True, [bc, be], [pb[6]])
                        b.act(lnb, ps[6], AF.Ln, [pb[6], bc], [be], scale=1.0 / 128.0, bias=epsc)
                        b.act(lnb, lnb, AF.Exp, [be], [be], scale=-0.5)
                        b.stt(ob, o1, gsub, lnb, ALU.mult, ALU.mult, [be, bc], [bob])
                        S.dma(OBt[hh * 128:(hh + 1) * 128, q0:q0 + 512], ob, reads=[bob], q="sp")
                    pending[0] = part2
            if pending[0] is not None:
                pending[0]()
                pending[0] = None

        def phase_p2a(l, segs):
            b.barrier()
            ka = b.abf.alloc([2, 4096]); bka = Buf("ka")
            qaz = [b.abf.alloc([2, 2048]) for _ in range(2)]; bqa = Buf("qa")
            for hp_ in range(2):
                b.memset(qaz[hp_], 0.0, [bqa])
            vts = [b.abf.alloc([2, 256]) for _ in range(3)]; bvts = [Buf("va") for _ in range(3)]
            pts = [b.abf.alloc([256]) for _ in range(3)]; bpts = [Buf("pa") for _ in range(3)]
            accN = b.af.alloc([2, 2048]); accD = b.af.alloc([2, 2048]); bacc = Buf("acc")
            oa = b.abf.alloc([2, 2048]); boa = Buf("oa")
            vbc = [0]; gic = [0]
            for sg in segs:
                s0 = sg * 2048
                first_seg = (sg == 0)
                own_first = (s0 == TO)
                for g, dil in enumerate((1, 4, 16)):
                    for p in range(2):
                        r0 = g * 256 + p * 128
                        if first_seg:
                            S.dma(ka[:, p, 2048:4096], KAt[r0:r0 + 128, s0:s0 + 2048], writes=[bka], q="sp")
                        else:
                            S.dma(ka[:, p, :], KAt[r0:r0 + 128, s0 - 2048:s0 + 2048], writes=[bka], q="sp")
                        S.dma(qaz[0][0:64, p, :], QAt[r0:r0 + 64, s0:s0 + 2048], writes=[bqa], q="sp")
                        S.dma(qaz[1][64:128, p, :], QAt[r0 + 64:r0 + 128, s0:s0 + 2048], writes=[bqa], q="sp")
                    nblk = 16 // dil
                    blocks = [(r, n) for r in range(dil) for n in range(nblk)]
                    nb = len(blocks)
                    vb0 = vbc[0]; vbc[0] += nb
                    g0 = gic[0]; gic[0] += nb * 4

                    def binfo(k):
                        r, n = blocks[k]
                        cs = n * 128 * dil + r
                        has_prev = not (first_seg and n == 0)
                        return r, n, cs, has_prev, cs - 128 * dil

                    def loadV(k):
                        r, n, cs, has_prev, ps_ = binfo(k)
                        vi = (vb0 + k) % 3
                        vt = vts[vi]; bv = bvts[vi]
                        tok = s0 + cs
                        S.dma(vt[:, 0, :], VA[tok:tok + 127 * dil + 1:dil, g * 256:(g + 1) * 256], writes=[bv], q="sp")
                        if has_prev:
                            tokp = s0 + ps_
                            S.dma(vt[:, 1, :], VA[tokp:tokp + 127 * dil + 1:dil, g * 256:(g + 1) * 256], writes=[bv], q="sp")

                    def stA(i):
                        k, hg = divmod(i, 4)
                        r, n, cs, has_prev, ps_ = binfo(k)
                        p = hg // 2; hp = hg % 2
                        rows = slice(64 * hp, 64 * hp + 64)
                        si = (g0 + i) % 3
                        pst = ps[si]; psb_ = pb[si]
                        qsl = slice(cs, cs + 127 * dil + 1, dil)
                        b.mm(pst[:, 0:128], ka[:, p, 2048 + cs:2048 + cs + 127 * dil + 1:dil], qaz[hp][:, p, qsl],
                             True, False, [bka, bqa], [psb_])
                        b.mm(pst[:, 0:128], ident, maskA[:, 0:128], False, True, [bc], [psb_])
                        if has_prev:
                            b.mm(pst[:, 128:256], ka[:, p, 2048 + ps_:2048 + ps_ + 127 * dil + 1:dil],
                                 qaz[hp][:, p, qsl], True, False, [bka, bqa], [psb_])
                            b.mm(pst[:, 128:256], ident, maskA[:, 128:256], False, True, [bc], [psb_])

                    def stBCD(i):
                        k, hg = divmod(i, 4)
                        r, n, cs, has_prev, ps_ = binfo(k)
                        p = hg // 2; hp = hg % 2
                        rows = slice(64 * hp, 64 * hp + 64)
                        si = (g0 + i) % 3
                        pst = ps[si]; psb_ = pb[si]
                        qsl = slice(cs, cs + 127 * dil + 1, dil)
                        vi = (vb0 + k) % 3
                        vt = vts[vi]; bv = bvts[vi]
                        pt = pts[si]; bp = bpts[si]
                        b.act(pt[:, 0:128], pst[:, 0:128], AF.Exp, [psb_], [bp], scale=0.125)
                        if has_prev:
                            pref = own_first and n == 0
                            b.act(pt[:, 128:256], pst[:, 128:256], AF.Exp, [psb_, bc], [bp], scale=0.125,
                                  bias=pbias if pref else None)
                        po = 3 + ((g0 + i) % 3)
                        pso = ps[po]; pbo = pb[po]
                        b.mm(pso[:, 0:128], vt[:, 0, p * 128:(p + 1) * 128], pt[:, 0:128], True, not has_prev,
                             [bv, bp], [pbo])
                        if has_prev:
                            b.mm(pso[:, 0:128], vt[:, 1, p * 128:(p + 1) * 128], pt[:, 128:256], False, True,
                                 [bv, bp], [pbo])
                        b.mm(pso[:, 128:256], ones, pt[:, 0:128], True, not has_prev, [bc, bp], [pbo])
                        if has_prev:
                            b.mm(pso[:, 128:256], ones, pt[:, 128:256], False, True, [bc, bp], [pbo])
                        if g == 0:
                            b.act(accN[rows, p, qsl], pso[rows, 0:128], AF.Copy, [pbo], [bacc])
                            b.copy(accD[rows, p, qsl], pso[rows, 128:256], [pbo], [bacc])
                        else:
                            b.tt(accN[rows, p, qsl], accN[rows, p, qsl], pso[rows, 0:128], ALU.add,
                                 [pbo, bacc], [bacc])
                            b.tt(accD[rows, p, qsl], accD[rows, p, qsl], pso[rows, 128:256], ALU.add,
                                 [pbo, bacc], [bacc], eng="dve")

                    n_it = nb * 4
                    for _w in range(WARM_B):
                        b.mm(ps[6], ones, maskB[:, 0, :], True, True, [bc], [pb[6]])
                    loadV(0)
                    if nb > 1:
                        loadV(1)
                    stA(0); stA(1)
                    for i in range(n_it):
                        k, hg = divmod(i, 4)
                        if hg == 0 and k + 2 < nb:
                            loadV(k + 2)
                        if i + 2 < n_it:
                            stA(i + 2)
                        stBCD(i)
                for p in range(2):
                    b.recip(accD[:, p, :], accD[:, p, :], [bacc], [bacc])
                    b.tt(oa[:, p, :], accN[:, p, :], accD[:, p, :], ALU.mult, [bacc], [boa])
                    S.dma(OAt[p * 128:(p + 1) * 128, s0:s0 + 2048], oa[:, p, :], reads=[boa], q="sp")

        def phase_p2c(l, halves):
            nonlocal bw
            b.barrier()
            bw = Buf("w")
            PW = b.abf.alloc([4, 128])
            for g in range(4):
                S.dma(PW[:, g, :], pool_w[l, g], writes=[bw], q="pool")
            cb = b.af.alloc([16 + 2048]); s2 = b.af.alloc([16 + 2048]); s4 = b.af.alloc([16 + 2048])
            icn = b.af.alloc([2048])
            bcb = Buf("cb"); bs = Buf("s"); bic = Buf("icn")
            cbf = b.abf.alloc([16 + 2048]); bcbf = Buf("cbf")
            dd = b.abf.alloc([2048]); bdd = Buf("dd")
            oc = [b.abf.alloc([512]) for _ in range(2)]; boc = [Buf("oc") for _ in range(2)]
            it = 0
            for hf in halves:
                for q4 in range(2):
                    t0 = hf * TO + q4 * 2048
                    for g, w in enumerate((2, 4, 8, 16)):
                        S.dma(cbf[:, 16:], Ct[g * 128:(g + 1) * 128, t0:t0 + 2048], writes=[bcbf], q="sp")
                        if t0 == 0:
                            b.memset(cbf[:, 0:16], 0.0, [bcbf])
                        else:
                            S.dma(cbf[:, 0:16], Ct[g * 128:(g + 1) * 128, t0 - 16:t0], writes=[bcbf], q="sp")
                        S.dma(icn, invcnt[g:g + 1, t0:t0 + 2048].broadcast_to([128, 2048]), writes=[bic], q="sp")
                        b.copy(cb, cbf, [bcbf], [bcb])
                        if t0 == TO:
                            b.ts(cb[:, 0:16], cb[:, 0:16], pflag, None, ALU.mult, None, [bcb, bc], [bcb])
                        N = 16 + 2048
                        b.tt(s2[:, 1:N], cb[:, 1:N], cb[:, 0:N - 1], ALU.add, [bcb], [bs])
                        cur = s2
                        if w >= 4:
                            b.tt(s4[:, 3:N], s2[:, 3:N], s2[:, 1:N - 2], ALU.add, [bs], [bs])
                            cur = s4
                        if w >= 8:
                            b.tt(s2[:, 7:N], s4[:, 7:N], s4[:, 3:N - 4], ALU.add, [bs], [bs])
                            cur = s2
                        if w >= 16:
                            b.tt(s4[:, 15:N], s2[:, 15:N], s2[:, 7:N - 8], ALU.add, [bs], [bs])
                            cur = s4
                        b.tt(cur[:, 16:N], cur[:, 16:N], icn, ALU.mult, [bs, bic], [bs])
                        b.tt(dd, cur[:, 16:N], cb[:, 16:N], ALU.subtract, [bs, bcb], [bdd])
                        for s in range(4):
                            pi = it % 4; it += 1
                            b.mm(ps[pi], PW[:, g, :], dd[:, s * 512:(s + 1) * 512], True, True, [bw, bdd], [pb[pi]])
                            j = it % 2
                            b.act(oc[j], ps[pi], AF.Copy, [pb[pi], bc], [boc[j]], scale=vec_sb[:, l, 41 + g:42 + g])
                            S.dma(OCt[g * 128:(g + 1) * 128, t0 + s * 512:t0 + (s + 1) * 512], oc[j], reads=[boc[j]], q="sp")

        def phase_p3(l, tiles):
            nonlocal bw
            b.barrier()
            bw = Buf("w")
            WG = b.abf.alloc([8, 3072])
            load_w(WG, w_in[l][:, 4352:7424], 8)
            WP = b.abf.alloc([10, 1024])
            load_w(WP[:, 0:2, :], w_pa[l], 2); load_w(WP[:, 2:6, :], w_pb[l], 4); load_w(WP[:, 6:10, :], w_pc[l], 4)
            WO = b.abf.alloc([8, 1024])
            load_w(WO, w_out[l], 8)
            h = b.abf.alloc([8, 512]); bh = Buf("h")
            om = b.abf.alloc([10, 512]); bom = Buf("om")
            mixed = b.abf.alloc([8, 512]); bmx = Buf("mixed")
            xt = b.af.alloc([8, 512]); bxt = Buf("xt")
            sg = [b.af.alloc([512]) for _ in range(2)]; bsg = [Buf("sg") for _ in range(2)]
            macc = b.af.alloc([512]); bma = Buf("macc")
            it = 0
            for ti in tiles:
                t0 = ti * 512
                S.dma(h, hT.rearrange("(k p) t -> p k t", p=128)[:, :, t0:t0 + 512], writes=[bh], q="sp")
                S.dma(om[:, 0:2, :], OAt.rearrange("(k p) t -> p k t", p=128)[:, :, t0:t0 + 512], writes=[bom], q="sp")
                S.dma(om[:, 2:6, :], OBt.rearrange("(k p) t -> p k t", p=128)[:, :, t0:t0 + 512], writes=[bom], q="sp")
                S.dma(om[:, 6:10, :], OCt.rearrange("(k p) t -> p k t", p=128)[:, :, t0:t0 + 512], writes=[bom], q="sp")
                S.dma(xt, xs.rearrange("(k p) t -> p k t", p=128)[:, :, t0:t0 + 512], writes=[bxt], q="sp")
                for c in range(8):
                    for br, (k0, k1) in enumerate(((0, 2), (2, 6), (6, 10))):
                        pg = (it % 3) * 2; it += 1
                        for k in range(8):
                            b.mm(ps[pg], WG[:, k, br * 1024 + c * 128: br * 1024 + (c + 1) * 128], h[:, k, :],
                                 k == 0, k == 7, [bw, bh], [pb[pg]])
                        for k in range(k0, k1):
                            b.mm(ps[pg + 1], WP[:, k, c * 128:(c + 1) * 128], om[:, k, :], k == k0, k == k1 - 1,
                                 [bw, bom], [pb[pg + 1]])
                        j = it % 2
                        b.act(sg[j], ps[pg], AF.Sigmoid, [pb[pg], bc], [bsg[j]], bias=vec_sb[:, l, 16 + br * 8 + c:17 + br * 8 + c])
                        if br == 0:
                            b.tt(macc, sg[j], ps[pg + 1], ALU.mult, [bsg[j], pb[pg + 1]], [bma])
                        else:
                            b.tt(sg[j], sg[j], ps[pg + 1], ALU.mult, [bsg[j], pb[pg + 1]], [bsg[j]])
                            if br == 1:
                                b.tt(macc, macc, sg[j], ALU.add, [bma, bsg[j]], [bma], eng="pool")
                            else:
                                b.tt(mixed[:, c, :], macc, sg[j], ALU.add, [bma, bsg[j]], [bmx], eng="pool")
                for c in range(8):
                    po = 6 + (c % 2)
                    for k in range(8):
                        b.mm(ps[po], WO[:, k, c * 128:(c + 1) * 128], mixed[:, k, :], k == 0, k == 7, [bw, bmx], [pb[po]])
                    b.tt(xt[:, c, :], xt[:, c, :], ps[po], ALU.add, [bxt, pb[po]], [bxt])
                S.dma(xs.rearrange("(k p) t -> p k t", p=128)[:, :, t0:t0 + 512], xt, reads=[bxt], q="sp")

        def phase_p4(l, tiles2, final):
            b.barrier()
            moe = (l % 2 == 1)
            wg = [b.abf.alloc([8, 256]) for _ in range(2)]
            wu = [b.abf.alloc([8, 256]) for _ in range(2)]
            bwb = [Buf("wbuf") for _ in range(2)]
            wdh = [b.abf.alloc([NF, 128]) for _ in range(2)]
            bwd = [Buf("wd") for _ in range(2)]
            actT = b.abf.alloc([NF, 1024]); bact = Buf("act")
            h = b.abf.alloc([8, 1024]); bh = Buf("h")
            lnb = b.af.alloc([1024]); rstd = b.af.alloc([1024]); bscr = Buf("scr")
            xt = b.af.alloc([8, 1024]); bxt = Buf("xt")
            sl = [b.abf.alloc([512]) for _ in range(2)]; bsl = [Buf("sl") for _ in range(2)]
            g2 = vec_sb[:, l, 8:16]
            if moe:
                hf = b.af.alloc([8, 512]); bhf = Buf("hf")
                sq = actT[:, 0:8, :]
            else:
                hbufs = [h, b.abf.alloc([8, 1024])]; bhb = [bh, Buf("h2")]
                sqd = b.af.alloc([2048]).bitcast(BF16).rearrange("p (k t) -> p k t", t=512); bsqd = Buf("sqd")
                xc = [b.af.alloc([1024]) for _ in range(2)]; bxc = [Buf("xc") for _ in range(2)]

            def prep_dense(ti, hb):
                t0_ = ti * 1024
                S.dma(xt, xs.rearrange("(k p) t -> p k t", p=128)[:, :, t0_:t0_ + 1024], writes=[bxt], q="sp")
                for hv in range(2):
                    hs = slice(hv * 512, (hv + 1) * 512)
                    for k in range(8):
                        b.act(sqd[:, k, :], xt[:, k, hs], AF.Square, [bxt], [bsqd])
                    for k in range(8):
                        b.mm(ps[0], ones, sqd[:, k, :], k == 0, k == 7, [bsqd, bc], [pb[0]])
                    b.act(lnb[:, hs], ps[0], AF.Ln, [pb[0], bc], [bscr], scale=1.0 / 1024.0, bias=epsc)
                b.act(rstd, lnb, AF.Exp, [bscr], [bscr], scale=-0.5)
                for k in range(8):
                    b.stt(hbufs[hb][:, k, :], xt[:, k, :], g2[:, k:k + 1], rstd, ALU.mult, ALU.mult,
                          [bxt, bscr, bc], [bhb[hb]])
            if moe:
                rw = sbt("rw", [128, 8, NE], F32)
                brw = Buf("rw")
                S.dma(rw[:, :, :], router[:, :, :], writes=[brw], q="sp")
                lg = b.af.alloc([4, 8]); mx = b.af.alloc([4, 8]); ex = b.af.alloc([4, 8]); msk = b.af.alloc([4, 8])
                den = b.af.alloc([4]); gT = b.abf.alloc([512]); gbc = b.abf.alloc([NE, 1024])
                gate_bf = b.abf.alloc([4, 8])
                sel = b.abf.alloc([NE, 128])
                bg = Buf("gate"); bgb = Buf("gbc"); bsel = Buf("sel")
                ytmp = [b.af.alloc([512]) for _ in range(2)]; byt = [Buf("yt") for _ in range(2)]
                for e in range(NE):
                    b.copy(sel[0:8, e, :], ident[0:8, e:e + 1].broadcast_to([8, 128]), [bc], [bsel])
            nexp = NE if moe else 1
            wcount = 0
            dcount = 0
            it = 0
            if not moe:
                prep_dense(tiles2[0], 0)
            for tix, ti in enumerate(tiles2):
                t0 = ti * 1024
                if not moe:
                    h = hbufs[tix % 2]; bh = bhb[tix % 2]
                else:
                    S.dma(xt, xs.rearrange("(k p) t -> p k t", p=128)[:, :, t0:t0 + 1024], writes=[bxt], q="sp")
                    for k in range(8):
                        b.act(sq[:, k, :], xt[:, k, :], AF.Square, [bxt], [bact])
                    for hv in range(2):
                        hs = slice(hv * 512, (hv + 1) * 512)
                        for k in range(8):
                            b.mm(ps[0], ones, sq[:, k, hs], k == 0, k == 7, [bact, bc], [pb[0]])
                        b.act(lnb[:, hs], ps[0], AF.Ln, [pb[0], bc], [bscr], scale=1.0 / 1024.0, bias=epsc)
                    b.act(rstd, lnb, AF.Exp, [bscr], [bscr], scale=-0.5)
                    for k in range(8):
                        b.stt(h[:, k, :], xt[:, k, :], g2[:, k:k + 1], rstd, ALU.mult, ALU.mult, [bxt, bscr, bc], [bh])
                if moe:
                    for hv in range(2):
                        hs = slice(hv * 512, (hv + 1) * 512)
                        for k in range(8):
                            b.stt(hf[:, k, :], xt[:, k, hs], g2[:, k:k + 1], rstd[:, hs], ALU.mult, ALU.mult,
                                  [bxt, bscr, bc], [bhf])
                        for s in range(4):
                            for k in range(8):
                                b.mm(ps[1][:, s * 8:(s + 1) * 8], hf[:, k, s * 128:(s + 1) * 128], rw[:, k, :], k == 0, k == 7,
                                     [bhf, brw], [pb[1]])
                        b.copy(lg, ps[1][:, 0:32].rearrange("p (s e) -> p s e", e=8), [pb[1]], [bg])
                        for s in range(4):
                            S.emit("dve", lambda e_, o=mx[:, s, :], i=lg[:, s, :]: e_.max(out=o, in_=i), [bg], [bg])
                            b.ts(den[:, s:s + 1], mx[:, s, 0:1], -1.0, None, ALU.mult, None, [bg], [bg])
                            b.act(ex[:, s, :], lg[:, s, :], AF.Exp, [bg], [bg], bias=den[:, s:s + 1])
                            b.ts(msk[:, s, :], lg[:, s, :], mx[:, s, 1:2], None, ALU.is_ge, None, [bg], [bg])
                            b.tt(ex[:, s, :], ex[:, s, :], msk[:, s, :], ALU.mult, [bg], [bg])
                            S.emit("dve", lambda e_, o=den[:, s:s + 1], i=ex[:, s, :]: e_.reduce_sum(
                                out=o, in_=i, axis=mybir.AxisListType.X), [bg], [bg])
                            b.recip(den[:, s:s + 1], den[:, s:s + 1], [bg], [bg])
                            b.ts(gate_bf[:, s, :], ex[:, s, :], den[:, s:s + 1], None, ALU.mult, None, [bg], [bg])
                        for s in range(4):
                            b.mm(ps[2][0:8, s * 128:(s + 1) * 128], gate_bf[:, s, :], ident, True, True, [bg, bc], [pb[2]])
                        b.copy(gT[0:8, :], ps[2][0:8, :], [pb[2]], [bg])
                        for e in range(NE):
                            b.mm(ps[3], sel[0:8, e, :], gT[0:8, :], True, True, [bsel, bg], [pb[3]])
                            b.act(gbc[:, e, hs], ps[3], AF.Copy, [pb[3]], [bgb])
                for e in range(nexp):
                    widx = 1 + e if moe else 0
                    for gi in range(14):
                        wb = wcount % 2; wcount += 1
                        S.dma(wg[wb], Wg_b[widx, gi].rearrange("p (k j) -> p k j", j=256), reads=[bconv[widx]],
                              writes=[bwb[wb]], q="sp")
                        S.dma(wu[wb], Wu_b[widx, gi].rearrange("p (k j) -> p k j", j=256), reads=[bconv[widx]],
                              writes=[bwb[wb]], q="sp")
                        for fi in range(2):
                            f = gi * 2 + fi
                            for k in range(8):
                                for hv in range(2):
                                    b.mm(ps[4 + hv], wg[wb][:, k, fi * 128:(fi + 1) * 128], h[:, k, hv * 512:(hv + 1) * 512],
                                         k == 0, k == 7, [bwb[wb], bh], [pb[4 + hv]])
                            for k in range(8):
                                for hv in range(2):
                                    b.mm(ps[6 + hv], wu[wb][:, k, fi * 128:(fi + 1) * 128], h[:, k, hv * 512:(hv + 1) * 512],
                                         k == 0, k == 7, [bwb[wb], bh], [pb[6 + hv]])
                            for hv in range(2):
                                j = it % 2; it += 1
                                b.act(sl[j], ps[4 + hv], AF.Silu, [pb[4 + hv]], [bsl[j]])
                                b.tt(actT[:, f, hv * 512:(hv + 1) * 512], sl[j], ps[6 + hv], ALU.mult,
                                     [bsl[j], pb[6 + hv]], [bact])
                    if (not moe) and tix + 1 < len(tiles2):
                        prep_dense(tiles2[tix + 1], (tix + 1) % 2)
                    for c in range(8):
                        db = dcount % 2; dcount += 1
                        S.dma(wdh[db], Wd_b[widx, c].rearrange("p (f j) -> p f j", j=128), reads=[bconv[widx]],
                              writes=[bwd[db]], q="sp")
                        pp = (1, 2) if c % 2 == 0 else (3, 5)
                        if not moe:
                            xj = c % 2
                            S.dma(xc[xj], xs[c * 128:(c + 1) * 128, t0:t0 + 1024], writes=[bxc[xj]], q="sp")
                        for f in range(NF):
                            for hv in range(2):
                                b.mm(ps[pp[hv]], wdh[db][:, f, :], actT[:, f, hv * 512:(hv + 1) * 512], f == 0, f == NF - 1,
                                     [bwd[db], bact], [pb[pp[hv]]])
                        for hv in range(2):
                            hs = slice(hv * 512, (hv + 1) * 512)
                            po = pp[hv]
                            if moe:
                                j = hv
                                b.tt(ytmp[j], ps[po], gbc[:, e, hs], ALU.mult, [pb[po], bgb], [byt[j]])
                                b.tt(xt[:, c, hs], xt[:, c, hs], ytmp[j], ALU.add, [bxt, byt[j]], [bxt], eng="pool")
                            else:
                                b.tt(xc[xj][:, hs], xc[xj][:, hs], ps[po], ALU.add, [bxc[xj], pb[po]], [bxc[xj]])
                        if not moe:
                            S.dma(xs[c * 128:(c + 1) * 128, t0:t0 + 1024], xc[xj], reads=[bxc[xj]], q="sp")
                if final:
                    for k in range(8):
                        b.act(sq[:, k, :], xt[:, k, :], AF.Square, [bxt], [bact])
                    for hv in range(2):
                        hs = slice(hv * 512, (hv + 1) * 512)
                        for k in range(8):
                            b.mm(ps[0], ones, sq[:, k, hs], k == 0, k == 7, [bact, bc], [pb[0]])
                        b.act(lnb[:, hs], ps[0], AF.Ln, [pb[0], bc], [bscr], scale=1.0 / 1024.0, bias=epsc)
                    b.act(rstd, lnb, AF.Exp, [bscr], [bscr], scale=-0.5)
                    for hv in range(2):
                        hs = slice(hv * 512, (hv + 1) * 512)
                        for k in range(8):
                            b.stt(hf[:, k, :], xt[:, k, hs], fing_sb[:, k:k + 1], rstd[:, hs], ALU.mult, ALU.mult,
                                  [bxt, bscr, bc], [bhf])
                        S.dma(outT.rearrange("(k p) t -> p k t", p=128)[:, :, t0 - TO + hv * 512:t0 - TO + (hv + 1) * 512],
                              hf, reads=[bhf], q="sp")
                elif moe:
                    S.dma(xs.rearrange("(k p) t -> p k t", p=128)[:, :, t0:t0 + 1024], xt, reads=[bxt], q="sp")

        all_tiles = list(range(16)); own_tiles = list(range(8, 16))
        for l in range(n_layers):
            last = (l == n_layers - 1)
            if not last:
                phase_p1(l, all_tiles, all_tiles)
                phase_p2b(l, all_tiles)
                phase_p2a(l, [0, 1, 2, 3])
                phase_p2c(l, [0, 1])
                phase_p3(l, all_tiles)
                phase_p4(l, list(range(8)), False)
            else:
                phase_p1(l, own_tiles, all_tiles)
                phase_p2b(l, own_tiles)
                phase_p2a(l, [2, 3])
                phase_p2c(l, [1])
                phase_p3(l, own_tiles)
                phase_p4(l, list(range(4, 8)), True)
        if debug:
            b.barrier()
            for k in range(8):
                S.dma(dbg[k * 128:(k + 1) * 128, :], xs[k * 128:(k + 1) * 128, :], q="sp")
        S.run(nc)
    return nc


def _consts():
    ident = np.eye(128, dtype=np.float32)
    ones = np.ones((128, 128), np.float32)
    R = np.zeros((128, 128), np.float32)
    for hb in (0, 64):
        for d in range(16):
            src = d + 8 if d < 8 else d - 8
            R[hb + src, hb + d] = 1.0
    k = np.arange(128)[:, None]
    q = np.arange(128)[None, :]
    cur = np.where(q >= k, 0.0, NEGM).astype(np.float32)
    prev = np.where(k >= q, 0.0, NEGM).astype(np.float32)
    maskA = np.concatenate([cur, prev], axis=1)
    q5 = np.arange(512)[None, :]
    maskB = np.concatenate([np.where(128 * i + k <= q5, 0.0, NEGM).astype(np.float32) for i in range(4)], axis=1)
    inv_freq = (500000.0 ** (-np.arange(0, 16, 2, dtype=np.float32) / 16.0)).astype(np.float32)
    freq = np.zeros((128, 1), np.float32)
    sgn = np.zeros((128, 1), np.float32)
    for p in range(128):
        d = p % 64
        if d < 16:
            freq[p, 0] = inv_freq[d % 8] / (2.0 * np.pi)
            sgn[p, 0] = -1.0 if d < 8 else 1.0
    return np.concatenate([ident, ones, R, maskA, maskB, freq, sgn], axis=1).astype(np.float32)


def _feat(v):
    v = np.asarray(v, np.float32)
    return np.ascontiguousarray(v.reshape(-1, 128).T)


_NC_CACHE = {}


def make_in_maps(x, positions, norm1_g, w_in, b_gate, diff_lambda, diff_subln_g, pool_w, pool_scale,
                 w_proj_a, w_proj_b, w_proj_c, w_out, norm2_g, ffn_w_gate, ffn_w_up, ffn_w_down,
                 moe_router, moe_w_gate, moe_w_up, moe_w_down, final_norm_g):
    f32 = lambda a: np.ascontiguousarray(np.asarray(a, dtype=np.float32))
    x = f32(x); positions = np.asarray(positions).astype(np.int32)
    consts = _consts()
    vecs = np.zeros((2, 128, 64), np.float32)
    for l in range(2):
        vecs[l, :, 0:8] = _feat(norm1_g[l])
        vecs[l, :, 8:16] = _feat(norm2_g[l])
        for i in range(3):
            vecs[l, :, 16 + i * 8:24 + i * 8] = _feat(b_gate[l][i])
        vecs[l, :, 40] = np.asarray(diff_subln_g[l], np.float32)
        vecs[l, :, 41:45] = _feat(pool_scale[l])
    lam_in = f32(diff_lambda).reshape(2, 1, 256)
    shared = {
        "consts": consts, "vecs": vecs, "lam_in": lam_in, "fin_g": _feat(final_norm_g),
        "w_in": f32(w_in), "pool_w": f32(pool_w), "w_pa": f32(w_proj_a), "w_pb": f32(w_proj_b),
        "w_pc": f32(w_proj_c), "w_out": f32(w_out), "ffn_g": f32(ffn_w_gate)[0], "ffn_u": f32(ffn_w_up)[0],
        "ffn_d": f32(ffn_w_down)[0],
        "router": np.ascontiguousarray(f32(moe_router)[0].reshape(8, 128, NE).transpose(1, 0, 2)),
        "moe_g": f32(moe_w_gate)[0], "moe_u": f32(moe_w_up)[0], "moe_d": f32(moe_w_down)[0],
    }
    in_maps = []
    tpos = np.arange(8192)
    for c in range(8):
        bi, j = c // 2, c % 2
        order = np.concatenate([np.arange(4096), np.arange(4096, 8192)]) if j == 1 else \
            np.concatenate([np.arange(4096, 8192), np.arange(4096)])
        m = dict(shared)
        m["xT"] = np.ascontiguousarray(x[bi][order].T)
        m["pos"] = np.ascontiguousarray(positions[bi][order][None, :])
        tp = tpos[order]
        m["invcnt"] = np.stack([1.0 / np.minimum(tp + 1, w) for w in (2, 4, 8, 16)]).astype(np.float32)
        pc = np.zeros((128, 2), np.float32)
        pc[:, 0] = 0.0 if j == 1 else NEGM
        pc[:, 1] = 1.0 if j == 1 else 0.0
        m["percore"] = pc
        in_maps.append(m)
    return in_maps


def kernel(**inputs):
    if "nc" not in _NC_CACHE:
        _NC_CACHE["nc"] = build_program()
    nc = _NC_CACHE["nc"]
    in_maps = make_in_maps(**inputs)
    res = run_bass_kernel_spmd(nc, in_maps, core_ids=list(range(8)))
    out = np.zeros((4, 8192, 1024), np.float32)
    for c in range(8):
        bi, j = c // 2, c % 2
        out[bi, j * 4096:(j + 1) * 4096, :] = res.results[c]["outT"].T
    return out
```

```python
import math
from contextlib import ExitStack

import numpy as np
import concourse.bass as bass
import concourse.mybir as mybir
from concourse.bass_utils import run_bass_kernel_spmd

F32 = mybir.dt.float32
BF16 = mybir.dt.bfloat16
I32 = mybir.dt.int32
AF = mybir.ActivationFunctionType
ALU = mybir.AluOpType

SEM_BLOCK = 16384
N_ENG_SEMS = 12
N_DMA_SLOTS = 24
SAME_ENGINE_SYNC = True

D = 1024
T = 8192
TO = 4096
NEGM = -30000.0
EPS = 1e-6
DFF = 3584
NF = DFF // 128
NE = 8
TWO_PI = 6.28318
WARM_B = 12
WARM_P2B = 0


class Buf:
    __slots__ = ("name", "w", "r", "rd")

    def __init__(self, name=""):
        self.name = name
        self.w = None
        self.r = {}
        self.rd = []


class Op:
    __slots__ = ("idx", "eng", "fn", "deps", "needs_inc", "dma", "cnt", "slot", "sval")

    def __init__(self, idx, eng, fn, dma):
        self.idx = idx
        self.eng = eng
        self.fn = fn
        self.deps = []
        self.needs_inc = False
        self.dma = dma
        self.cnt = None
        self.slot = None
        self.sval = None


class Sched:
    ENGS = ("pe", "act", "dve", "pool", "sp")

    def __init__(self):
        self.ops = []
        self.last = {e: None for e in self.ENGS}
        self.dmas = {e: [] for e in self.ENGS}

    def emit(self, eng, fn, reads=(), writes=(), dma=False, extra=(), bg=False):
        op = Op(len(self.ops), eng, fn, dma)
        deps = {}
        for b in reads:
            if b.w is not None:
                deps[b.w.idx] = b.w
        for b in writes:
            if b.w is not None:
                deps[b.w.idx] = b.w
            for o in b.r.values():
                deps[o.idx] = o
            for o in b.rd:
                deps[o.idx] = o
        for o in extra:
            if o is not None:
                deps[o.idx] = o
        for d in deps.values():
            if (not d.dma) and d.eng == eng and (eng == "pe" or not SAME_ENGINE_SYNC):
                continue
            op.deps.append(d)
            d.needs_inc = True
        for b in reads:
            if dma:
                b.rd.append(op)
            else:
                b.r[eng] = op
        for b in writes:
            b.w = op
            b.r = {}
            b.rd = []
        self.ops.append(op)
        if dma:
            if not bg:
                self.dmas[eng].append(op)
        else:
            self.last[eng] = op
        return op

    def dma(self, out, in_, reads=(), writes=(), q="sp", bg=False):
        return self.emit(q, lambda e, o=out, i=in_: e.dma_start(out=o, in_=i), reads, writes, dma=True, bg=bg)

    def finalize(self):
        cnt = {e: 0 for e in self.ENGS}
        ndma = {e: 0 for e in self.ENGS}
        self.dma_ops = {e: [] for e in self.ENGS}
        for op in self.ops:
            if op.dma:
                i = ndma[op.eng]
                ndma[op.eng] += 1
                op.slot = i % N_DMA_SLOTS
                op.sval = 16 * (i // N_DMA_SLOTS + 1)
                if i >= N_DMA_SLOTS:
                    op.deps.append(self.dma_ops[op.eng][i - N_DMA_SLOTS])
                self.dma_ops[op.eng].append(op)
            elif op.needs_inc:
                cnt[op.eng] += 1
                op.cnt = cnt[op.eng]
        self.cnt = cnt
        self.ndma = ndma

    def run(self, nc):
        self.finalize()
        with ExitStack() as st:
            esems = {}
            for e in self.ENGS:
                nblk = (self.cnt[e] + SEM_BLOCK - 1) // SEM_BLOCK
                assert nblk <= N_ENG_SEMS, (e, self.cnt[e])
                esems[e] = [st.enter_context(nc.semaphore(f"s_{e}{k}")) for k in range(max(nblk, 1))]
            dsems = {}
            for e in self.ENGS:
                if self.ndma[e]:
                    dsems[e] = [st.enter_context(nc.semaphore(f"d_{e}{k}")) for k in range(N_DMA_SLOTS)]
            block = st.enter_context(nc.Block())

            def target(d):
                if d.dma:
                    return ("d", d.eng, d.slot), dsems[d.eng][d.slot], d.sval, d.sval
                blk = (d.cnt - 1) // SEM_BLOCK
                return ("c", d.eng), esems[d.eng][blk], (d.cnt - 1) % SEM_BLOCK + 1, d.cnt

            by_eng = {e: [o for o in self.ops if o.eng == e] for e in self.ENGS}

            def body(ename):
                def f(eng):
                    known = {}
                    for op in by_eng[ename]:
                        for d in op.deps:
                            key, sem, val, gval = target(d)
                            if known.get(key, 0) >= gval:
                                continue
                            eng.wait_ge(sem, val)
                            known[key] = gval
                        inst = op.fn(eng)
                        if op.dma:
                            inst.then_inc(dsems[ename][op.slot], 16)
                        elif op.needs_inc:
                            blk = (op.cnt - 1) // SEM_BLOCK
                            inst.then_inc(esems[ename][blk], 1)
                    if ename == "sp":
                        for q in self.ENGS:
                            for o in self.dma_ops[q][-N_DMA_SLOTS:]:
                                key, sem, val, gval = target(o)
                                if known.get(key, 0) >= gval:
                                    continue
                                eng.wait_ge(sem, val)
                                known[key] = gval
                return f

            block.tensor(body("pe"))
            block.scalar(body("act"))
            block.vector(body("dve"))
            block.gpsimd(body("pool"))
            block.sync(body("sp"))


class Arena:
    def __init__(self, ap, n):
        self.ap = ap
        self.n = n
        self.off = 0

    def reset(self):
        self.off = 0

    def alloc(self, shape):
        n = 1
        for s in shape:
            n *= s
        assert self.off + n <= self.n, ("arena overflow", self.off, n, self.n)
        a = self.ap[:, self.off:self.off + n]
        self.off += n
        if len(shape) == 2:
            a = a.rearrange("p (a b) -> p a b", b=shape[1])
        elif len(shape) == 3:
            a = a.rearrange("p (a b c) -> p a b c", b=shape[1], c=shape[2])
        return a


class B:
    def __init__(self, nc, st):
        self.nc = nc
        self.S = Sched()
        self.st = st
        self.nbuf = 0

    def buf(self, name="b"):
        self.nbuf += 1
        return Buf(f"{name}{self.nbuf}")

    def mm(self, out, lhsT, rhs, start, stop, reads, writes):
        return self.S.emit("pe", lambda e, o=out, l=lhsT, r=rhs, a=start, b=stop:
                           e.matmul(o, l, r, start=a, stop=b), reads, writes)

    def act(self, out, in_, func, reads, writes, scale=1.0, bias=None, eng="act"):
        if bias is None:
            fn = lambda e, o=out, i=in_, f=func, s=scale: e.activation(out=o, in_=i, func=f, scale=s)
        else:
            fn = lambda e, o=out, i=in_, f=func, s=scale, b=bias: e.activation(out=o, in_=i, func=f, scale=s, bias=b)
        return self.S.emit(eng, fn, reads, writes)

    def tt(self, out, in0, in1, op, reads, writes, eng="dve"):
        return self.S.emit(eng, lambda e, o=out, a=in0, b=in1, p=op: e.tensor_tensor(out=o, in0=a, in1=b, op=p),
                           reads, writes)

    def ts(self, out, in0, s1, s2, op0, op1, reads, writes, eng="dve"):
        if s2 is None:
            fn = lambda e, o=out, a=in0, x=s1, p=op0: e.tensor_scalar(out=o, in0=a, scalar1=x, scalar2=None, op0=p)
        else:
            fn = lambda e, o=out, a=in0, x=s1, y=s2, p=op0, q=op1: e.tensor_scalar(
                out=o, in0=a, scalar1=x, scalar2=y, op0=p, op1=q)
        return self.S.emit(eng, fn, reads, writes)

    def stt(self, out, in0, scalar, in1, op0, op1, reads, writes, eng="dve"):
        return self.S.emit(eng, lambda e, o=out, a=in0, s=scalar, b=in1, p=op0, q=op1:
                           e.scalar_tensor_tensor(out=o, in0=a, scalar=s, in1=b, op0=p, op1=q), reads, writes)

    def copy(self, out, in_, reads, writes, eng="dve"):
        return self.S.emit(eng, lambda e, o=out, i=in_: e.tensor_copy(out=o, in_=i), reads, writes)

    def recip(self, out, in_, reads, writes):
        return self.S.emit("dve", lambda e, o=out, i=in_: e.reciprocal(out=o, in_=i), reads, writes)

    def memset(self, ap, val, writes, eng="dve"):
        return self.S.emit(eng, lambda e, a=ap, v=val: e.memset(a, v), (), writes)

    def barrier(self):
        S = self.S
        deps = [S.last[e] for e in S.ENGS]
        for q in S.ENGS:
            deps += S.dmas[q][-N_DMA_SLOTS:]
        z = self.bar_t
        S.emit("act", lambda e: e.memset(z[:, 0:1], 0.0) if hasattr(e, "memset") else e.activation(
            out=z[:, 0:1], in_=z[:, 0:1], func=AF.Copy), (), (), extra=deps)
        S.emit("dve", lambda e: e.memset(z[:, 1:2], 0.0), (), (), extra=deps)
        S.emit("pool", lambda e: e.memset(z[:, 2:3], 0.0), (), (), extra=deps)
        S.emit("pe", lambda e: e.matmul(self.bar_ps[0:1, 0:1], self.ones[0:1, 0:1], self.ones[0:1, 0:1],
                                        start=True, stop=True), (), (), extra=deps)
        S.emit("sp", lambda e: e.dma_start(out=self.bar_d[0:1, 0:8], in_=self.bar_d[1:2, 0:8]), (), (),
               dma=True, extra=deps)
        self.abf.reset()
        self.af.reset()


def build_program(debug=False, n_layers=2, dense_moe=True):
    nc = bass.Bass("TRN2", target_bir_lowering=False)
    dt_in = lambda n, s, d=F32: nc.dram_tensor(n, list(s), d, kind="ExternalInput").ap()
    dt_sc = lambda n, s, d=BF16: nc.dram_tensor(n, list(s), d, kind="Internal").ap()

    xT = dt_in("xT", [D, T])
    pos = dt_in("pos", [1, T], I32)
    invcnt = dt_in("invcnt", [4, T])
    percore = dt_in("percore", [128, 2])
    consts = dt_in("consts", [128, 128 * 3 + 256 + 2048 + 2])
    vecs = dt_in("vecs", [2, 128, 64])
    lam_in = dt_in("lam_in", [2, 1, 256])
    fin_g = dt_in("fin_g", [128, 8])
    w_in = dt_in("w_in", [2, D, 7424])
    pool_w = dt_in("pool_w", [2, 4, 128, 128])
    w_pa = dt_in("w_pa", [2, 256, D])
    w_pb = dt_in("w_pb", [2, 512, D])
    w_pc = dt_in("w_pc", [2, 512, D])
    w_out = dt_in("w_out", [2, D, D])
    ffn_g = dt_in("ffn_g", [D, DFF])
    ffn_u = dt_in("ffn_u", [D, DFF])
    ffn_d = dt_in("ffn_d", [DFF, D])
    router = dt_in("router", [128, 8, NE])
    moe_g = dt_in("moe_g", [NE, D, DFF])
    moe_u = dt_in("moe_u", [NE, D, DFF])
    moe_d = dt_in("moe_d", [NE, DFF, D])
    outT = nc.dram_tensor("outT", [D, TO], F32, kind="ExternalOutput").ap()
    dbg = nc.dram_tensor("dbg", [D, T], F32, kind="ExternalOutput").ap() if debug else None

    xs = dt_sc("xs", [D, T], F32)
    hT = dt_sc("hT", [D, T])
    QAt = dt_sc("QAt", [768, T]); KAt = dt_sc("KAt", [768, T]); VA = dt_sc("VA", [T, 768])
    QBt = dt_sc("QBt", [512, T]); KBt = dt_sc("KBt", [512, T]); VB = dt_sc("VB", [T, 512])
    Ct = dt_sc("Ct", [512, T])
    OAt = dt_sc("OAt", [256, T]); OBt = dt_sc("OBt", [512, T]); OCt = dt_sc("OCt", [512, T])
    bar_d = dt_sc("bar_d", [2, 8], F32)
    Wg_b = dt_sc("Wg_b", [9, 14, 128, 2048]); Wu_b = dt_sc("Wu_b", [9, 14, 128, 2048])
    Wd_b = dt_sc("Wd_b", [9, 8, 128, 3584])

    with ExitStack() as st:
        b = B(nc, st)
        S = b.S
        sbt = lambda n, s, d: st.enter_context(nc.sbuf_tensor(n, list(s), d))
        NBF = 63600
        NF32 = 16128
        abf_t = sbt("abf", [128, NBF], BF16)
        af_t = sbt("af32", [128, NF32], F32)
        b.abf = Arena(abf_t, NBF)
        b.af = Arena(af_t, NF32)
        cst = sbt("cst", [128, 128 * 3 + 256 + 2048], BF16)
        cf = sbt("cf", [128, 8], F32)
        ropeC = dt_sc("ropeC", [128, T])
        ropeS = dt_sc("ropeS", [128, T])
        rtab = sbt("rtab", [128, 2, 2048], BF16)
        brt = Buf("rtab")
        vec_sb = sbt("vec_sb", [128, 2, 64], F32)
        lam_sb = sbt("lam_sb", [128, 2, 8], F32)
        fing_sb = sbt("fing_sb", [128, 8], F32)
        b.bar_t = sbt("bar_t", [128, 4], F32)
        b.bar_d = bar_d
        ps = [st.enter_context(nc.psum_tensor(f"ps{i}", [128, 512], F32))[:, :] for i in range(8)]
        pb = [Buf(f"ps{i}") for i in range(8)]
        b.bar_ps = st.enter_context(nc.psum_tensor("barps", [128, 16], F32)) if False else ps[7]
        ident = cst[:, 0:128]
        ones = cst[:, 128:256]
        Rblk = cst[:, 256:384]
        maskA = cst[:, 384:640]
        maskB = cst[:, 640:2688].rearrange("p (i q) -> p i q", q=512)
        b.ones = ones
        freq = cf[:, 0:1]; sgn = cf[:, 1:2]; pbias = cf[:, 2:3]; pflag = cf[:, 3:4]; epsc = cf[:, 4:5]
        bc = Buf("consts")

        S.dma(cst[:, :], consts[:, 0:2688], writes=[bc], q="pool")
        S.dma(cf[:, 0:2], consts[:, 2688:2690], writes=[bc], q="sp")
        S.dma(cf[:, 2:4], percore[:, :], writes=[bc], q="sp")
        b.memset(cf[:, 4:5], EPS, [bc])
        S.dma(vec_sb[:, 0, :], vecs[0], writes=[bc], q="sp")
        S.dma(vec_sb[:, 1, :], vecs[1], writes=[bc], q="sp")
        S.dma(fing_sb[:, :], fin_g[:, :], writes=[bc], q="sp")
        for k in range(8):
            S.dma(xs[k * 128:(k + 1) * 128, :], xT[k * 128:(k + 1) * 128, :], q="sp")
        lraw = b.af.alloc([2, 256])
        blr = Buf("lraw")
        for l in range(2):
            S.dma(lraw[:, l, :], lam_in[l].broadcast_to([128, 256]), writes=[blr], q="sp")
        for l in range(2):
            linit = 0.8 - 0.6 * math.exp(-0.3 * l)
            pr = b.af.alloc([128])
            b.tt(pr[:, 0:64], lraw[:, l, 0:64], lraw[:, l, 64:128], ALU.mult, [blr], [blr])
            b.tt(pr[:, 64:128], lraw[:, l, 128:192], lraw[:, l, 192:256], ALU.mult, [blr], [blr])
            S.emit("dve", lambda e, o=lam_sb[:, l, 0:1], i=pr[:, 0:64]: e.reduce_sum(out=o, in_=i, axis=mybir.AxisListType.X), [blr], [bc])
            S.emit("dve", lambda e, o=lam_sb[:, l, 1:2], i=pr[:, 64:128]: e.reduce_sum(out=o, in_=i, axis=mybir.AxisListType.X), [blr], [bc])
            b.act(lam_sb[:, l, 2:4], lam_sb[:, l, 0:2], AF.Exp, [bc], [bc])
            b.stt(lam_sb[:, l, 4:5], lam_sb[:, l, 3:4], -linit, lam_sb[:, l, 2:3], ALU.add, ALU.subtract, [bc], [bc])
            b.ts(vec_sb[:, l, 40:41], vec_sb[:, l, 40:41], 1.0 - linit, None, ALU.mult, None, [bc], [bc])
        posi = b.af.alloc([2048]).bitcast(I32)
        bpi = Buf("posi")
        u = b.af.alloc([2048]); kf = b.af.alloc([2048]); fr = b.af.alloc([2048])
        bu = Buf("u")
        for ch in range(4):
            sl = slice(ch * 2048, (ch + 1) * 2048)
            S.dma(posi[:, :], pos[0:1, sl].broadcast_to([128, 2048]), writes=[bpi], q="sp")
            b.copy(u, posi[:, :], [bpi], [bu])
            b.ts(u, u, freq, None, ALU.mult, None, [bu, bc], [bu])
            for which, tab in ((0, ropeS), (1, ropeC)):
                if which == 1:
                    b.ts(u, u, 0.25, None, ALU.add, None, [bu], [bu])
                b.copy(posi[:, :], u, [bu], [bpi])
                b.copy(kf, posi[:, :], [bpi], [bu])
                b.tt(fr, u, kf, ALU.subtract, [bu], [bu])
                b.act(fr, fr, AF.Sin, [bu], [bu], scale=TWO_PI)
                if which == 0:
                    b.ts(rtab[:, 0, :], fr, sgn, None, ALU.mult, None, [bu, bc], [brt])
                else:
                    b.copy(rtab[:, 1, :], fr, [bu], [brt])
                S.dma(tab[:, sl], rtab[:, which, :], reads=[brt], q="sp")

        def load_w(dst, src, k_chunks, q="pool"):
            for k in range(k_chunks):
                S.dma(dst[:, k, :], src[k * 128:(k + 1) * 128, :], writes=[bw], q=q)

        def rms_tile(xt, bxt, gcol, h, bh, psb, psbuf, scr, bscr, n=512, kch=8, dim=1024.0, bsq=None):
            sq = scr["sq"]
            if bsq is None:
                bsq = bscr
            for k in range(kch):
                b.act(sq[:, k, :], xt[:, k, :], AF.Square, [bxt], [bsq])
            for k in range(kch):
                b.mm(psb[:, 0:n], ones, sq[:, k, :], k == 0, k == kch - 1, [bsq, bc], [psbuf])
            b.act(scr["ln"], psb[:, 0:n], AF.Ln, [psbuf, bc], [bscr], scale=1.0 / dim, bias=epsc)
            b.act(scr["rstd"], scr["ln"], AF.Exp, [bscr], [bscr], scale=-0.5)
            for k in range(kch):
                b.stt(h[:, k, :], xt[:, k, :], gcol[:, k:k + 1], scr["rstd"], ALU.mult, ALU.mult,
                      [bxt, bscr, bc], [bh])

        bw = Buf("w")

        def phase_p1(l, tiles_full, tiles_kvc):
            nonlocal bw
            b.barrier()
            bw = Buf("w")
            W = b.abf.alloc([8, 4352])
            load_w(W, w_in[l][:, 0:4352], 8)
            xts = [b.af.alloc([8, 512]) for _ in range(2)]
            bxts = [Buf("xt") for _ in range(2)]
            scrs = [{"sq": b.abf.alloc([8, 512]), "ln": b.af.alloc([512]), "rstd": b.af.alloc([512])} for _ in range(2)]
            bscrs = [Buf("scr") for _ in range(2)]
            hs_ = [b.abf.alloc([8, 512]) for _ in range(2)]; bhs = [Buf("h") for _ in range(2)]
            qs = [b.abf.alloc([512]) for _ in range(3)]; bqs = [Buf("q") for _ in range(3)]
            t1 = [b.af.alloc([512]) for _ in range(3)]; bt1 = [Buf("t1") for _ in range(3)]
            qo = [b.abf.alloc([512]) for _ in range(3)]; bqo = [Buf("qo") for _ in range(3)]
            vsb = b.abf.alloc([4, 1280]); bvs = Buf("vsb")
            g1 = vec_sb[:, l, 0:8]
            rc = [b.abf.alloc([512]) for _ in range(2)]; rs_ = [b.abf.alloc([512]) for _ in range(2)]
            brc = [Buf("rc") for _ in range(2)]
            nt = 0
            alltiles = sorted(set(tiles_full) | set(tiles_kvc))
            it = 0
            for ti in alltiles:
                t0 = ti * 512
                full = ti in tiles_full
                rj = nt % 2; nt += 1
                xt = xts[rj]; bxt = bxts[rj]
                h = hs_[rj]; bh = bhs[rj]; scr = scrs[rj]; bscr = bscrs[rj]

                def p1_load(tix_, jj):
                    tt0 = tix_ * 512
                    S.dma(xts[jj], xs.rearrange("(k p) t -> p k t", p=128)[:, :, tt0:tt0 + 512], writes=[bxts[jj]], q="sp")
                    S.dma(rc[jj], ropeC[:, tt0:tt0 + 512], writes=[brc[jj]], q="sp")
                    S.dma(rs_[jj], ropeS[:, tt0:tt0 + 512], writes=[brc[jj]], q="sp")
                if nt == 1:
                    p1_load(ti, rj)
                if nt < len(alltiles):
                    p1_load(alltiles[nt], nt % 2)
                rms_tile(xt, bxt, g1, h, bh, ps[0], pb[0], scr, bscr)
                if full:
                    S.dma(hT.rearrange("(k p) t -> p k t", p=128)[:, :, t0:t0 + 512], h, reads=[bh], q="sp")
                chunks = []
                if full:
                    chunks += [(0 + i * 128, QAt, i * 128, True) for i in range(6)]
                chunks += [(768 + i * 128, KAt, i * 128, True) for i in range(6)]
                if full:
                    chunks += [(2304 + i * 128, QBt, i * 128, True) for i in range(4)]
                chunks += [(2816 + i * 128, KBt, i * 128, True) for i in range(4)]
                chunks += [(3840 + i * 128, Ct, i * 128, False) for i in range(4)]
                def p1_main(ci):
                    co, dst, dr, rope = chunks[ci]
                    pi = 1 + (ci % 3)
                    for k in range(8):
                        b.mm(ps[pi], W[:, k, co:co + 128], h[:, k, :], k == 0, k == 7, [bw, bh], [pb[pi]])
                    j = ci % 3
                    if not rope:
                        b.act(qo[j], ps[pi], AF.Copy, [pb[pi]], [bqo[j]])
                    else:
                        b.act(qs[j], ps[pi], AF.Copy, [pb[pi]], [bqs[j]])

                def p1_post(ci):
                    co, dst, dr, rope = chunks[ci]
                    j = ci % 3
                    if rope:
                        pr = 4 + (ci % 2)
                        b.mm(ps[pr], Rblk, qs[j], True, True, [bc, bqs[j]], [pb[pr]])
                        b.tt(t1[j], qs[j], rc[rj], ALU.mult, [bqs[j], brc[rj]], [bt1[j]], eng="pool")
                        b.tt(qo[j], ps[pr], rs_[rj], ALU.mult, [pb[pr], brc[rj]], [bqo[j]])
                        b.tt(qo[j], qo[j], t1[j], ALU.add, [bqo[j], bt1[j]], [bqo[j]])
                    S.dma(dst[dr:dr + 128, t0:t0 + 512], qo[j], reads=[bqo[j]], q="sp")

                for ci in range(len(chunks)):
                    p1_main(ci)
                    if ci >= 1:
                        p1_post(ci - 1)
                p1_post(len(chunks) - 1)
                it += 1
                for s in range(4):
                    for (co, n, vo) in ((1536, 512, 0), (2048, 256, 512), (3328, 512, 768)):
                        pi = 1 + (it % 3); it += 1
                        for k in range(8):
                            b.mm(ps[pi][:, 0:n], h[:, k, s * 128:(s + 1) * 128], W[:, k, co:co + n], k == 0, k == 7,
                                 [bw, bh], [pb[pi]])
                        b.act(vsb[:, s, vo:vo + n], ps[pi][:, 0:n], AF.Copy, [pb[pi]], [bvs])
                S.dma(VA[t0:t0 + 512, :].rearrange("(s p) c -> p s c", p=128), vsb[:, :, 0:768], reads=[bvs], q="sp")
                S.dma(VB[t0:t0 + 512, :].rearrange("(s p) c -> p s c", p=128), vsb[:, :, 768:1280], reads=[bvs], q="sp")

        bconv = [Buf(f"conv{i}") for i in range(9)]

        def emit_conv(idx, g_src, u_src, d_src):
            for gi in range(14):
                S.dma(Wg_b[idx, gi].rearrange("p (k j) -> p k j", j=256),
                      g_src[:, gi * 256:(gi + 1) * 256].rearrange("(k p) j -> p k j", p=128),
                      writes=[bconv[idx]], q="pool", bg=True)
                S.dma(Wu_b[idx, gi].rearrange("p (k j) -> p k j", j=256),
                      u_src[:, gi * 256:(gi + 1) * 256].rearrange("(k p) j -> p k j", p=128),
                      writes=[bconv[idx]], q="pool", bg=True)
            for c in range(8):
                S.dma(Wd_b[idx, c].rearrange("p (f j) -> p f j", j=128),
                      d_src[:, c * 128:(c + 1) * 128].rearrange("(f p) j -> p f j", p=128),
                      writes=[bconv[idx]], q="pool", bg=True)

        def phase_p2b(l, qtiles):
            b.barrier()
            if l == 0:
                emit_conv(0, ffn_g, ffn_u, ffn_d)
                if n_layers > 1:
                    for e in range(NE):
                        emit_conv(1 + e, moe_g[e], moe_u[e], moe_d[e])
            nkb_max = (max(qtiles) + 1) * 4
            kts = [b.abf.alloc([T]) for _ in range(1)]; bkt = [Buf("kt") for _ in range(1)]
            vts = [b.abf.alloc([64, 128]) for _ in range(1)]; bvt = [Buf("vt") for _ in range(1)]
            qzs = [[b.abf.alloc([512]) for _ in range(2)] for _ in range(2)]; bqt = [Buf("qt") for _ in range(2)]
            for par in range(2):
                for c in range(2):
                    b.memset(qzs[par][c], 0.0, [bqt[par]])
            pts = [b.abf.alloc([512]) for _ in range(4)]; bpt = [Buf("pt") for _ in range(4)]
            rd = b.af.alloc([512]); o1 = b.af.alloc([512]); o2 = b.af.alloc([512]); lnb = b.af.alloc([512])
            sqb = b.abf.alloc([512]); ob = b.abf.alloc([512])
            be = Buf("epi"); bob = Buf("ob")
            accD = [b.af.alloc([512]) for _ in range(2)]; bacc = [Buf("accD") for _ in range(2)]
            dh = b.abf.alloc([512]); dl = b.abf.alloc([512]); bdh = Buf("dh")
            pending = [None]
            neglam = lam_sb[:, l, 4:5]
            gsub = vec_sb[:, l, 40:41]
            nq = 0
            for hh in range(4):
                kt = kts[0]; vt = vts[0]
                S.dma(kt[:, 0:nkb_max * 128], KBt[hh * 128:(hh + 1) * 128, 0:nkb_max * 128], writes=[bkt[0]], q="sp")
                for c8 in range(0, nkb_max, 8):
                    S.dma(vt[:, c8:c8 + 8, :],
                          VB[c8 * 128:(c8 + 8) * 128, hh * 128:(hh + 1) * 128].rearrange("(n p) c -> p n c", p=128),
                          writes=[bvt[0]], q="sp")
                for qi in qtiles:
                    q0 = qi * 512
                    own = q0 >= TO
                    qz = qzs[nq % 2]; bq = bqt[nq % 2]; nq += 1
                    S.dma(qz[0][0:64, :], QBt[hh * 128:hh * 128 + 64, q0:q0 + 512], writes=[bq], q="sp")
                    S.dma(qz[1][64:128, :], QBt[hh * 128 + 64:(hh + 1) * 128, q0:q0 + 512], writes=[bq], q="sp")
                    nkb = (qi + 1) * 4
                    if own:
                        kbs = list(range(0, nkb))
                    else:
                        kbs = list(range(0, nkb))
                    items = [(c, kb) for kb in kbs for c in (0, 1)]

                    def qk(i):
                        c, kb = items[i]
                        sb_i = i % 4
                        diag = kb >= qi * 4
                        rows = slice(64 * c, 64 * c + 64)
                        b.mm(ps[sb_i], kt[:, kb * 128:(kb + 1) * 128], qz[c], True, not diag,
                             [bkt[0], bq], [pb[sb_i]])
                        if diag:
                            b.mm(ps[sb_i], ident, maskB[:, kb - qi * 4, :], False, True, [bc], [pb[sb_i]])

                    LA = 3
                    n_it = len(items)
                    for _w in range(WARM_P2B if qi != qtiles[0] else WARM_B):
                        b.mm(ps[7], ones, maskB[:, 0, :], True, True, [bc], [pb[7]])
                    for i0 in range(min(LA, n_it)):
                        qk(i0)
                    for i, (c, kb) in enumerate(items):
                        if i + LA < n_it:
                            qk(i + LA)
                        sb_i = i % 4
                        pt = pts[i % 4]; bp = bpt[i % 4]
                        pref = own and kb < TO // 128
                        b.act(pt, ps[sb_i], AF.Exp, [pb[sb_i], bc], [bp], scale=0.125, bias=pbias if pref else None)
                        first = kb == kbs[0]; last = kb == kbs[-1]
                        b.mm(ps[4 + c], vt[:, kb, :], pt, first, last, [bvt[0], bp], [pb[4 + c]])
                        if first:
                            b.copy(accD[c], pt, [bp], [bacc[c]])
                        else:
                            b.tt(accD[c], accD[c], pt, ALU.add, [bp, bacc[c]], [bacc[c]])
                        if i == 6 and pending[0] is not None:
                            pending[0]()
                            pending[0] = None
                    if pending[0] is not None:
                        pending[0]()
                        pending[0] = None
                    for c in (0, 1):
                        b.copy(dh, accD[c], [bacc[c]], [bdh])
                        b.tt(dl, accD[c], dh, ALU.subtract, [bacc[c], bdh], [bdh])
                        b.mm(ps[6 + c], ones, dh, True, False, [bc, bdh], [pb[6 + c]])
                        b.mm(ps[6 + c], ones, dl, False, True, [bc, bdh], [pb[6 + c]])
                        b.act(rd, ps[6 + c], AF.Ln, [pb[6 + c]], [be])
                        b.act(rd, rd, AF.Exp, [be], [be], scale=-1.0)
                        b.tt(o1 if c == 0 else o2, ps[4 + c], rd, ALU.mult, [pb[4 + c], be], [be])
                    b.stt(o1, o2, neglam, o1, ALU.mult, ALU.add, [be, bc], [be])

                    def part2(hh=hh, q0=q0):
                        b.act(sqb, o1, AF.Square, [be], [be])
                        b.mm(ps[6], ones, sqb, True, True, [bc, be], [pb[6]])
                        b.act(lnb, ps[6], AF.Ln, [pb[6], bc], [be], scale=1.0 / 128.0, bias=epsc)
                        b.act(lnb, lnb, AF.Exp, [be], [be], scale=-0.5)
                        b.stt(ob, o1, gsub, lnb, ALU.mult, ALU.mult, [be, bc], [bob])
                        S.dma(OBt[hh * 128:(hh + 1) * 128, q0:q0 + 512], ob, reads=[bob], q="sp")
                    pending[0] = part2
            if pending[0] is not None:
                pending[0]()
                pending[0] = None

        def phase_p2a(l, segs):
            b.barrier()
            ka = b.abf.alloc([2, 4096]); bka = Buf("ka")
            qaz = [b.abf.alloc([2, 2048]) for _ in range(2)]; bqa = Buf("qa")
            for hp_ in range(2):
                b.memset(qaz[hp_], 0.0, [bqa])
            vts = [b.abf.alloc([2, 256]) for _ in range(3)]; bvts = [Buf("va") for _ in range(3)]
            pts = [b.abf.alloc([256]) for _ in range(3)]; bpts = [Buf("pa") for _ in range(3)]
            accN = b.af.alloc([2, 2048]); accD = b.af.alloc([2, 2048]); bacc = Buf("acc")
            oa = b.abf.alloc([2, 2048]); boa = Buf("oa")
            vbc = [0]; gic = [0]
            for sg in segs:
                s0 = sg * 2048
                first_seg = (sg == 0)
                own_first = (s0 == TO)
                for g, dil in enumerate((1, 4, 16)):
                    for p in range(2):
                        r0 = g * 256 + p * 128
                        if first_seg:
                            S.dma(ka[:, p, 2048:4096], KAt[r0:r0 + 128, s0:s0 + 2048], writes=[bka], q="sp")
                        else:
                            S.dma(ka[:, p, :], KAt[r0:r0 + 128, s0 - 2048:s0 + 2048], writes=[bka], q="sp")
                        S.dma(qaz[0][0:64, p, :], QAt[r0:r0 + 64, s0:s0 + 2048], writes=[bqa], q="sp")
                        S.dma(qaz[1][64:128, p, :], QAt[r0 + 64:r0 + 128, s0:s0 + 2048], writes=[bqa], q="sp")
                    nblk = 16 // dil
                    blocks = [(r, n) for r in range(dil) for n in range(nblk)]
                    nb = len(blocks)
                    vb0 = vbc[0]; vbc[0] += nb
                    g0 = gic[0]; gic[0] += nb * 4

                    def binfo(k):
                        r, n = blocks[k]
                        cs = n * 128 * dil + r
                        has_prev = not (first_seg and n == 0)
                        return r, n, cs, has_prev, cs - 128 * dil

                    def loadV(k):
                        r, n, cs, has_prev, ps_ = binfo(k)
                        vi = (vb0 + k) % 3
                        vt = vts[vi]; bv = bvts[vi]
                        tok = s0 + cs
                        S.dma(vt[:, 0, :], VA[tok:tok + 127 * dil + 1:dil, g * 256:(g + 1) * 256], writes=[bv], q="sp")
                        if has_prev:
                            tokp = s0 + ps_
                            S.dma(vt[:, 1, :], VA[tokp:tokp + 127 * dil + 1:dil, g * 256:(g + 1) * 256], writes=[bv], q="sp")

                    def stA(i):
                        k, hg = divmod(i, 4)
                        r, n, cs, has_prev, ps_ = binfo(k)
                        p = hg // 2; hp = hg % 2
                        rows = slice(64 * hp, 64 * hp + 64)
                        si = (g0 + i) % 3
                        pst = ps[si]; psb_ = pb[si]
                        qsl = slice(cs, cs + 127 * dil + 1, dil)
                        b.mm(pst[:, 0:128], ka[:, p, 2048 + cs:2048 + cs + 127 * dil + 1:dil], qaz[hp][:, p, qsl],
                             True, False, [bka, bqa], [psb_])
                        b.mm(pst[:, 0:128], ident, maskA[:, 0:128], False, True, [bc], [psb_])
                        if has_prev:
                            b.mm(pst[:, 128:256], ka[:, p, 2048 + ps_:2048 + ps_ + 127 * dil + 1:dil],
                                 qaz[hp][:, p, qsl], True, False, [bka, bqa], [psb_])
                            b.mm(pst[:, 128:256], ident, maskA[:, 128:256], False, True, [bc], [psb_])

                    def stBCD(i):
                        k, hg = divmod(i, 4)
                        r, n, cs, has_prev, ps_ = binfo(k)
                        p = hg // 2; hp = hg % 2
                        rows = slice(64 * hp, 64 * hp + 64)
                        si = (g0 + i) % 3
                        pst = ps[si]; psb_ = pb[si]
                        qsl = slice(cs, cs + 127 * dil + 1, dil)
                        vi = (vb0 + k) % 3
                        vt = vts[vi]; bv = bvts[vi]
                        pt = pts[si]; bp = bpts[si]
                        b.act(pt[:, 0:128], pst[:, 0:128], AF.Exp, [psb_], [bp], scale=0.125)
                        if has_prev:
                            pref = own_first and n == 0
                            b.act(pt[:, 128:256], pst[:, 128:256], AF.Exp, [psb_, bc], [bp], scale=0.125,
                                  bias=pbias if pref else None)
                        po = 3 + ((g0 + i) % 3)
                        pso = ps[po]; pbo = pb[po]
                        b.mm(pso[:, 0:128], vt[:, 0, p * 128:(p + 1) * 128], pt[:, 0:128], True, not has_prev,
                             [bv, bp], [pbo])
                        if has_prev:
                            b.mm(pso[:, 0:128], vt[:, 1, p * 128:(p + 1) * 128], pt[:, 128:256], False, True,
                                 [bv, bp], [pbo])
                        b.mm(pso[:, 128:256], ones, pt[:, 0:128], True, not has_prev, [bc, bp], [pbo])
                        if has_prev:
                            b.mm(pso[:, 128:256], ones, pt[:, 128:256], False, True, [bc, bp], [pbo])
                        if g == 0:
                            b.act(accN[rows, p, qsl], pso[rows, 0:128], AF.Copy, [pbo], [bacc])
                            b.copy(accD[rows, p, qsl], pso[rows, 128:256], [pbo], [bacc])
                        else:
                            b.tt(accN[rows, p, qsl], accN[rows, p, qsl], pso[rows, 0:128], ALU.add,
                                 [pbo, bacc], [bacc])
                            b.tt(accD[rows, p, qsl], accD[rows, p, qsl], pso[rows, 128:256], ALU.add,
                                 [pbo, bacc], [bacc], eng="dve")

                    n_it = nb * 4
                    for _w in range(WARM_B):
                        b.mm(ps[6], ones, maskB[:, 0, :], True, True, [bc], [pb[6]])
                    loadV(0)
                    if nb > 1:
                        loadV(1)
                    stA(0); stA(1)
                    for i in range(n_it):
                        k, hg = divmod(i, 4)
                        if hg == 0 and k + 2 < nb:
                            loadV(k + 2)
                        if i + 2 < n_it:
                            stA(i + 2)
                        stBCD(i)
                for p in range(2):
                    b.recip(accD[:, p, :], accD[:, p, :], [bacc], [bacc])
                    b.tt(oa[:, p, :], accN[:, p, :], accD[:, p, :], ALU.mult, [bacc], [boa])
                    S.dma(OAt[p * 128:(p + 1) * 128, s0:s0 + 2048], oa[:, p, :], reads=[boa], q="sp")

        def phase_p2c(l, halves):
            nonlocal bw
            b.barrier()
            bw = Buf("w")
            PW = b.abf.alloc([4, 128])
            for g in range(4):
                S.dma(PW[:, g, :], pool_w[l, g], writes=[bw], q="pool")
            cb = b.af.alloc([16 + 2048]); s2 = b.af.alloc([16 + 2048]); s4 = b.af.alloc([16 + 2048])
            icn = b.af.alloc([2048])
            bcb = Buf("cb"); bs = Buf("s"); bic = Buf("icn")
            cbf = b.abf.alloc([16 + 2048]); bcbf = Buf("cbf")
            dd = b.abf.alloc([2048]); bdd = Buf("dd")
            oc = [b.abf.alloc([512]) for _ in range(2)]; boc = [Buf("oc") for _ in range(2)]
            it = 0
            for hf in halves:
                for q4 in range(2):
                    t0 = hf * TO + q4 * 2048
                    for g, w in enumerate((2, 4, 8, 16)):
                        S.dma(cbf[:, 16:], Ct[g * 128:(g + 1) * 128, t0:t0 + 2048], writes=[bcbf], q="sp")
                        if t0 == 0:
                            b.memset(cbf[:, 0:16], 0.0, [bcbf])
                        else:
                            S.dma(cbf[:, 0:16], Ct[g * 128:(g + 1) * 128, t0 - 16:t0], writes=[bcbf], q="sp")
                        S.dma(icn, invcnt[g:g + 1, t0:t0 + 2048].broadcast_to([128, 2048]), writes=[bic], q="sp")
                        b.copy(cb, cbf, [bcbf], [bcb])
                        if t0 == TO:
                            b.ts(cb[:, 0:16], cb[:, 0:16], pflag, None, ALU.mult, None, [bcb, bc], [bcb])
                        N = 16 + 2048
                        b.tt(s2[:, 1:N], cb[:, 1:N], cb[:, 0:N - 1], ALU.add, [bcb], [bs])
                        cur = s2
                        if w >= 4:
                            b.tt(s4[:, 3:N], s2[:, 3:N], s2[:, 1:N - 2], ALU.add, [bs], [bs])
                            cur = s4
                        if w >= 8:
                            b.tt(s2[:, 7:N], s4[:, 7:N], s4[:, 3:N - 4], ALU.add, [bs], [bs])
                            cur = s2
                        if w >= 16:
                            b.tt(s4[:, 15:N], s2[:, 15:N], s2[:, 7:N - 8], ALU.add, [bs], [bs])
                            cur = s4
                        b.tt(cur[:, 16:N], cur[:, 16:N], icn, ALU.mult, [bs, bic], [bs])
                        b.tt(dd, cur[:, 16:N], cb[:, 16:N], ALU.subtract, [bs, bcb], [bdd])
                        for s in range(4):
                            pi = it % 4; it += 1
                            b.mm(ps[pi], PW[:, g, :], dd[:, s * 512:(s + 1) * 512], True, True, [bw, bdd], [pb[pi]])
                            j = it % 2
                            b.act(oc[j], ps[pi], AF.Copy, [pb[pi], bc], [boc[j]], scale=vec_sb[:, l, 41 + g:42 + g])
                            S.dma(OCt[g * 128:(g + 1) * 128, t0 + s * 512:t0 + (s + 1) * 512], oc[j], reads=[boc[j]], q="sp")

        def phase_p3(l, tiles):
            nonlocal bw
            b.barrier()
            bw = Buf("w")
            WG = b.abf.alloc([8, 3072])
            load_w(WG, w_in[l][:, 4352:7424], 8)
            WP = b.abf.alloc([10, 1024])
            load_w(WP[:, 0:2, :], w_pa[l], 2); load_w(WP[:, 2:6, :], w_pb[l], 4); load_w(WP[:, 6:10, :], w_pc[l], 4)
            WO = b.abf.alloc([8, 1024])
            load_w(WO, w_out[l], 8)
            h = b.abf.alloc([8, 512]); bh = Buf("h")
            om = b.abf.alloc([10, 512]); bom = Buf("om")
            mixed = b.abf.alloc([8, 512]); bmx = Buf("mixed")
            xt = b.af.alloc([8, 512]); bxt = Buf("xt")
            sg = [b.af.alloc([512]) for _ in range(2)]; bsg = [Buf("sg") for _ in range(2)]
            macc = b.af.alloc([512]); bma = Buf("macc")
            it = 0
            for ti in tiles:
                t0 = ti * 512
                S.dma(h, hT.rearrange("(k p) t -> p k t", p=128)[:, :, t0:t0 + 512], writes=[bh], q="sp")
                S.dma(om[:, 0:2, :], OAt.rearrange("(k p) t -> p k t", p=128)[:, :, t0:t0 + 512], writes=[bom], q="sp")
                S.dma(om[:, 2:6, :], OBt.rearrange("(k p) t -> p k t", p=128)[:, :, t0:t0 + 512], writes=[bom], q="sp")
                S.dma(om[:, 6:10, :], OCt.rearrange("(k p) t -> p k t", p=128)[:, :, t0:t0 + 512], writes=[bom], q="sp")
                S.dma(xt, xs.rearrange("(k p) t -> p k t", p=128)[:, :, t0:t0 + 512], writes=[bxt], q="sp")
                for c in range(8):
                    for br, (k0, k1) in enumerate(((0, 2), (2, 6), (6, 10))):
                        pg = (it % 3) * 2; it += 1
                        for k in range(8):
                            b.mm(ps[pg], WG[:, k, br * 1024 + c * 128: br * 1024 + (c + 1) * 128], h[:, k, :],
                                 k == 0, k == 7, [bw, bh], [pb[pg]])
                        for k in range(k0, k1):
                            b.mm(ps[pg + 1], WP[:, k, c * 128:(c + 1) * 128], om[:, k, :], k == k0, k == k1 - 1,
                                 [bw, bom], [pb[pg + 1]])
                        j = it % 2
                        b.act(sg[j], ps[pg], AF.Sigmoid, [pb[pg], bc], [bsg[j]], bias=vec_sb[:, l, 16 + br * 8 + c:17 + br * 8 + c])
                        if br == 0:
                            b.tt(macc, sg[j], ps[pg + 1], ALU.mult, [bsg[j], pb[pg + 1]], [bma])
                        else:
                            b.tt(sg[j], sg[j], ps[pg + 1], ALU.mult, [bsg[j], pb[pg + 1]], [bsg[j]])
                            if br == 1:
                                b.tt(macc, macc, sg[j], ALU.add, [bma, bsg[j]], [bma], eng="pool")
                            else:
                                b.tt(mixed[:, c, :], macc, sg[j], ALU.add, [bma, bsg[j]], [bmx], eng="pool")
                for c in range(8):
                    po = 6 + (c % 2)
                    for k in range(8):
                        b.mm(ps[po], WO[:, k, c * 128:(c + 1) * 128], mixed[:, k, :], k == 0, k == 7, [bw, bmx], [pb[po]])
                    b.tt(xt[:, c, :], xt[:, c, :], ps[po], ALU.add, [bxt, pb[po]], [bxt])
                S.dma(xs.rearrange("(k p) t -> p k t", p=128)[:, :, t0:t0 + 512], xt, reads=[bxt], q="sp")

        def phase_p4(l, tiles2, final):
            b.barrier()
            moe = (l % 2 == 1)
            wg = [b.abf.alloc([8, 256]) for _ in range(2)]
            wu = [b.abf.alloc([8, 256]) for _ in range(2)]
            bwb = [Buf("wbuf") for _ in range(2)]
            wdh = [b.abf.alloc([NF, 128]) for _ in range(2)]
            bwd = [Buf("wd") for _ in range(2)]
            actT = b.abf.alloc([NF, 1024]); bact = Buf("act")
            h = b.abf.alloc([8, 1024]); bh = Buf("h")
            lnb = b.af.alloc([1024]); rstd = b.af.alloc([1024]); bscr = Buf("scr")
            xt = b.af.alloc([8, 1024]); bxt = Buf("xt")
            sl = [b.abf.alloc([512]) for _ in range(2)]; bsl = [Buf("sl") for _ in range(2)]
            g2 = vec_sb[:, l, 8:16]
            if moe:
                hf = b.af.alloc([8, 512]); bhf = Buf("hf")
                sq = actT[:, 0:8, :]
            else:
                hbufs = [h, b.abf.alloc([8, 1024])]; bhb = [bh, Buf("h2")]
                sqd = b.af.alloc([2048]).bitcast(BF16).rearrange("p (k t) -> p k t", t=512); bsqd = Buf("sqd")
                xc = [b.af.alloc([1024]) for _ in range(2)]; bxc = [Buf("xc") for _ in range(2)]

            def prep_dense(ti, hb):
                t0_ = ti * 1024
                S.dma(xt, xs.rearrange("(k p) t -> p k t", p=128)[:, :, t0_:t0_ + 1024], writes=[bxt], q="sp")
                for hv in range(2):
                    hs = slice(hv * 512, (hv + 1) * 512)
                    for k in range(8):
                        b.act(sqd[:, k, :], xt[:, k, hs], AF.Square, [bxt], [bsqd])
                    for k in range(8):
                        b.mm(ps[0], ones, sqd[:, k, :], k == 0, k == 7, [bsqd, bc], [pb[0]])
                    b.act(lnb[:, hs], ps[0], AF.Ln, [pb[0], bc], [bscr], scale=1.0 / 1024.0, bias=epsc)
                b.act(rstd, lnb, AF.Exp, [bscr], [bscr], scale=-0.5)
                for k in range(8):
                    b.stt(hbufs[hb][:, k, :], xt[:, k, :], g2[:, k:k + 1], rstd, ALU.mult, ALU.mult,
                          [bxt, bscr, bc], [bhb[hb]])
            if moe:
                rw = sbt("rw", [128, 8, NE], F32)
                brw = Buf("rw")
                S.dma(rw[:, :, :], router[:, :, :], writes=[brw], q="sp")
                lg = b.af.alloc([4, 8]); mx = b.af.alloc([4, 8]); ex = b.af.alloc([4, 8]); msk = b.af.alloc([4, 8])
                den = b.af.alloc([4]); gT = b.abf.alloc([512]); gbc = b.abf.alloc([NE, 1024])
                gate_bf = b.abf.alloc([4, 8])
                sel = b.abf.alloc([NE, 128])
                bg = Buf("gate"); bgb = Buf("gbc"); bsel = Buf("sel")
                ytmp = [b.af.alloc([512]) for _ in range(2)]; byt = [Buf("yt") for _ in range(2)]
                for e in range(NE):
                    b.copy(sel[0:8, e, :], ident[0:8, e:e + 1].broadcast_to([8, 128]), [bc], [bsel])
            nexp = NE if moe else 1
            wcount = 0
            dcount = 0
            it = 0
            if not moe:
                prep_dense(tiles2[0], 0)
            for tix, ti in enumerate(tiles2):
                t0 = ti * 1024
                if not moe:
                    h = hbufs[tix % 2]; bh = bhb[tix % 2]
                else:
                    S.dma(xt, xs.rearrange("(k p) t -> p k t", p=128)[:, :, t0:t0 + 1024], writes=[bxt], q="sp")
                    for k in range(8):
                        b.act(sq[:, k, :], xt[:, k, :], AF.Square, [bxt], [bact])
                    for hv in range(2):
                        hs = slice(hv * 512, (hv + 1) * 512)
                        for k in range(8):
                            b.mm(ps[0], ones, sq[:, k, hs], k == 0, k == 7, [bact, bc], [pb[0]])
                        b.act(lnb[:, hs], ps[0], AF.Ln, [pb[0], bc], [bscr], scale=1.0 / 1024.0, bias=epsc)
                    b.act(rstd, lnb, AF.Exp, [bscr], [bscr], scale=-0.5)
                    for k in range(8):
                        b.stt(h[:, k, :], xt[:, k, :], g2[:, k:k + 1], rstd, ALU.mult, ALU.mult, [bxt, bscr, bc], [bh])
                if moe:
                    for hv in range(2):
                        hs = slice(hv * 512, (hv + 1) * 512)
                        for k in range(8):
                            b.stt(hf[:, k, :], xt[:, k, hs], g2[:, k:k + 1], rstd[:, hs], ALU.mult, ALU.mult,
                                  [bxt, bscr, bc], [bhf])
                        for s in range(4):
                            for k in range(8):
                                b.mm(ps[1][:, s * 8:(s + 1) * 8], hf[:, k, s * 128:(s + 1) * 128], rw[:, k, :], k == 0, k == 7,
                                     [bhf, brw], [pb[1]])
                        b.copy(lg, ps[1][:, 0:32].rearrange("p (s e) -> p s e", e=8), [pb[1]], [bg])
                        for s in range(4):
                            S.emit("dve", lambda e_, o=mx[:, s, :], i=lg[:, s, :]: e_.max(out=o, in_=i), [bg], [bg])
                            b.ts(den[:, s:s + 1], mx[:, s, 0:1], -1.0, None, ALU.mult, None, [bg], [bg])
                            b.act(ex[:, s, :], lg[:, s, :], AF.Exp, [bg], [bg], bias=den[:, s:s + 1])
                            b.ts(msk[:, s, :], lg[:, s, :], mx[:, s, 1:2], None, ALU.is_ge, None, [bg], [bg])
                            b.tt(ex[:, s, :], ex[:, s, :], msk[:, s, :], ALU.mult, [bg], [bg])
                            S.emit("dve", lambda e_, o=den[:, s:s + 1], i=ex[:, s, :]: e_.reduce_sum(
                                out=o, in_=i, axis=mybir.AxisListType.X), [bg], [bg])
                            b.recip(den[:, s:s + 1], den[:, s:s + 1], [bg], [bg])
                            b.ts(gate_bf[:, s, :], ex[:, s, :], den[:, s:s + 1], None, ALU.mult, None, [bg], [bg])
                        for s in range(4):
                            b.mm(ps[2][0:8, s * 128:(s + 1) * 128], gate_bf[:, s, :], ident, True, True, [bg, bc], [pb[2]])
                        b.copy(gT[0:8, :], ps[2][0:8, :], [pb[2]], [bg])
                        for e in range(NE):
                            b.mm(ps[3], sel[0:8, e, :], gT[0:8, :], True, True, [bsel, bg], [pb[3]])
                            b.act(gbc[:, e, hs], ps[3], AF.Copy, [pb[3]], [bgb])
                for e in range(nexp):
                    widx = 1 + e if moe else 0
                    for gi in range(14):
                        wb = wcount % 2; wcount += 1
                        S.dma(wg[wb], Wg_b[widx, gi].rearrange("p (k j) -> p k j", j=256), reads=[bconv[widx]],
                              writes=[bwb[wb]], q="sp")
                        S.dma(wu[wb], Wu_b[widx, gi].rearrange("p (k j) -> p k j", j=256), reads=[bconv[widx]],
                              writes=[bwb[wb]], q="sp")
                        for fi in range(2):
                            f = gi * 2 + fi
                            for k in range(8):
                                for hv in range(2):
                                    b.mm(ps[4 + hv], wg[wb][:, k, fi * 128:(fi + 1) * 128], h[:, k, hv * 512:(hv + 1) * 512],
                                         k == 0, k == 7, [bwb[wb], bh], [pb[4 + hv]])
                            for k in range(8):
                                for hv in range(2):
                                    b.mm(ps[6 + hv], wu[wb][:, k, fi * 128:(fi + 1) * 128], h[:, k, hv * 512:(hv + 1) * 512],
                                         k == 0, k == 7, [bwb[wb], bh], [pb[6 + hv]])
                            for hv in range(2):
                                j = it % 2; it += 1
                                b.act(sl[j], ps[4 + hv], AF.Silu, [pb[4 + hv]], [bsl[j]])
                                b.tt(actT[:, f, hv * 512:(hv + 1) * 512], sl[j], ps[6 + hv], ALU.mult,
                                     [bsl[j], pb[6 + hv]], [bact])
                    if (not moe) and tix + 1 < len(tiles2):
                        prep_dense(tiles2[tix + 1], (tix + 1) % 2)
                    dbase = dcount; dcount += 8

                    def load_wd(c_):
                        db_ = (dbase + c_) % 2
                        S.dma(wdh[db_], Wd_b[widx, c_].rearrange("p (f j) -> p f j", j=128), reads=[bconv[widx]],
                              writes=[bwd[db_]], q="sp")
                    load_wd(0)
                    for c in range(8):
                        db = (dbase + c) % 2
                        pp = (1, 2) if c % 2 == 0 else (3, 5)
                        if not moe:
                            xj = c % 2
                            S.dma(xc[xj], xs[c * 128:(c + 1) * 128, t0:t0 + 1024], writes=[bxc[xj]], q="sp")
                        if c + 1 < 8:
                            load_wd(c + 1)
                        for f in range(NF):
                            for hv in range(2):
                                b.mm(ps[pp[hv]], wdh[db][:, f, :], actT[:, f, hv * 512:(hv + 1) * 512], f == 0, f == NF - 1,
                                     [bwd[db], bact], [pb[pp[hv]]])
                        for hv in range(2):
                            hs = slice(hv * 512, (hv + 1) * 512)
                            po = pp[hv]
                            if moe:
                                j = hv
                                b.tt(ytmp[j], ps[po], gbc[:, e, hs], ALU.mult, [pb[po], bgb], [byt[j]])
                                b.tt(xt[:, c, hs], xt[:, c, hs], ytmp[j], ALU.add, [bxt, byt[j]], [bxt], eng="pool")
                            else:
                                b.tt(xc[xj][:, hs], xc[xj][:, hs], ps[po], ALU.add, [bxc[xj], pb[po]], [bxc[xj]])
                        if not moe:
                            S.dma(xs[c * 128:(c + 1) * 128, t0:t0 + 1024], xc[xj], reads=[bxc[xj]], q="sp")
                if final:
                    for k in range(8):
                        b.act(sq[:, k, :], xt[:, k, :], AF.Square, [bxt], [bact])
                    for hv in range(2):
                        hs = slice(hv * 512, (hv + 1) * 512)
                        for k in range(8):
                            b.mm(ps[0], ones, sq[:, k, hs], k == 0, k == 7, [bact, bc], [pb[0]])
                        b.act(lnb[:, hs], ps[0], AF.Ln, [pb[0], bc], [bscr], scale=1.0 / 1024.0, bias=epsc)
                    b.act(rstd, lnb, AF.Exp, [bscr], [bscr], scale=-0.5)
                    for hv in range(2):
                        hs = slice(hv * 512, (hv + 1) * 512)
                        for k in range(8):
                            b.stt(hf[:, k, :], xt[:, k, hs], fing_sb[:, k:k + 1], rstd[:, hs], ALU.mult, ALU.mult,
                                  [bxt, bscr, bc], [bhf])
                        S.dma(outT.rearrange("(k p) t -> p k t", p=128)[:, :, t0 - TO + hv * 512:t0 - TO + (hv + 1) * 512],
                              hf, reads=[bhf], q="sp")
                elif moe:
                    S.dma(xs.rearrange("(k p) t -> p k t", p=128)[:, :, t0:t0 + 1024], xt, reads=[bxt], q="sp")

        all_tiles = list(range(16)); own_tiles = list(range(8, 16))
        for l in range(n_layers):
            last = (l == n_layers - 1)
            if not last:
                phase_p1(l, all_tiles, all_tiles)
                phase_p2b(l, all_tiles)
                phase_p2a(l, [0, 1, 2, 3])
                phase_p2c(l, [0, 1])
                phase_p3(l, all_tiles)
                phase_p4(l, list(range(8)), False)
            else:
                phase_p1(l, own_tiles, all_tiles)
                phase_p2b(l, own_tiles)
                phase_p2a(l, [2, 3])
                phase_p2c(l, [1])
                phase_p3(l, own_tiles)
                phase_p4(l, list(range(4, 8)), True)
        if debug:
            b.barrier()
            for k in range(8):
                S.dma(dbg[k * 128:(k + 1) * 128, :], xs[k * 128:(k + 1) * 128, :], q="sp")
        S.run(nc)
    return nc


def _consts():
    ident = np.eye(128, dtype=np.float32)
    ones = np.ones((128, 128), np.float32)
    R = np.zeros((128, 128), np.float32)
    for hb in (0, 64):
        for d in range(16):
            src = d + 8 if d < 8 else d - 8
            R[hb + src, hb + d] = 1.0
    k = np.arange(128)[:, None]
    q = np.arange(128)[None, :]
    cur = np.where(q >= k, 0.0, NEGM).astype(np.float32)
    prev = np.where(k >= q, 0.0, NEGM).astype(np.float32)
    maskA = np.concatenate([cur, prev], axis=1)
    q5 = np.arange(512)[None, :]
    maskB = np.concatenate([np.where(128 * i + k <= q5, 0.0, NEGM).astype(np.float32) for i in range(4)], axis=1)
    inv_freq = (500000.0 ** (-np.arange(0, 16, 2, dtype=np.float32) / 16.0)).astype(np.float32)
    freq = np.zeros((128, 1), np.float32)
    sgn = np.zeros((128, 1), np.float32)
    for p in range(128):
        d = p % 64
        if d < 16:
            freq[p, 0] = inv_freq[d % 8] / (2.0 * np.pi)
            sgn[p, 0] = -1.0 if d < 8 else 1.0
    return np.concatenate([ident, ones, R, maskA, maskB, freq, sgn], axis=1).astype(np.float32)


def _feat(v):
    v = np.asarray(v, np.float32)
    return np.ascontiguousarray(v.reshape(-1, 128).T)


_NC_CACHE = {}


def make_in_maps(x, positions, norm1_g, w_in, b_gate, diff_lambda, diff_subln_g, pool_w, pool_scale,
                 w_proj_a, w_proj_b, w_proj_c, w_out, norm2_g, ffn_w_gate, ffn_w_up, ffn_w_down,
                 moe_router, moe_w_gate, moe_w_up, moe_w_down, final_norm_g):
    f32 = lambda a: np.ascontiguousarray(np.asarray(a, dtype=np.float32))
    x = f32(x); positions = np.asarray(positions).astype(np.int32)
    consts = _consts()
    vecs = np.zeros((2, 128, 64), np.float32)
    for l in range(2):
        vecs[l, :, 0:8] = _feat(norm1_g[l])
        vecs[l, :, 8:16] = _feat(norm2_g[l])
        for i in range(3):
            vecs[l, :, 16 + i * 8:24 + i * 8] = _feat(b_gate[l][i])
        vecs[l, :, 40] = np.asarray(diff_subln_g[l], np.float32)
        vecs[l, :, 41:45] = _feat(pool_scale[l])
    lam_in = f32(diff_lambda).reshape(2, 1, 256)
    shared = {
        "consts": consts, "vecs": vecs, "lam_in": lam_in, "fin_g": _feat(final_norm_g),
        "w_in": f32(w_in), "pool_w": f32(pool_w), "w_pa": f32(w_proj_a), "w_pb": f32(w_proj_b),
        "w_pc": f32(w_proj_c), "w_out": f32(w_out), "ffn_g": f32(ffn_w_gate)[0], "ffn_u": f32(ffn_w_up)[0],
        "ffn_d": f32(ffn_w_down)[0],
        "router": np.ascontiguousarray(f32(moe_router)[0].reshape(8, 128, NE).transpose(1, 0, 2)),
        "moe_g": f32(moe_w_gate)[0], "moe_u": f32(moe_w_up)[0], "moe_d": f32(moe_w_down)[0],
    }
    in_maps = []
    tpos = np.arange(8192)
    for c in range(8):
        bi, j = c // 2, c % 2
        order = np.concatenate([np.arange(4096), np.arange(4096, 8192)]) if j == 1 else \
            np.concatenate([np.arange(4096, 8192), np.arange(4096)])
        m = dict(shared)
        m["xT"] = np.ascontiguousarray(x[bi][order].T)
        m["pos"] = np.ascontiguousarray(positions[bi][order][None, :])
        tp = tpos[order]
        m["invcnt"] = np.stack([1.0 / np.minimum(tp + 1, w) for w in (2, 4, 8, 16)]).astype(np.float32)
        pc = np.zeros((128, 2), np.float32)
        pc[:, 0] = 0.0 if j == 1 else NEGM
        pc[:, 1] = 1.0 if j == 1 else 0.0
        m["percore"] = pc
        in_maps.append(m)
    return in_maps


def kernel(**inputs):
    if "nc" not in _NC_CACHE:
        _NC_CACHE["nc"] = build_program()
    nc = _NC_CACHE["nc"]
    in_maps = make_in_maps(**inputs)
    res = run_bass_kernel_spmd(nc, in_maps, core_ids=list(range(8)))
    out = np.zeros((4, 8192, 1024), np.float32)
    for c in range(8):
        bi, j = c // 2, c % 2
        out[bi, j * 4096:(j + 1) * 4096, :] = res.results[c]["outT"].T
    return out
```

```python
import math
from contextlib import ExitStack

import numpy as np
import concourse.bass as bass
import concourse.mybir as mybir
from concourse.bass_utils import run_bass_kernel_spmd

F32 = mybir.dt.float32
BF16 = mybir.dt.bfloat16
I32 = mybir.dt.int32
AF = mybir.ActivationFunctionType
ALU = mybir.AluOpType

SEM_BLOCK = 16384
N_ENG_SEMS = 12
N_DMA_SLOTS = 24
SAME_ENGINE_SYNC = True

D = 1024
T = 8192
TO = 4096
NEGM = -30000.0
EPS = 1e-6
DFF = 3584
NF = DFF // 128
NE = 8
TWO_PI = 6.28318
WARM_B = 12
WARM_P2B = 0


class Buf:
    __slots__ = ("name", "w", "r", "rd")

    def __init__(self, name=""):
        self.name = name
        self.w = None
        self.r = {}
        self.rd = []


class Op:
    __slots__ = ("idx", "eng", "fn", "deps", "needs_inc", "dma", "cnt", "slot", "sval")

    def __init__(self, idx, eng, fn, dma):
        self.idx = idx
        self.eng = eng
        self.fn = fn
        self.deps = []
        self.needs_inc = False
        self.dma = dma
        self.cnt = None
        self.slot = None
        self.sval = None


class Sched:
    ENGS = ("pe", "act", "dve", "pool", "sp")

    def __init__(self):
        self.ops = []
        self.last = {e: None for e in self.ENGS}
        self.dmas = {e: [] for e in self.ENGS}

    def emit(self, eng, fn, reads=(), writes=(), dma=False, extra=(), bg=False):
        op = Op(len(self.ops), eng, fn, dma)
        deps = {}
        for b in reads:
            if b.w is not None:
                deps[b.w.idx] = b.w
        for b in writes:
            if b.w is not None:
                deps[b.w.idx] = b.w
            for o in b.r.values():
                deps[o.idx] = o
            for o in b.rd:
                deps[o.idx] = o
        for o in extra:
            if o is not None:
                deps[o.idx] = o
        for d in deps.values():
            if (not d.dma) and d.eng == eng and (eng == "pe" or not SAME_ENGINE_SYNC):
                continue
            op.deps.append(d)
            d.needs_inc = True
        for b in reads:
            if dma:
                b.rd.append(op)
            else:
                b.r[eng] = op
        for b in writes:
            b.w = op
            b.r = {}
            b.rd = []
        self.ops.append(op)
        if dma:
            if not bg:
                self.dmas[eng].append(op)
        else:
            self.last[eng] = op
        return op

    def dma(self, out, in_, reads=(), writes=(), q="sp", bg=False):
        return self.emit(q, lambda e, o=out, i=in_: e.dma_start(out=o, in_=i), reads, writes, dma=True, bg=bg)

    def finalize(self):
        cnt = {e: 0 for e in self.ENGS}
        ndma = {e: 0 for e in self.ENGS}
        self.dma_ops = {e: [] for e in self.ENGS}
        for op in self.ops:
            if op.dma:
                i = ndma[op.eng]
                ndma[op.eng] += 1
                op.slot = i % N_DMA_SLOTS
                op.sval = 16 * (i // N_DMA_SLOTS + 1)
                if i >= N_DMA_SLOTS:
                    op.deps.append(self.dma_ops[op.eng][i - N_DMA_SLOTS])
                self.dma_ops[op.eng].append(op)
            elif op.needs_inc:
                cnt[op.eng] += 1
                op.cnt = cnt[op.eng]
        self.cnt = cnt
        self.ndma = ndma

    def run(self, nc):
        self.finalize()
        with ExitStack() as st:
            esems = {}
            for e in self.ENGS:
                nblk = (self.cnt[e] + SEM_BLOCK - 1) // SEM_BLOCK
                assert nblk <= N_ENG_SEMS, (e, self.cnt[e])
                esems[e] = [st.enter_context(nc.semaphore(f"s_{e}{k}")) for k in range(max(nblk, 1))]
            dsems = {}
            for e in self.ENGS:
                if self.ndma[e]:
                    dsems[e] = [st.enter_context(nc.semaphore(f"d_{e}{k}")) for k in range(N_DMA_SLOTS)]
            block = st.enter_context(nc.Block())

            def target(d):
                if d.dma:
                    return ("d", d.eng, d.slot), dsems[d.eng][d.slot], d.sval, d.sval
                blk = (d.cnt - 1) // SEM_BLOCK
                return ("c", d.eng), esems[d.eng][blk], (d.cnt - 1) % SEM_BLOCK + 1, d.cnt

            by_eng = {e: [o for o in self.ops if o.eng == e] for e in self.ENGS}

            def body(ename):
                def f(eng):
                    known = {}
                    for op in by_eng[ename]:
                        for d in op.deps:
                            key, sem, val, gval = target(d)
                            if known.get(key, 0) >= gval:
                                continue
                            eng.wait_ge(sem, val)
                            known[key] = gval
                        inst = op.fn(eng)
                        if op.dma:
                            inst.then_inc(dsems[ename][op.slot], 16)
                        elif op.needs_inc:
                            blk = (op.cnt - 1) // SEM_BLOCK
                            inst.then_inc(esems[ename][blk], 1)
                    if ename == "sp":
                        for q in self.ENGS:
                            for o in self.dma_ops[q][-N_DMA_SLOTS:]:
                                key, sem, val, gval = target(o)
                                if known.get(key, 0) >= gval:
                                    continue
                                eng.wait_ge(sem, val)
                                known[key] = gval
                return f

            block.tensor(body("pe"))
            block.scalar(body("act"))
            block.vector(body("dve"))
            block.gpsimd(body("pool"))
            block.sync(body("sp"))


class Arena:
    def __init__(self, ap, n):
        self.ap = ap
        self.n = n
        self.off = 0

    def reset(self):
        self.off = 0

    def alloc(self, shape):
        n = 1
        for s in shape:
            n *= s
        assert self.off + n <= self.n, ("arena overflow", self.off, n, self.n)
        a = self.ap[:, self.off:self.off + n]
        self.off += n
        if len(shape) == 2:
            a = a.rearrange("p (a b) -> p a b", b=shape[1])
        elif len(shape) == 3:
            a = a.rearrange("p (a b c) -> p a b c", b=shape[1], c=shape[2])
        return a


class B:
    def __init__(self, nc, st):
        self.nc = nc
        self.S = Sched()
        self.st = st
        self.nbuf = 0

    def buf(self, name="b"):
        self.nbuf += 1
        return Buf(f"{name}{self.nbuf}")

    def mm(self, out, lhsT, rhs, start, stop, reads, writes):
        return self.S.emit("pe", lambda e, o=out, l=lhsT, r=rhs, a=start, b=stop:
                           e.matmul(o, l, r, start=a, stop=b), reads, writes)

    def act(self, out, in_, func, reads, writes, scale=1.0, bias=None, eng="act"):
        if bias is None:
            fn = lambda e, o=out, i=in_, f=func, s=scale: e.activation(out=o, in_=i, func=f, scale=s)
        else:
            fn = lambda e, o=out, i=in_, f=func, s=scale, b=bias: e.activation(out=o, in_=i, func=f, scale=s, bias=b)
        return self.S.emit(eng, fn, reads, writes)

    def tt(self, out, in0, in1, op, reads, writes, eng="dve"):
        return self.S.emit(eng, lambda e, o=out, a=in0, b=in1, p=op: e.tensor_tensor(out=o, in0=a, in1=b, op=p),
                           reads, writes)

    def ts(self, out, in0, s1, s2, op0, op1, reads, writes, eng="dve"):
        if s2 is None:
            fn = lambda e, o=out, a=in0, x=s1, p=op0: e.tensor_scalar(out=o, in0=a, scalar1=x, scalar2=None, op0=p)
        else:
            fn = lambda e, o=out, a=in0, x=s1, y=s2, p=op0, q=op1: e.tensor_scalar(
                out=o, in0=a, scalar1=x, scalar2=y, op0=p, op1=q)
        return self.S.emit(eng, fn, reads, writes)

    def stt(self, out, in0, scalar, in1, op0, op1, reads, writes, eng="dve"):
        return self.S.emit(eng, lambda e, o=out, a=in0, s=scalar, b=in1, p=op0, q=op1:
                           e.scalar_tensor_tensor(out=o, in0=a, scalar=s, in1=b, op0=p, op1=q), reads, writes)

    def copy(self, out, in_, reads, writes, eng="dve"):
        return self.S.emit(eng, lambda e, o=out, i=in_: e.tensor_copy(out=o, in_=i), reads, writes)

    def recip(self, out, in_, reads, writes):
        return self.S.emit("dve", lambda e, o=out, i=in_: e.reciprocal(out=o, in_=i), reads, writes)

    def memset(self, ap, val, writes, eng="dve"):
        return self.S.emit(eng, lambda e, a=ap, v=val: e.memset(a, v), (), writes)

    def barrier(self):
        S = self.S
        deps = [S.last[e] for e in S.ENGS]
        for q in S.ENGS:
            deps += S.dmas[q][-N_DMA_SLOTS:]
        z = self.bar_t
        S.emit("act", lambda e: e.memset(z[:, 0:1], 0.0) if hasattr(e, "memset") else e.activation(
            out=z[:, 0:1], in_=z[:, 0:1], func=AF.Copy), (), (), extra=deps)
        S.emit("dve", lambda e: e.memset(z[:, 1:2], 0.0), (), (), extra=deps)
        S.emit("pool", lambda e: e.memset(z[:, 2:3], 0.0), (), (), extra=deps)
        S.emit("pe", lambda e: e.matmul(self.bar_ps[0:1, 0:1], self.ones[0:1, 0:1], self.ones[0:1, 0:1],
                                        start=True, stop=True), (), (), extra=deps)
        S.emit("sp", lambda e: e.dma_start(out=self.bar_d[0:1, 0:8], in_=self.bar_d[1:2, 0:8]), (), (),
               dma=True, extra=deps)
        self.abf.reset()
        self.af.reset()


def build_program(debug=False, n_layers=2, dense_moe=True):
    nc = bass.Bass("TRN2", target_bir_lowering=False)
    dt_in = lambda n, s, d=F32: nc.dram_tensor(n, list(s), d, kind="ExternalInput").ap()
    dt_sc = lambda n, s, d=BF16: nc.dram_tensor(n, list(s), d, kind="Internal").ap()

    xT = dt_in("xT", [D, T])
    pos = dt_in("pos", [1, T], I32)
    invcnt = dt_in("invcnt", [4, T])
    percore = dt_in("percore", [128, 2])
    consts = dt_in("consts", [128, 128 * 3 + 256 + 2048 + 2])
    vecs = dt_in("vecs", [2, 128, 64])
    lam_in = dt_in("lam_in", [2, 1, 256])
    fin_g = dt_in("fin_g", [128, 8])
    w_in = dt_in("w_in", [2, D, 7424])
    pool_w = dt_in("pool_w", [2, 4, 128, 128])
    w_pa = dt_in("w_pa", [2, 256, D])
    w_pb = dt_in("w_pb", [2, 512, D])
    w_pc = dt_in("w_pc", [2, 512, D])
    w_out = dt_in("w_out", [2, D, D])
    ffn_g = dt_in("ffn_g", [D, DFF])
    ffn_u = dt_in("ffn_u", [D, DFF])
    ffn_d = dt_in("ffn_d", [DFF, D])
    router = dt_in("router", [128, 8, NE])
    moe_g = dt_in("moe_g", [NE, D, DFF])
    moe_u = dt_in("moe_u", [NE, D, DFF])
    moe_d = dt_in("moe_d", [NE, DFF, D])
    outT = nc.dram_tensor("outT", [D, TO], F32, kind="ExternalOutput").ap()
    dbg = nc.dram_tensor("dbg", [D, T], F32, kind="ExternalOutput").ap() if debug else None

    xs = dt_sc("xs", [D, T], F32)
    hT = dt_sc("hT", [D, T])
    QAt = dt_sc("QAt", [768, T]); KAt = dt_sc("KAt", [768, T]); VA = dt_sc("VA", [T, 768])
    QBt = dt_sc("QBt", [512, T]); KBt = dt_sc("KBt", [512, T]); VB = dt_sc("VB", [T, 512])
    Ct = dt_sc("Ct", [512, T])
    OAt = dt_sc("OAt", [256, T]); OBt = dt_sc("OBt", [512, T]); OCt = dt_sc("OCt", [512, T])
    bar_d = dt_sc("bar_d", [2, 8], F32)
    Wg_b = dt_sc("Wg_b", [9, 14, 128, 2048]); Wu_b = dt_sc("Wu_b", [9, 14, 128, 2048])
    Wd_b = dt_sc("Wd_b", [9, 8, 128, 3584])

    with ExitStack() as st:
        b = B(nc, st)
        S = b.S
        sbt = lambda n, s, d: st.enter_context(nc.sbuf_tensor(n, list(s), d))
        NBF = 63600
        NF32 = 16128
        abf_t = sbt("abf", [128, NBF], BF16)
        af_t = sbt("af32", [128, NF32], F32)
        b.abf = Arena(abf_t, NBF)
        b.af = Arena(af_t, NF32)
        cst = sbt("cst", [128, 128 * 3 + 256 + 2048], BF16)
        cf = sbt("cf", [128, 8], F32)
        ropeC = dt_sc("ropeC", [128, T])
        ropeS = dt_sc("ropeS", [128, T])
        rtab = sbt("rtab", [128, 2, 2048], BF16)
        brt = Buf("rtab")
        vec_sb = sbt("vec_sb", [128, 2, 64], F32)
        lam_sb = sbt("lam_sb", [128, 2, 8], F32)
        fing_sb = sbt("fing_sb", [128, 8], F32)
        b.bar_t = sbt("bar_t", [128, 4], F32)
        b.bar_d = bar_d
        ps = [st.enter_context(nc.psum_tensor(f"ps{i}", [128, 512], F32))[:, :] for i in range(8)]
        pb = [Buf(f"ps{i}") for i in range(8)]
        b.bar_ps = st.enter_context(nc.psum_tensor("barps", [128, 16], F32)) if False else ps[7]
        ident = cst[:, 0:128]
        ones = cst[:, 128:256]
        Rblk = cst[:, 256:384]
        maskA = cst[:, 384:640]
        maskB = cst[:, 640:2688].rearrange("p (i q) -> p i q", q=512)
        b.ones = ones
        freq = cf[:, 0:1]; sgn = cf[:, 1:2]; pbias = cf[:, 2:3]; pflag = cf[:, 3:4]; epsc = cf[:, 4:5]
        bc = Buf("consts")

        S.dma(cst[:, :], consts[:, 0:2688], writes=[bc], q="pool")
        S.dma(cf[:, 0:2], consts[:, 2688:2690], writes=[bc], q="sp")
        S.dma(cf[:, 2:4], percore[:, :], writes=[bc], q="sp")
        b.memset(cf[:, 4:5], EPS, [bc])
        S.dma(vec_sb[:, 0, :], vecs[0], writes=[bc], q="sp")
        S.dma(vec_sb[:, 1, :], vecs[1], writes=[bc], q="sp")
        S.dma(fing_sb[:, :], fin_g[:, :], writes=[bc], q="sp")
        for k in range(8):
            S.dma(xs[k * 128:(k + 1) * 128, :], xT[k * 128:(k + 1) * 128, :], q="sp")
        lraw = b.af.alloc([2, 256])
        blr = Buf("lraw")
        for l in range(2):
            S.dma(lraw[:, l, :], lam_in[l].broadcast_to([128, 256]), writes=[blr], q="sp")
        for l in range(2):
            linit = 0.8 - 0.6 * math.exp(-0.3 * l)
            pr = b.af.alloc([128])
            b.tt(pr[:, 0:64], lraw[:, l, 0:64], lraw[:, l, 64:128], ALU.mult, [blr], [blr])
            b.tt(pr[:, 64:128], lraw[:, l, 128:192], lraw[:, l, 192:256], ALU.mult, [blr], [blr])
            S.emit("dve", lambda e, o=lam_sb[:, l, 0:1], i=pr[:, 0:64]: e.reduce_sum(out=o, in_=i, axis=mybir.AxisListType.X), [blr], [bc])
            S.emit("dve", lambda e, o=lam_sb[:, l, 1:2], i=pr[:, 64:128]: e.reduce_sum(out=o, in_=i, axis=mybir.AxisListType.X), [blr], [bc])
            b.act(lam_sb[:, l, 2:4], lam_sb[:, l, 0:2], AF.Exp, [bc], [bc])
            b.stt(lam_sb[:, l, 4:5], lam_sb[:, l, 3:4], -linit, lam_sb[:, l, 2:3], ALU.add, ALU.subtract, [bc], [bc])
            b.ts(vec_sb[:, l, 40:41], vec_sb[:, l, 40:41], 1.0 - linit, None, ALU.mult, None, [bc], [bc])
        posi = b.af.alloc([2048]).bitcast(I32)
        bpi = Buf("posi")
        u = b.af.alloc([2048]); kf = b.af.alloc([2048]); fr = b.af.alloc([2048])
        bu = Buf("u")
        for ch in range(4):
            sl = slice(ch * 2048, (ch + 1) * 2048)
            S.dma(posi[:, :], pos[0:1, sl].broadcast_to([128, 2048]), writes=[bpi], q="sp")
            b.copy(u, posi[:, :], [bpi], [bu])
            b.ts(u, u, freq, None, ALU.mult, None, [bu, bc], [bu])
            for which, tab in ((0, ropeS), (1, ropeC)):
                if which == 1:
                    b.ts(u, u, 0.25, None, ALU.add, None, [bu], [bu])
                b.copy(posi[:, :], u, [bu], [bpi])
                b.copy(kf, posi[:, :], [bpi], [bu])
                b.tt(fr, u, kf, ALU.subtract, [bu], [bu])
                b.act(fr, fr, AF.Sin, [bu], [bu], scale=TWO_PI)
                if which == 0:
                    b.ts(rtab[:, 0, :], fr, sgn, None, ALU.mult, None, [bu, bc], [brt])
                else:
                    b.copy(rtab[:, 1, :], fr, [bu], [brt])
                S.dma(tab[:, sl], rtab[:, which, :], reads=[brt], q="sp")

        def load_w(dst, src, k_chunks, q="pool"):
            for k in range(k_chunks):
                S.dma(dst[:, k, :], src[k * 128:(k + 1) * 128, :], writes=[bw], q=q)

        def rms_tile(xt, bxt, gcol, h, bh, psb, psbuf, scr, bscr, n=512, kch=8, dim=1024.0, bsq=None):
            sq = scr["sq"]
            if bsq is None:
                bsq = bscr
            for k in range(kch):
                b.act(sq[:, k, :], xt[:, k, :], AF.Square, [bxt], [bsq])
            for k in range(kch):
                b.mm(psb[:, 0:n], ones, sq[:, k, :], k == 0, k == kch - 1, [bsq, bc], [psbuf])
            b.act(scr["ln"], psb[:, 0:n], AF.Ln, [psbuf, bc], [bscr], scale=1.0 / dim, bias=epsc)
            b.act(scr["rstd"], scr["ln"], AF.Exp, [bscr], [bscr], scale=-0.5)
            for k in range(kch):
                b.stt(h[:, k, :], xt[:, k, :], gcol[:, k:k + 1], scr["rstd"], ALU.mult, ALU.mult,
                      [bxt, bscr, bc], [bh])

        bw = Buf("w")

        def phase_p1(l, tiles_full, tiles_kvc):
            nonlocal bw
            b.barrier()
            bw = Buf("w")
            W = b.abf.alloc([8, 4352])
            load_w(W, w_in[l][:, 0:4352], 8)
            xts = [b.af.alloc([8, 512]) for _ in range(2)]
            bxts = [Buf("xt") for _ in range(2)]
            scrs = [{"sq": b.abf.alloc([8, 512]), "ln": b.af.alloc([512]), "rstd": b.af.alloc([512])} for _ in range(2)]
            bscrs = [Buf("scr") for _ in range(2)]
            hs_ = [b.abf.alloc([8, 512]) for _ in range(2)]; bhs = [Buf("h") for _ in range(2)]
            qs = [b.abf.alloc([512]) for _ in range(3)]; bqs = [Buf("q") for _ in range(3)]
            t1 = [b.af.alloc([512]) for _ in range(3)]; bt1 = [Buf("t1") for _ in range(3)]
            qo = [b.abf.alloc([512]) for _ in range(3)]; bqo = [Buf("qo") for _ in range(3)]
            vsb = b.abf.alloc([4, 1280]); bvs = Buf("vsb")
            g1 = vec_sb[:, l, 0:8]
            rc = [b.abf.alloc([512]) for _ in range(2)]; rs_ = [b.abf.alloc([512]) for _ in range(2)]
            brc = [Buf("rc") for _ in range(2)]
            nt = 0
            alltiles = sorted(set(tiles_full) | set(tiles_kvc))
            it = 0
            for ti in alltiles:
                t0 = ti * 512
                full = ti in tiles_full
                rj = nt % 2; nt += 1
                xt = xts[rj]; bxt = bxts[rj]
                h = hs_[rj]; bh = bhs[rj]; scr = scrs[rj]; bscr = bscrs[rj]

                def p1_load(tix_, jj):
                    tt0 = tix_ * 512
                    S.dma(xts[jj], xs.rearrange("(k p) t -> p k t", p=128)[:, :, tt0:tt0 + 512], writes=[bxts[jj]], q="sp")
                    S.dma(rc[jj], ropeC[:, tt0:tt0 + 512], writes=[brc[jj]], q="sp")
                    S.dma(rs_[jj], ropeS[:, tt0:tt0 + 512], writes=[brc[jj]], q="sp")
                if nt == 1:
                    p1_load(ti, rj)
                if nt < len(alltiles):
                    p1_load(alltiles[nt], nt % 2)
                rms_tile(xt, bxt, g1, h, bh, ps[0], pb[0], scr, bscr)
                if full:
                    S.dma(hT.rearrange("(k p) t -> p k t", p=128)[:, :, t0:t0 + 512], h, reads=[bh], q="sp")
                chunks = []
                if full:
                    chunks += [(0 + i * 128, QAt, i * 128, True) for i in range(6)]
                chunks += [(768 + i * 128, KAt, i * 128, True) for i in range(6)]
                if full:
                    chunks += [(2304 + i * 128, QBt, i * 128, True) for i in range(4)]
                chunks += [(2816 + i * 128, KBt, i * 128, True) for i in range(4)]
                chunks += [(3840 + i * 128, Ct, i * 128, False) for i in range(4)]
                def p1_main(ci):
                    co, dst, dr, rope = chunks[ci]
                    pi = 1 + (ci % 3)
                    for k in range(8):
                        b.mm(ps[pi], W[:, k, co:co + 128], h[:, k, :], k == 0, k == 7, [bw, bh], [pb[pi]])
                    j = ci % 3
                    if not rope:
                        b.act(qo[j], ps[pi], AF.Copy, [pb[pi]], [bqo[j]])
                    else:
                        b.act(qs[j], ps[pi], AF.Copy, [pb[pi]], [bqs[j]])

                def p1_post(ci):
                    co, dst, dr, rope = chunks[ci]
                    j = ci % 3
                    if rope:
                        pr = 4 + (ci % 2)
                        b.mm(ps[pr], Rblk, qs[j], True, True, [bc, bqs[j]], [pb[pr]])
                        b.tt(t1[j], qs[j], rc[rj], ALU.mult, [bqs[j], brc[rj]], [bt1[j]], eng="pool")
                        b.tt(qo[j], ps[pr], rs_[rj], ALU.mult, [pb[pr], brc[rj]], [bqo[j]])
                        b.tt(qo[j], qo[j], t1[j], ALU.add, [bqo[j], bt1[j]], [bqo[j]])
                    S.dma(dst[dr:dr + 128, t0:t0 + 512], qo[j], reads=[bqo[j]], q="sp")

                for ci in range(len(chunks)):
                    p1_main(ci)
                    if ci >= 1:
                        p1_post(ci - 1)
                p1_post(len(chunks) - 1)
                it += 1
                for s in range(4):
                    for (co, n, vo) in ((1536, 512, 0), (2048, 256, 512), (3328, 512, 768)):
                        pi = 1 + (it % 3); it += 1
                        for k in range(8):
                            b.mm(ps[pi][:, 0:n], h[:, k, s * 128:(s + 1) * 128], W[:, k, co:co + n], k == 0, k == 7,
                                 [bw, bh], [pb[pi]])
                        b.act(vsb[:, s, vo:vo + n], ps[pi][:, 0:n], AF.Copy, [pb[pi]], [bvs])
                S.dma(VA[t0:t0 + 512, :].rearrange("(s p) c -> p s c", p=128), vsb[:, :, 0:768], reads=[bvs], q="sp")
                S.dma(VB[t0:t0 + 512, :].rearrange("(s p) c -> p s c", p=128), vsb[:, :, 768:1280], reads=[bvs], q="sp")

        bconv = [Buf(f"conv{i}") for i in range(9)]

        def emit_conv(idx, g_src, u_src, d_src):
            for gi in range(14):
                S.dma(Wg_b[idx, gi].rearrange("p (k j) -> p k j", j=256),
                      g_src[:, gi * 256:(gi + 1) * 256].rearrange("(k p) j -> p k j", p=128),
                      writes=[bconv[idx]], q="pool", bg=True)
                S.dma(Wu_b[idx, gi].rearrange("p (k j) -> p k j", j=256),
                      u_src[:, gi * 256:(gi + 1) * 256].rearrange("(k p) j -> p k j", p=128),
                      writes=[bconv[idx]], q="pool", bg=True)
            for c in range(8):
                S.dma(Wd_b[idx, c].rearrange("p (f j) -> p f j", j=128),
                      d_src[:, c * 128:(c + 1) * 128].rearrange("(f p) j -> p f j", p=128),
                      writes=[bconv[idx]], q="pool", bg=True)

        def phase_p2b(l, qtiles):
            b.barrier()
            if l == 0:
                emit_conv(0, ffn_g, ffn_u, ffn_d)
                if n_layers > 1:
                    for e in range(NE):
                        emit_conv(1 + e, moe_g[e], moe_u[e], moe_d[e])
            nkb_max = (max(qtiles) + 1) * 4
            kts = [b.abf.alloc([T]) for _ in range(1)]; bkt = [Buf("kt") for _ in range(1)]
            vts = [b.abf.alloc([64, 128]) for _ in range(1)]; bvt = [Buf("vt") for _ in range(1)]
            qzs = [[b.abf.alloc([512]) for _ in range(2)] for _ in range(2)]; bqt = [Buf("qt") for _ in range(2)]
            for par in range(2):
                for c in range(2):
                    b.memset(qzs[par][c], 0.0, [bqt[par]])
            pts = [b.abf.alloc([512]) for _ in range(4)]; bpt = [Buf("pt") for _ in range(4)]
            rd = b.af.alloc([512]); o1 = b.af.alloc([512]); o2 = b.af.alloc([512]); lnb = b.af.alloc([512])
            sqb = b.abf.alloc([512]); ob = b.abf.alloc([512])
            be = Buf("epi"); bob = Buf("ob")
            accD = [b.af.alloc([512]) for _ in range(2)]; bacc = [Buf("accD") for _ in range(2)]
            dh = b.abf.alloc([512]); dl = b.abf.alloc([512]); bdh = Buf("dh")
            pending = [None]
            neglam = lam_sb[:, l, 4:5]
            gsub = vec_sb[:, l, 40:41]
            nq = 0
            for hh in range(4):
                kt = kts[0]; vt = vts[0]
                S.dma(kt[:, 0:nkb_max * 128], KBt[hh * 128:(hh + 1) * 128, 0:nkb_max * 128], writes=[bkt[0]], q="sp")
                for c8 in range(0, nkb_max, 8):
                    S.dma(vt[:, c8:c8 + 8, :],
                          VB[c8 * 128:(c8 + 8) * 128, hh * 128:(hh + 1) * 128].rearrange("(n p) c -> p n c", p=128),
                          writes=[bvt[0]], q="sp")
                for qi in qtiles:
                    q0 = qi * 512
                    own = q0 >= TO
                    qz = qzs[nq % 2]; bq = bqt[nq % 2]; nq += 1
                    S.dma(qz[0][0:64, :], QBt[hh * 128:hh * 128 + 64, q0:q0 + 512], writes=[bq], q="sp")
                    S.dma(qz[1][64:128, :], QBt[hh * 128 + 64:(hh + 1) * 128, q0:q0 + 512], writes=[bq], q="sp")
                    nkb = (qi + 1) * 4
                    if own:
                        kbs = list(range(0, nkb))
                    else:
                        kbs = list(range(0, nkb))
                    items = [(c, kb) for kb in kbs for c in (0, 1)]

                    def qk(i):
                        c, kb = items[i]
                        sb_i = i % 4
                        diag = kb >= qi * 4
                        rows = slice(64 * c, 64 * c + 64)
                        b.mm(ps[sb_i], kt[:, kb * 128:(kb + 1) * 128], qz[c], True, not diag,
                             [bkt[0], bq], [pb[sb_i]])
                        if diag:
                            b.mm(ps[sb_i], ident, maskB[:, kb - qi * 4, :], False, True, [bc], [pb[sb_i]])

                    LA = 3
                    n_it = len(items)
                    for _w in range(WARM_P2B if qi != qtiles[0] else WARM_B):
                        b.mm(ps[7], ones, maskB[:, 0, :], True, True, [bc], [pb[7]])
                    for i0 in range(min(LA, n_it)):
                        qk(i0)
                    for i, (c, kb) in enumerate(items):
                        if i + LA < n_it:
                            qk(i + LA)
                        sb_i = i % 4
                        pt = pts[i % 4]; bp = bpt[i % 4]
                        pref = own and kb < TO // 128
                        b.act(pt, ps[sb_i], AF.Exp, [pb[sb_i], bc], [bp], scale=0.125, bias=pbias if pref else None)
                        first = kb == kbs[0]; last = kb == kbs[-1]
                        b.mm(ps[4 + c], vt[:, kb, :], pt, first, last, [bvt[0], bp], [pb[4 + c]])
                        if first:
                            b.copy(accD[c], pt, [bp], [bacc[c]])
                        else:
                            b.tt(accD[c], accD[c], pt, ALU.add, [bp, bacc[c]], [bacc[c]])
                        if i == 6 and pending[0] is not None:
                            pending[0]()
                            pending[0] = None
                    if pending[0] is not None:
                        pending[0]()
                        pending[0] = None
                    for c in (0, 1):
                        b.copy(dh, accD[c], [bacc[c]], [bdh])
                        b.tt(dl, accD[c], dh, ALU.subtract, [bacc[c], bdh], [bdh])
                        b.mm(ps[6 + c], ones, dh, True, False, [bc, bdh], [pb[6 + c]])
                        b.mm(ps[6 + c], ones, dl, False, True, [bc, bdh], [pb[6 + c]])
                        b.act(rd, ps[6 + c], AF.Ln, [pb[6 + c]], [be])
                        b.act(rd, rd, AF.Exp, [be], [be], scale=-1.0)
                        b.tt(o1 if c == 0 else o2, ps[4 + c], rd, ALU.mult, [pb[4 + c], be], [be])
                    b.stt(o1, o2, neglam, o1, ALU.mult, ALU.add, [be, bc], [be])

                    def part2(hh=hh, q0=q0):
                        b.act(sqb, o1, AF.Square, [be], [be])
                        b.mm(ps[6], ones, sqb, True, True, [bc, be], [pb[6]])
                        b.act(lnb, ps[6], AF.Ln, [pb[6], bc], [be], scale=1.0 / 128.0, bias=epsc)
                        b.act(lnb, lnb, AF.Exp, [be], [be], scale=-0.5)
                        b.stt(ob, o1, gsub, lnb, ALU.mult, ALU.mult, [be, bc], [bob])
                        S.dma(OBt[hh * 128:(hh + 1) * 128, q0:q0 + 512], ob, reads=[bob], q="sp")
                    pending[0] = part2
            if pending[0] is not None:
                pending[0]()
                pending[0] = None

        def phase_p2a(l, segs):
            b.barrier()
            ka = b.abf.alloc([2, 4096]); bka = Buf("ka")
            qaz = [b.abf.alloc([2, 2048]) for _ in range(2)]; bqa = Buf("qa")
            for hp_ in range(2):
                b.memset(qaz[hp_], 0.0, [bqa])
            vts = [b.abf.alloc([2, 256]) for _ in range(3)]; bvts = [Buf("va") for _ in range(3)]
            pts = [b.abf.alloc([256]) for _ in range(3)]; bpts = [Buf("pa") for _ in range(3)]
            accN = b.af.alloc([2, 2048]); accD = b.af.alloc([2, 2048]); bacc = Buf("acc")
            oa = b.abf.alloc([2, 2048]); boa = Buf("oa")
            vbc = [0]; gic = [0]
            for sg in segs:
                s0 = sg * 2048
                first_seg = (sg == 0)
                own_first = (s0 == TO)
                for g, dil in enumerate((1, 4, 16)):
                    for p in range(2):
                        r0 = g * 256 + p * 128
                        if first_seg:
                            S.dma(ka[:, p, 2048:4096], KAt[r0:r0 + 128, s0:s0 + 2048], writes=[bka], q="sp")
                        else:
                            S.dma(ka[:, p, :], KAt[r0:r0 + 128, s0 - 2048:s0 + 2048], writes=[bka], q="sp")
                        S.dma(qaz[0][0:64, p, :], QAt[r0:r0 + 64, s0:s0 + 2048], writes=[bqa], q="sp")
                        S.dma(qaz[1][64:128, p, :], QAt[r0 + 64:r0 + 128, s0:s0 + 2048], writes=[bqa], q="sp")
                    nblk = 16 // dil
                    blocks = [(r, n) for r in range(dil) for n in range(nblk)]
                    nb = len(blocks)
                    vb0 = vbc[0]; vbc[0] += nb
                    g0 = gic[0]; gic[0] += nb * 4

                    def binfo(k):
                        r, n = blocks[k]
                        cs = n * 128 * dil + r
                        has_prev = not (first_seg and n == 0)
                        return r, n, cs, has_prev, cs - 128 * dil

                    def loadV(k):
                        r, n, cs, has_prev, ps_ = binfo(k)
                        vi = (vb0 + k) % 3
                        vt = vts[vi]; bv = bvts[vi]
                        tok = s0 + cs
                        S.dma(vt[:, 0, :], VA[tok:tok + 127 * dil + 1:dil, g * 256:(g + 1) * 256], writes=[bv], q="sp")
                        if has_prev:
                            tokp = s0 + ps_
                            S.dma(vt[:, 1, :], VA[tokp:tokp + 127 * dil + 1:dil, g * 256:(g + 1) * 256], writes=[bv], q="sp")

                    def stA(i):
                        k, hg = divmod(i, 4)
                        r, n, cs, has_prev, ps_ = binfo(k)
                        p = hg // 2; hp = hg % 2
                        rows = slice(64 * hp, 64 * hp + 64)
                        si = (g0 + i) % 3
                        pst = ps[si]; psb_ = pb[si]
                        qsl = slice(cs, cs + 127 * dil + 1, dil)
                        b.mm(pst[:, 0:128], ka[:, p, 2048 + cs:2048 + cs + 127 * dil + 1:dil], qaz[hp][:, p, qsl],
                             True, False, [bka, bqa], [psb_])
                        b.mm(pst[:, 0:128], ident, maskA[:, 0:128], False, True, [bc], [psb_])
                        if has_prev:
                            b.mm(pst[:, 128:256], ka[:, p, 2048 + ps_:2048 + ps_ + 127 * dil + 1:dil],
                                 qaz[hp][:, p, qsl], True, False, [bka, bqa], [psb_])
                            b.mm(pst[:, 128:256], ident, maskA[:, 128:256], False, True, [bc], [psb_])

                    def stBCD(i):
                        k, hg = divmod(i, 4)
                        r, n, cs, has_prev, ps_ = binfo(k)
                        p = hg // 2; hp = hg % 2
                        rows = slice(64 * hp, 64 * hp + 64)
                        si = (g0 + i) % 3
                        pst = ps[si]; psb_ = pb[si]
                        qsl = slice(cs, cs + 127 * dil + 1, dil)
                        vi = (vb0 + k) % 3
                        vt = vts[vi]; bv = bvts[vi]
                        pt = pts[si]; bp = bpts[si]
                        b.act(pt[:, 0:128], pst[:, 0:128], AF.Exp, [psb_], [bp], scale=0.125)
                        if has_prev:
                            pref = own_first and n == 0
                            b.act(pt[:, 128:256], pst[:, 128:256], AF.Exp, [psb_, bc], [bp], scale=0.125,
                                  bias=pbias if pref else None)
                        po = 3 + ((g0 + i) % 3)
                        pso = ps[po]; pbo = pb[po]
                        b.mm(pso[:, 0:128], vt[:, 0, p * 128:(p + 1) * 128], pt[:, 0:128], True, not has_prev,
                             [bv, bp], [pbo])
                        if has_prev:
                            b.mm(pso[:, 0:128], vt[:, 1, p * 128:(p + 1) * 128], pt[:, 128:256], False, True,
                                 [bv, bp], [pbo])
                        b.mm(pso[:, 128:256], ones, pt[:, 0:128], True, not has_prev, [bc, bp], [pbo])
                        if has_prev:
                            b.mm(pso[:, 128:256], ones, pt[:, 128:256], False, True, [bc, bp], [pbo])
                        if g == 0:
                            b.act(accN[rows, p, qsl], pso[rows, 0:128], AF.Copy, [pbo], [bacc])
                            b.copy(accD[rows, p, qsl], pso[rows, 128:256], [pbo], [bacc])
                        else:
                            b.tt(accN[rows, p, qsl], accN[rows, p, qsl], pso[rows, 0:128], ALU.add,
                                 [pbo, bacc], [bacc])
                            b.tt(accD[rows, p, qsl], accD[rows, p, qsl], pso[rows, 128:256], ALU.add,
                                 [pbo, bacc], [bacc], eng="dve")

                    n_it = nb * 4
                    for _w in range(WARM_B):
                        b.mm(ps[6], ones, maskB[:, 0, :], True, True, [bc], [pb[6]])
                    loadV(0)
                    if nb > 1:
                        loadV(1)
                    stA(0); stA(1)
                    for i in range(n_it):
                        k, hg = divmod(i, 4)
                        if hg == 0 and k + 2 < nb:
                            loadV(k + 2)
                        if i + 2 < n_it:
                            stA(i + 2)
                        stBCD(i)
                for p in range(2):
                    b.recip(accD[:, p, :], accD[:, p, :], [bacc], [bacc])
                    b.tt(oa[:, p, :], accN[:, p, :], accD[:, p, :], ALU.mult, [bacc], [boa])
                    S.dma(OAt[p * 128:(p + 1) * 128, s0:s0 + 2048], oa[:, p, :], reads=[boa], q="sp")

        def phase_p2c(l, halves):
            nonlocal bw
            b.barrier()
            bw = Buf("w")
            PW = b.abf.alloc([4, 128])
            for g in range(4):
                S.dma(PW[:, g, :], pool_w[l, g], writes=[bw], q="pool")
            cb = b.af.alloc([16 + 2048]); s2 = b.af.alloc([16 + 2048]); s4 = b.af.alloc([16 + 2048])
            icn = b.af.alloc([2048])
            bcb = Buf("cb"); bs = Buf("s"); bic = Buf("icn")
            cbf = b.abf.alloc([16 + 2048]); bcbf = Buf("cbf")
            dd = b.abf.alloc([2048]); bdd = Buf("dd")
            oc = [b.abf.alloc([512]) for _ in range(2)]; boc = [Buf("oc") for _ in range(2)]
            it = 0
            for hf in halves:
                for q4 in range(2):
                    t0 = hf * TO + q4 * 2048
                    for g, w in enumerate((2, 4, 8, 16)):
                        S.dma(cbf[:, 16:], Ct[g * 128:(g + 1) * 128, t0:t0 + 2048], writes=[bcbf], q="sp")
                        if t0 == 0:
                            b.memset(cbf[:, 0:16], 0.0, [bcbf])
                        else:
                            S.dma(cbf[:, 0:16], Ct[g * 128:(g + 1) * 128, t0 - 16:t0], writes=[bcbf], q="sp")
                        S.dma(icn, invcnt[g:g + 1, t0:t0 + 2048].broadcast_to([128, 2048]), writes=[bic], q="sp")
                        b.copy(cb, cbf, [bcbf], [bcb])
                        if t0 == TO:
                            b.ts(cb[:, 0:16], cb[:, 0:16], pflag, None, ALU.mult, None, [bcb, bc], [bcb])
                        N = 16 + 2048
                        b.tt(s2[:, 1:N], cb[:, 1:N], cb[:, 0:N - 1], ALU.add, [bcb], [bs])
                        cur = s2
                        if w >= 4:
                            b.tt(s4[:, 3:N], s2[:, 3:N], s2[:, 1:N - 2], ALU.add, [bs], [bs])
                            cur = s4
                        if w >= 8:
                            b.tt(s2[:, 7:N], s4[:, 7:N], s4[:, 3:N - 4], ALU.add, [bs], [bs])
                            cur = s2
                        if w >= 16:
                            b.tt(s4[:, 15:N], s2[:, 15:N], s2[:, 7:N - 8], ALU.add, [bs], [bs])
                            cur = s4
                        b.tt(cur[:, 16:N], cur[:, 16:N], icn, ALU.mult, [bs, bic], [bs])
                        b.tt(dd, cur[:, 16:N], cb[:, 16:N], ALU.subtract, [bs, bcb], [bdd])
                        for s in range(4):
                            pi = it % 4; it += 1
                            b.mm(ps[pi], PW[:, g, :], dd[:, s * 512:(s + 1) * 512], True, True, [bw, bdd], [pb[pi]])
                            j = it % 2
                            b.act(oc[j], ps[pi], AF.Copy, [pb[pi], bc], [boc[j]], scale=vec_sb[:, l, 41 + g:42 + g])
                            S.dma(OCt[g * 128:(g + 1) * 128, t0 + s * 512:t0 + (s + 1) * 512], oc[j], reads=[boc[j]], q="sp")

        def phase_p3(l, tiles):
            nonlocal bw
            b.barrier()
            bw = Buf("w")
            WG = b.abf.alloc([8, 3072])
            load_w(WG, w_in[l][:, 4352:7424], 8)
            WP = b.abf.alloc([10, 1024])
            load_w(WP[:, 0:2, :], w_pa[l], 2); load_w(WP[:, 2:6, :], w_pb[l], 4); load_w(WP[:, 6:10, :], w_pc[l], 4)
            WO = b.abf.alloc([8, 1024])
            load_w(WO, w_out[l], 8)
            h = b.abf.alloc([8, 512]); bh = Buf("h")
            om = b.abf.alloc([10, 512]); bom = Buf("om")
            mixed = b.abf.alloc([8, 512]); bmx = Buf("mixed")
            xts3 = [b.af.alloc([8, 512]) for _ in range(2)]; bxts3 = [Buf("xt") for _ in range(2)]
            sg = [b.af.alloc([512]) for _ in range(2)]; bsg = [Buf("sg") for _ in range(2)]
            macc = b.af.alloc([512]); bma = Buf("macc")
            it = 0
            pend3 = [None]
            for n3, ti in enumerate(tiles):
                t0 = ti * 512
                xt = xts3[n3 % 2]; bxt = bxts3[n3 % 2]
                S.dma(h, hT.rearrange("(k p) t -> p k t", p=128)[:, :, t0:t0 + 512], writes=[bh], q="sp")
                S.dma(om[:, 0:2, :], OAt.rearrange("(k p) t -> p k t", p=128)[:, :, t0:t0 + 512], writes=[bom], q="sp")
                S.dma(om[:, 2:6, :], OBt.rearrange("(k p) t -> p k t", p=128)[:, :, t0:t0 + 512], writes=[bom], q="sp")
                S.dma(om[:, 6:10, :], OCt.rearrange("(k p) t -> p k t", p=128)[:, :, t0:t0 + 512], writes=[bom], q="sp")
                S.dma(xt, xs.rearrange("(k p) t -> p k t", p=128)[:, :, t0:t0 + 512], writes=[bxt], q="sp")
                if pend3[0] is not None:
                    pend3[0]()
                    pend3[0] = None
                for c in range(8):
                    for br, (k0, k1) in enumerate(((0, 2), (2, 6), (6, 10))):
                        pg = (it % 3) * 2; it += 1
                        for k in range(8):
                            b.mm(ps[pg], WG[:, k, br * 1024 + c * 128: br * 1024 + (c + 1) * 128], h[:, k, :],
                                 k == 0, k == 7, [bw, bh], [pb[pg]])
                        for k in range(k0, k1):
                            b.mm(ps[pg + 1], WP[:, k, c * 128:(c + 1) * 128], om[:, k, :], k == k0, k == k1 - 1,
                                 [bw, bom], [pb[pg + 1]])
                        j = it % 2
                        b.act(sg[j], ps[pg], AF.Sigmoid, [pb[pg], bc], [bsg[j]], bias=vec_sb[:, l, 16 + br * 8 + c:17 + br * 8 + c])
                        if br == 0:
                            b.tt(macc, sg[j], ps[pg + 1], ALU.mult, [bsg[j], pb[pg + 1]], [bma])
                        else:
                            b.tt(sg[j], sg[j], ps[pg + 1], ALU.mult, [bsg[j], pb[pg + 1]], [bsg[j]])
                            if br == 1:
                                b.tt(macc, macc, sg[j], ALU.add, [bma, bsg[j]], [bma], eng="pool")
                            else:
                                b.tt(mixed[:, c, :], macc, sg[j], ALU.add, [bma, bsg[j]], [bmx], eng="pool")
                for c in range(8):
                    po = 6 + (c % 2)
                    for k in range(8):
                        b.mm(ps[po], WO[:, k, c * 128:(c + 1) * 128], mixed[:, k, :], k == 0, k == 7, [bw, bmx], [pb[po]])
                    b.tt(xt[:, c, :], xt[:, c, :], ps[po], ALU.add, [bxt, pb[po]], [bxt])

                def st3(t0=t0, xt=xt, bxt=bxt):
                    S.dma(xs.rearrange("(k p) t -> p k t", p=128)[:, :, t0:t0 + 512], xt, reads=[bxt], q="sp")
                pend3[0] = st3
            if pend3[0] is not None:
                pend3[0]()
                pend3[0] = None

        def phase_p4(l, tiles2, final):
            b.barrier()
            moe = (l % 2 == 1)
            wg = [b.abf.alloc([8, 256]) for _ in range(2)]
            wu = [b.abf.alloc([8, 256]) for _ in range(2)]
            bwb = [Buf("wbuf") for _ in range(2)]
            wdh = [b.abf.alloc([NF, 128]) for _ in range(2)]
            bwd = [Buf("wd") for _ in range(2)]
            actT = b.abf.alloc([NF, 1024]); bact = Buf("act")
            h = b.abf.alloc([8, 1024]); bh = Buf("h")
            lnb = b.af.alloc([1024]); rstd = b.af.alloc([1024]); bscr = Buf("scr")
            xt = b.af.alloc([8, 1024]); bxt = Buf("xt")
            sl = [b.abf.alloc([512]) for _ in range(2)]; bsl = [Buf("sl") for _ in range(2)]
            g2 = vec_sb[:, l, 8:16]
            if moe:
                hf = b.af.alloc([8, 512]); bhf = Buf("hf")
                sq = actT[:, 0:8, :]
            else:
                hbufs = [h, b.abf.alloc([8, 1024])]; bhb = [bh, Buf("h2")]
                sqd = b.af.alloc([2048]).bitcast(BF16).rearrange("p (k t) -> p k t", t=512); bsqd = Buf("sqd")
                xc = [b.af.alloc([1024]) for _ in range(2)]; bxc = [Buf("xc") for _ in range(2)]

            def prep_dense(ti, hb):
                t0_ = ti * 1024
                S.dma(xt, xs.rearrange("(k p) t -> p k t", p=128)[:, :, t0_:t0_ + 1024], writes=[bxt], q="sp")
                for hv in range(2):
                    hs = slice(hv * 512, (hv + 1) * 512)
                    for k in range(8):
                        b.act(sqd[:, k, :], xt[:, k, hs], AF.Square, [bxt], [bsqd])
                    for k in range(8):
                        b.mm(ps[0], ones, sqd[:, k, :], k == 0, k == 7, [bsqd, bc], [pb[0]])
                    b.act(lnb[:, hs], ps[0], AF.Ln, [pb[0], bc], [bscr], scale=1.0 / 1024.0, bias=epsc)
                b.act(rstd, lnb, AF.Exp, [bscr], [bscr], scale=-0.5)
                for k in range(8):
                    b.stt(hbufs[hb][:, k, :], xt[:, k, :], g2[:, k:k + 1], rstd, ALU.mult, ALU.mult,
                          [bxt, bscr, bc], [bhb[hb]])
            if moe:
                rw = sbt("rw", [128, 8, NE], F32)
                brw = Buf("rw")
                S.dma(rw[:, :, :], router[:, :, :], writes=[brw], q="sp")
                lg = b.af.alloc([4, 8]); mx = b.af.alloc([4, 8]); ex = b.af.alloc([4, 8]); msk = b.af.alloc([4, 8])
                den = b.af.alloc([4]); gT = b.abf.alloc([512]); gbc = b.abf.alloc([NE, 1024])
                gate_bf = b.abf.alloc([4, 8])
                sel = b.abf.alloc([NE, 128])
                bg = Buf("gate"); bgb = Buf("gbc"); bsel = Buf("sel")
                ytmp = [b.af.alloc([512]) for _ in range(2)]; byt = [Buf("yt") for _ in range(2)]
                for e in range(NE):
                    b.copy(sel[0:8, e, :], ident[0:8, e:e + 1].broadcast_to([8, 128]), [bc], [bsel])
            nexp = NE if moe else 1
            wcount = 0
            dcount = 0
            it = 0
            if not moe:
                prep_dense(tiles2[0], 0)
            for tix, ti in enumerate(tiles2):
                t0 = ti * 1024
                if not moe:
                    h = hbufs[tix % 2]; bh = bhb[tix % 2]
                else:
                    S.dma(xt, xs.rearrange("(k p) t -> p k t", p=128)[:, :, t0:t0 + 1024], writes=[bxt], q="sp")
                    for k in range(8):
                        b.act(sq[:, k, :], xt[:, k, :], AF.Square, [bxt], [bact])
                    for hv in range(2):
                        hs = slice(hv * 512, (hv + 1) * 512)
                        for k in range(8):
                            b.mm(ps[0], ones, sq[:, k, hs], k == 0, k == 7, [bact, bc], [pb[0]])
                        b.act(lnb[:, hs], ps[0], AF.Ln, [pb[0], bc], [bscr], scale=1.0 / 1024.0, bias=epsc)
                    b.act(rstd, lnb, AF.Exp, [bscr], [bscr], scale=-0.5)
                    for k in range(8):
                        b.stt(h[:, k, :], xt[:, k, :], g2[:, k:k + 1], rstd, ALU.mult, ALU.mult, [bxt, bscr, bc], [bh])
                if moe:
                    for hv in range(2):
                        hs = slice(hv * 512, (hv + 1) * 512)
                        for k in range(8):
                            b.stt(hf[:, k, :], xt[:, k, hs], g2[:, k:k + 1], rstd[:, hs], ALU.mult, ALU.mult,
                                  [bxt, bscr, bc], [bhf])
                        for s in range(4):
                            for k in range(8):
                                b.mm(ps[1][:, s * 8:(s + 1) * 8], hf[:, k, s * 128:(s + 1) * 128], rw[:, k, :], k == 0, k == 7,
                                     [bhf, brw], [pb[1]])
                        b.copy(lg, ps[1][:, 0:32].rearrange("p (s e) -> p s e", e=8), [pb[1]], [bg])
                        for s in range(4):
                            S.emit("dve", lambda e_, o=mx[:, s, :], i=lg[:, s, :]: e_.max(out=o, in_=i), [bg], [bg])
                            b.ts(den[:, s:s + 1], mx[:, s, 0:1], -1.0, None, ALU.mult, None, [bg], [bg])
                            b.act(ex[:, s, :], lg[:, s, :], AF.Exp, [bg], [bg], bias=den[:, s:s + 1])
                            b.ts(msk[:, s, :], lg[:, s, :], mx[:, s, 1:2], None, ALU.is_ge, None, [bg], [bg])
                            b.tt(ex[:, s, :], ex[:, s, :], msk[:, s, :], ALU.mult, [bg], [bg])
                            S.emit("dve", lambda e_, o=den[:, s:s + 1], i=ex[:, s, :]: e_.reduce_sum(
                                out=o, in_=i, axis=mybir.AxisListType.X), [bg], [bg])
                            b.recip(den[:, s:s + 1], den[:, s:s + 1], [bg], [bg])
                            b.ts(gate_bf[:, s, :], ex[:, s, :], den[:, s:s + 1], None, ALU.mult, None, [bg], [bg])
                        for s in range(4):
                            b.mm(ps[2][0:8, s * 128:(s + 1) * 128], gate_bf[:, s, :], ident, True, True, [bg, bc], [pb[2]])
                        b.copy(gT[0:8, :], ps[2][0:8, :], [pb[2]], [bg])
                        for e in range(NE):
                            b.mm(ps[3], sel[0:8, e, :], gT[0:8, :], True, True, [bsel, bg], [pb[3]])
                            b.act(gbc[:, e, hs], ps[3], AF.Copy, [pb[3]], [bgb])
                for e in range(nexp):
                    widx = 1 + e if moe else 0
                    for gi in range(14):
                        wb = wcount % 2; wcount += 1
                        S.dma(wg[wb], Wg_b[widx, gi].rearrange("p (k j) -> p k j", j=256), reads=[bconv[widx]],
                              writes=[bwb[wb]], q="sp")
                        S.dma(wu[wb], Wu_b[widx, gi].rearrange("p (k j) -> p k j", j=256), reads=[bconv[widx]],
                              writes=[bwb[wb]], q="sp")
                        for fi in range(2):
                            f = gi * 2 + fi
                            for k in range(8):
                                for hv in range(2):
                                    b.mm(ps[4 + hv], wg[wb][:, k, fi * 128:(fi + 1) * 128], h[:, k, hv * 512:(hv + 1) * 512],
                                         k == 0, k == 7, [bwb[wb], bh], [pb[4 + hv]])
                            for k in range(8):
                                for hv in range(2):
                                    b.mm(ps[6 + hv], wu[wb][:, k, fi * 128:(fi + 1) * 128], h[:, k, hv * 512:(hv + 1) * 512],
                                         k == 0, k == 7, [bwb[wb], bh], [pb[6 + hv]])
                            for hv in range(2):
                                j = it % 2; it += 1
                                b.act(sl[j], ps[4 + hv], AF.Silu, [pb[4 + hv]], [bsl[j]])
                                b.tt(actT[:, f, hv * 512:(hv + 1) * 512], sl[j], ps[6 + hv], ALU.mult,
                                     [bsl[j], pb[6 + hv]], [bact])
                    if (not moe) and tix + 1 < len(tiles2):
                        prep_dense(tiles2[tix + 1], (tix + 1) % 2)
                    dbase = dcount; dcount += 8

                    def load_wd(c_):
                        db_ = (dbase + c_) % 2
                        S.dma(wdh[db_], Wd_b[widx, c_].rearrange("p (f j) -> p f j", j=128), reads=[bconv[widx]],
                              writes=[bwd[db_]], q="sp")
                    load_wd(0)
                    for c in range(8):
                        db = (dbase + c) % 2
                        pp = (1, 2) if c % 2 == 0 else (3, 5)
                        if not moe:
                            xj = c % 2
                            S.dma(xc[xj], xs[c * 128:(c + 1) * 128, t0:t0 + 1024], writes=[bxc[xj]], q="sp")
                        if c + 1 < 8:
                            load_wd(c + 1)
                        for f in range(NF):
                            for hv in range(2):
                                b.mm(ps[pp[hv]], wdh[db][:, f, :], actT[:, f, hv * 512:(hv + 1) * 512], f == 0, f == NF - 1,
                                     [bwd[db], bact], [pb[pp[hv]]])
                        for hv in range(2):
                            hs = slice(hv * 512, (hv + 1) * 512)
                            po = pp[hv]
                            if moe:
                                j = hv
                                b.tt(ytmp[j], ps[po], gbc[:, e, hs], ALU.mult, [pb[po], bgb], [byt[j]])
                                b.tt(xt[:, c, hs], xt[:, c, hs], ytmp[j], ALU.add, [bxt, byt[j]], [bxt], eng="pool")
                            else:
                                b.tt(xc[xj][:, hs], xc[xj][:, hs], ps[po], ALU.add, [bxc[xj], pb[po]], [bxc[xj]])
                        if not moe:
                            S.dma(xs[c * 128:(c + 1) * 128, t0:t0 + 1024], xc[xj], reads=[bxc[xj]], q="sp")
                if final:
                    for k in range(8):
                        b.act(sq[:, k, :], xt[:, k, :], AF.Square, [bxt], [bact])
                    for hv in range(2):
                        hs = slice(hv * 512, (hv + 1) * 512)
                        for k in range(8):
                            b.mm(ps[0], ones, sq[:, k, hs], k == 0, k == 7, [bact, bc], [pb[0]])
                        b.act(lnb[:, hs], ps[0], AF.Ln, [pb[0], bc], [bscr], scale=1.0 / 1024.0, bias=epsc)
                    b.act(rstd, lnb, AF.Exp, [bscr], [bscr], scale=-0.5)
                    for hv in range(2):
                        hs = slice(hv * 512, (hv + 1) * 512)
                        for k in range(8):
                            b.stt(hf[:, k, :], xt[:, k, hs], fing_sb[:, k:k + 1], rstd[:, hs], ALU.mult, ALU.mult,
                                  [bxt, bscr, bc], [bhf])
                        S.dma(outT.rearrange("(k p) t -> p k t", p=128)[:, :, t0 - TO + hv * 512:t0 - TO + (hv + 1) * 512],
                              hf, reads=[bhf], q="sp")
                elif moe:
                    S.dma(xs.rearrange("(k p) t -> p k t", p=128)[:, :, t0:t0 + 1024], xt, reads=[bxt], q="sp")

        all_tiles = list(range(16)); own_tiles = list(range(8, 16))
        for l in range(n_layers):
            last = (l == n_layers - 1)
            if not last:
                phase_p1(l, all_tiles, all_tiles)
                phase_p2b(l, all_tiles)
                phase_p2a(l, [0, 1, 2, 3])
                phase_p2c(l, [0, 1])
                phase_p3(l, all_tiles)
                phase_p4(l, list(range(8)), False)
            else:
                phase_p1(l, own_tiles, all_tiles)
                phase_p2b(l, own_tiles)
                phase_p2a(l, [2, 3])
                phase_p2c(l, [1])
                phase_p3(l, own_tiles)
                phase_p4(l, list(range(4, 8)), True)
        if debug:
            b.barrier()
            for k in range(8):
                S.dma(dbg[k * 128:(k + 1) * 128, :], xs[k * 128:(k + 1) * 128, :], q="sp")
        S.run(nc)
    return nc


def _consts():
    ident = np.eye(128, dtype=np.float32)
    ones = np.ones((128, 128), np.float32)
    R = np.zeros((128, 128), np.float32)
    for hb in (0, 64):
        for d in range(16):
            src = d + 8 if d < 8 else d - 8
            R[hb + src, hb + d] = 1.0
    k = np.arange(128)[:, None]
    q = np.arange(128)[None, :]
    cur = np.where(q >= k, 0.0, NEGM).astype(np.float32)
    prev = np.where(k >= q, 0.0, NEGM).astype(np.float32)
    maskA = np.concatenate([cur, prev], axis=1)
    q5 = np.arange(512)[None, :]
    maskB = np.concatenate([np.where(128 * i + k <= q5, 0.0, NEGM).astype(np.float32) for i in range(4)], axis=1)
    inv_freq = (500000.0 ** (-np.arange(0, 16, 2, dtype=np.float32) / 16.0)).astype(np.float32)
    freq = np.zeros((128, 1), np.float32)
    sgn = np.zeros((128, 1), np.float32)
    for p in range(128):
        d = p % 64
        if d < 16:
            freq[p, 0] = inv_freq[d % 8] / (2.0 * np.pi)
            sgn[p, 0] = -1.0 if d < 8 else 1.0
    return np.concatenate([ident, ones, R, maskA, maskB, freq, sgn], axis=1).astype(np.float32)


def _feat(v):
    v = np.asarray(v, np.float32)
    return np.ascontiguousarray(v.reshape(-1, 128).T)


_NC_CACHE = {}


def make_in_maps(x, positions, norm1_g, w_in, b_gate, diff_lambda, diff_subln_g, pool_w, pool_scale,
                 w_proj_a, w_proj_b, w_proj_c, w_out, norm2_g, ffn_w_gate, ffn_w_up, ffn_w_down,
                 moe_router, moe_w_gate, moe_w_up, moe_w_down, final_norm_g):
    f32 = lambda a: np.ascontiguousarray(np.asarray(a, dtype=np.float32))
    x = f32(x); positions = np.asarray(positions).astype(np.int32)
    consts = _consts()
    vecs = np.zeros((2, 128, 64), np.float32)
    for l in range(2):
        vecs[l, :, 0:8] = _feat(norm1_g[l])
        vecs[l, :, 8:16] = _feat(norm2_g[l])
        for i in range(3):
            vecs[l, :, 16 + i * 8:24 + i * 8] = _feat(b_gate[l][i])
        vecs[l, :, 40] = np.asarray(diff_subln_g[l], np.float32)
        vecs[l, :, 41:45] = _feat(pool_scale[l])
    lam_in = f32(diff_lambda).reshape(2, 1, 256)
    shared = {
        "consts": consts, "vecs": vecs, "lam_in": lam_in, "fin_g": _feat(final_norm_g),
        "w_in": f32(w_in), "pool_w": f32(pool_w), "w_pa": f32(w_proj_a), "w_pb": f32(w_proj_b),
        "w_pc": f32(w_proj_c), "w_out": f32(w_out), "ffn_g": f32(ffn_w_gate)[0], "ffn_u": f32(ffn_w_up)[0],
        "ffn_d": f32(ffn_w_down)[0],
        "router": np.ascontiguousarray(f32(moe_router)[0].reshape(8, 128, NE).transpose(1, 0, 2)),
        "moe_g": f32(moe_w_gate)[0], "moe_u": f32(moe_w_up)[0], "moe_d": f32(moe_w_down)[0],
    }
    in_maps = []
    tpos = np.arange(8192)
    for c in range(8):
        bi, j = c // 2, c % 2
        order = np.concatenate([np.arange(4096), np.arange(4096, 8192)]) if j == 1 else \
            np.concatenate([np.arange(4096, 8192), np.arange(4096)])
        m = dict(shared)
        m["xT"] = np.ascontiguousarray(x[bi][order].T)
        m["pos"] = np.ascontiguousarray(positions[bi][order][None, :])
        tp = tpos[order]
        m["invcnt"] = np.stack([1.0 / np.minimum(tp + 1, w) for w in (2, 4, 8, 16)]).astype(np.float32)
        pc = np.zeros((128, 2), np.float32)
        pc[:, 0] = 0.0 if j == 1 else NEGM
        pc[:, 1] = 1.0 if j == 1 else 0.0
        m["percore"] = pc
        in_maps.append(m)
    return in_maps


def kernel(**inputs):
    if "nc" not in _NC_CACHE:
        _NC_CACHE["nc"] = build_program()
    nc = _NC_CACHE["nc"]
    in_maps = make_in_maps(**inputs)
    res = run_bass_kernel_spmd(nc, in_maps, core_ids=list(range(8)))
    out = np.zeros((4, 8192, 1024), np.float32)
    for c in range(8):
        bi, j = c // 2, c % 2
        out[bi, j * 4096:(j + 1) * 4096, :] = res.results[c]["outT"].T
    return out
```

```python
import math
from contextlib import ExitStack

import numpy as np
import concourse.bass as bass
import concourse.mybir as mybir
from concourse.bass_utils import run_bass_kernel_spmd

F32 = mybir.dt.float32
BF16 = mybir.dt.bfloat16
I32 = mybir.dt.int32
AF = mybir.ActivationFunctionType
ALU = mybir.AluOpType

SEM_BLOCK = 16384
N_ENG_SEMS = 12
N_DMA_SLOTS = 24
SAME_ENGINE_SYNC = True

D = 1024
T = 8192
TO = 4096
NEGM = -30000.0
EPS = 1e-6
DFF = 3584
NF = DFF // 128
NE = 8
TWO_PI = 6.28318
WARM_B = 12
WARM_P2B = 0


class Buf:
    __slots__ = ("name", "w", "r", "rd")

    def __init__(self, name=""):
        self.name = name
        self.w = None
        self.r = {}
        self.rd = []


class Op:
    __slots__ = ("idx", "eng", "fn", "deps", "needs_inc", "dma", "cnt", "slot", "sval")

    def __init__(self, idx, eng, fn, dma):
        self.idx = idx
        self.eng = eng
        self.fn = fn
        self.deps = []
        self.needs_inc = False
        self.dma = dma
        self.cnt = None
        self.slot = None
        self.sval = None


class Sched:
    ENGS = ("pe", "act", "dve", "pool", "sp")

    def __init__(self):
        self.ops = []
        self.last = {e: None for e in self.ENGS}
        self.dmas = {e: [] for e in self.ENGS}

    def emit(self, eng, fn, reads=(), writes=(), dma=False, extra=(), bg=False):
        op = Op(len(self.ops), eng, fn, dma)
        deps = {}
        for b in reads:
            if b.w is not None:
                deps[b.w.idx] = b.w
        for b in writes:
            if b.w is not None:
                deps[b.w.idx] = b.w
            for o in b.r.values():
                deps[o.idx] = o
            for o in b.rd:
                deps[o.idx] = o
        for o in extra:
            if o is not None:
                deps[o.idx] = o
        for d in deps.values():
            if (not d.dma) and d.eng == eng and (eng == "pe" or not SAME_ENGINE_SYNC):
                continue
            op.deps.append(d)
            d.needs_inc = True
        for b in reads:
            if dma:
                b.rd.append(op)
            else:
                b.r[eng] = op
        for b in writes:
            b.w = op
            b.r = {}
            b.rd = []
        self.ops.append(op)
        if dma:
            if not bg:
                self.dmas[eng].append(op)
        else:
            self.last[eng] = op
        return op

    def dma(self, out, in_, reads=(), writes=(), q="sp", bg=False):
        return self.emit(q, lambda e, o=out, i=in_: e.dma_start(out=o, in_=i), reads, writes, dma=True, bg=bg)

    def finalize(self):
        cnt = {e: 0 for e in self.ENGS}
        ndma = {e: 0 for e in self.ENGS}
        self.dma_ops = {e: [] for e in self.ENGS}
        for op in self.ops:
            if op.dma:
                i = ndma[op.eng]
                ndma[op.eng] += 1
                op.slot = i % N_DMA_SLOTS
                op.sval = 16 * (i // N_DMA_SLOTS + 1)
                if i >= N_DMA_SLOTS:
                    op.deps.append(self.dma_ops[op.eng][i - N_DMA_SLOTS])
                self.dma_ops[op.eng].append(op)
            elif op.needs_inc:
                cnt[op.eng] += 1
                op.cnt = cnt[op.eng]
        self.cnt = cnt
        self.ndma = ndma

    def run(self, nc):
        self.finalize()
        with ExitStack() as st:
            esems = {}
            for e in self.ENGS:
                nblk = (self.cnt[e] + SEM_BLOCK - 1) // SEM_BLOCK
                assert nblk <= N_ENG_SEMS, (e, self.cnt[e])
                esems[e] = [st.enter_context(nc.semaphore(f"s_{e}{k}")) for k in range(max(nblk, 1))]
            dsems = {}
            for e in self.ENGS:
                if self.ndma[e]:
                    dsems[e] = [st.enter_context(nc.semaphore(f"d_{e}{k}")) for k in range(N_DMA_SLOTS)]
            block = st.enter_context(nc.Block())

            def target(d):
                if d.dma:
                    return ("d", d.eng, d.slot), dsems[d.eng][d.slot], d.sval, d.sval
                blk = (d.cnt - 1) // SEM_BLOCK
                return ("c", d.eng), esems[d.eng][blk], (d.cnt - 1) % SEM_BLOCK + 1, d.cnt

            by_eng = {e: [o for o in self.ops if o.eng == e] for e in self.ENGS}

            def body(ename):
                def f(eng):
                    known = {}
                    for op in by_eng[ename]:
                        for d in op.deps:
                            key, sem, val, gval = target(d)
                            if known.get(key, 0) >= gval:
                                continue
                            eng.wait_ge(sem, val)
                            known[key] = gval
                        inst = op.fn(eng)
                        if op.dma:
                            inst.then_inc(dsems[ename][op.slot], 16)
                        elif op.needs_inc:
                            blk = (op.cnt - 1) // SEM_BLOCK
                            inst.then_inc(esems[ename][blk], 1)
                    if ename == "sp":
                        for q in self.ENGS:
                            for o in self.dma_ops[q][-N_DMA_SLOTS:]:
                                key, sem, val, gval = target(o)
                                if known.get(key, 0) >= gval:
                                    continue
                                eng.wait_ge(sem, val)
                                known[key] = gval
                return f

            block.tensor(body("pe"))
            block.scalar(body("act"))
            block.vector(body("dve"))
            block.gpsimd(body("pool"))
            block.sync(body("sp"))


class Arena:
    def __init__(self, ap, n):
        self.ap = ap
        self.n = n
        self.off = 0

    def reset(self):
        self.off = 0

    def alloc(self, shape):
        n = 1
        for s in shape:
            n *= s
        assert self.off + n <= self.n, ("arena overflow", self.off, n, self.n)
        a = self.ap[:, self.off:self.off + n]
        self.off += n
        if len(shape) == 2:
            a = a.rearrange("p (a b) -> p a b", b=shape[1])
        elif len(shape) == 3:
            a = a.rearrange("p (a b c) -> p a b c", b=shape[1], c=shape[2])
        return a


class B:
    def __init__(self, nc, st):
        self.nc = nc
        self.S = Sched()
        self.st = st
        self.nbuf = 0

    def buf(self, name="b"):
        self.nbuf += 1
        return Buf(f"{name}{self.nbuf}")

    def mm(self, out, lhsT, rhs, start, stop, reads, writes):
        return self.S.emit("pe", lambda e, o=out, l=lhsT, r=rhs, a=start, b=stop:
                           e.matmul(o, l, r, start=a, stop=b), reads, writes)

    def act(self, out, in_, func, reads, writes, scale=1.0, bias=None, eng="act"):
        if bias is None:
            fn = lambda e, o=out, i=in_, f=func, s=scale: e.activation(out=o, in_=i, func=f, scale=s)
        else:
            fn = lambda e, o=out, i=in_, f=func, s=scale, b=bias: e.activation(out=o, in_=i, func=f, scale=s, bias=b)
        return self.S.emit(eng, fn, reads, writes)

    def tt(self, out, in0, in1, op, reads, writes, eng="dve"):
        return self.S.emit(eng, lambda e, o=out, a=in0, b=in1, p=op: e.tensor_tensor(out=o, in0=a, in1=b, op=p),
                           reads, writes)

    def ts(self, out, in0, s1, s2, op0, op1, reads, writes, eng="dve"):
        if s2 is None:
            fn = lambda e, o=out, a=in0, x=s1, p=op0: e.tensor_scalar(out=o, in0=a, scalar1=x, scalar2=None, op0=p)
        else:
            fn = lambda e, o=out, a=in0, x=s1, y=s2, p=op0, q=op1: e.tensor_scalar(
                out=o, in0=a, scalar1=x, scalar2=y, op0=p, op1=q)
        return self.S.emit(eng, fn, reads, writes)

    def stt(self, out, in0, scalar, in1, op0, op1, reads, writes, eng="dve"):
        return self.S.emit(eng, lambda e, o=out, a=in0, s=scalar, b=in1, p=op0, q=op1:
                           e.scalar_tensor_tensor(out=o, in0=a, scalar=s, in1=b, op0=p, op1=q), reads, writes)

    def copy(self, out, in_, reads, writes, eng="dve"):
        return self.S.emit(eng, lambda e, o=out, i=in_: e.tensor_copy(out=o, in_=i), reads, writes)

    def recip(self, out, in_, reads, writes):
        return self.S.emit("dve", lambda e, o=out, i=in_: e.reciprocal(out=o, in_=i), reads, writes)

    def memset(self, ap, val, writes, eng="dve"):
        return self.S.emit(eng, lambda e, a=ap, v=val: e.memset(a, v), (), writes)

    def barrier(self):
        S = self.S
        deps = [S.last[e] for e in S.ENGS]
        for q in S.ENGS:
            deps += S.dmas[q][-N_DMA_SLOTS:]
        z = self.bar_t
        S.emit("act", lambda e: e.memset(z[:, 0:1], 0.0) if hasattr(e, "memset") else e.activation(
            out=z[:, 0:1], in_=z[:, 0:1], func=AF.Copy), (), (), extra=deps)
        S.emit("dve", lambda e: e.memset(z[:, 1:2], 0.0), (), (), extra=deps)
        S.emit("pool", lambda e: e.memset(z[:, 2:3], 0.0), (), (), extra=deps)
        S.emit("pe", lambda e: e.matmul(self.bar_ps[0:1, 0:1], self.ones[0:1, 0:1], self.ones[0:1, 0:1],
                                        start=True, stop=True), (), (), extra=deps)
        S.emit("sp", lambda e: e.dma_start(out=self.bar_d[0:1, 0:8], in_=self.bar_d[1:2, 0:8]), (), (),
               dma=True, extra=deps)
        self.abf.reset()
        self.af.reset()


def build_program(debug=False, n_layers=2, dense_moe=True):
    nc = bass.Bass("TRN2", target_bir_lowering=False)
    dt_in = lambda n, s, d=F32: nc.dram_tensor(n, list(s), d, kind="ExternalInput").ap()
    dt_sc = lambda n, s, d=BF16: nc.dram_tensor(n, list(s), d, kind="Internal").ap()

    xT = dt_in("xT", [D, T])
    pos = dt_in("pos", [1, T], I32)
    invcnt = dt_in("invcnt", [4, T])
    percore = dt_in("percore", [128, 2])
    consts = dt_in("consts", [128, 128 * 3 + 256 + 2048 + 2])
    vecs = dt_in("vecs", [2, 128, 64])
    lam_in = dt_in("lam_in", [2, 1, 256])
    fin_g = dt_in("fin_g", [128, 8])
    w_in = dt_in("w_in", [2, D, 7424])
    pool_w = dt_in("pool_w", [2, 4, 128, 128])
    w_pa = dt_in("w_pa", [2, 256, D])
    w_pb = dt_in("w_pb", [2, 512, D])
    w_pc = dt_in("w_pc", [2, 512, D])
    w_out = dt_in("w_out", [2, D, D])
    ffn_g = dt_in("ffn_g", [D, DFF])
    ffn_u = dt_in("ffn_u", [D, DFF])
    ffn_d = dt_in("ffn_d", [DFF, D])
    router = dt_in("router", [128, 8, NE])
    moe_g = dt_in("moe_g", [NE, D, DFF])
    moe_u = dt_in("moe_u", [NE, D, DFF])
    moe_d = dt_in("moe_d", [NE, DFF, D])
    outT = nc.dram_tensor("outT", [D, TO], F32, kind="ExternalOutput").ap()
    dbg = nc.dram_tensor("dbg", [D, T], F32, kind="ExternalOutput").ap() if debug else None

    xs = dt_sc("xs", [D, T], F32)
    hT = dt_sc("hT", [D, T])
    QAt = dt_sc("QAt", [768, T]); KAt = dt_sc("KAt", [768, T]); VA = dt_sc("VA", [T, 768])
    QBt = dt_sc("QBt", [512, T]); KBt = dt_sc("KBt", [512, T]); VB = dt_sc("VB", [T, 512])
    Ct = dt_sc("Ct", [512, T])
    OAt = dt_sc("OAt", [256, T]); OBt = dt_sc("OBt", [512, T]); OCt = dt_sc("OCt", [512, T])
    bar_d = dt_sc("bar_d", [2, 8], F32)
    Wg_b = dt_sc("Wg_b", [9, 14, 128, 2048]); Wu_b = dt_sc("Wu_b", [9, 14, 128, 2048])
    Wd_b = dt_sc("Wd_b", [9, 8, 128, 3584])

    with ExitStack() as st:
        b = B(nc, st)
        S = b.S
        sbt = lambda n, s, d: st.enter_context(nc.sbuf_tensor(n, list(s), d))
        NBF = 63600
        NF32 = 16128
        abf_t = sbt("abf", [128, NBF], BF16)
        af_t = sbt("af32", [128, NF32], F32)
        b.abf = Arena(abf_t, NBF)
        b.af = Arena(af_t, NF32)
        cst = sbt("cst", [128, 128 * 3 + 256 + 2048], BF16)
        cf = sbt("cf", [128, 8], F32)
        ropeC = dt_sc("ropeC", [128, T])
        ropeS = dt_sc("ropeS", [128, T])
        rtab = sbt("rtab", [128, 2, 2048], BF16)
        brt = Buf("rtab")
        vec_sb = sbt("vec_sb", [128, 2, 64], F32)
        lam_sb = sbt("lam_sb", [128, 2, 8], F32)
        fing_sb = sbt("fing_sb", [128, 8], F32)
        b.bar_t = sbt("bar_t", [128, 4], F32)
        b.bar_d = bar_d
        ps = [st.enter_context(nc.psum_tensor(f"ps{i}", [128, 512], F32))[:, :] for i in range(8)]
        pb = [Buf(f"ps{i}") for i in range(8)]
        b.bar_ps = st.enter_context(nc.psum_tensor("barps", [128, 16], F32)) if False else ps[7]
        ident = cst[:, 0:128]
        ones = cst[:, 128:256]
        Rblk = cst[:, 256:384]
        maskA = cst[:, 384:640]
        maskB = cst[:, 640:2688].rearrange("p (i q) -> p i q", q=512)
        b.ones = ones
        freq = cf[:, 0:1]; sgn = cf[:, 1:2]; pbias = cf[:, 2:3]; pflag = cf[:, 3:4]; epsc = cf[:, 4:5]
        bc = Buf("consts")

        S.dma(cst[:, :], consts[:, 0:2688], writes=[bc], q="pool")
        S.dma(cf[:, 0:2], consts[:, 2688:2690], writes=[bc], q="sp")
        S.dma(cf[:, 2:4], percore[:, :], writes=[bc], q="sp")
        b.memset(cf[:, 4:5], EPS, [bc])
        S.dma(vec_sb[:, 0, :], vecs[0], writes=[bc], q="sp")
        S.dma(vec_sb[:, 1, :], vecs[1], writes=[bc], q="sp")
        S.dma(fing_sb[:, :], fin_g[:, :], writes=[bc], q="sp")
        b.memset(b.bar_t[:, :], 0.0, [bc])
        S.dma(bar_d[0:2, 0:8], vec_sb[0:2, 0, 0:8], reads=[bc], q="sp")
        for k in range(8):
            S.dma(xs[k * 128:(k + 1) * 128, :], xT[k * 128:(k + 1) * 128, :], q="sp")
        lraw = b.af.alloc([2, 256])
        blr = Buf("lraw")
        for l in range(2):
            S.dma(lraw[:, l, :], lam_in[l].broadcast_to([128, 256]), writes=[blr], q="sp")
        for l in range(2):
            linit = 0.8 - 0.6 * math.exp(-0.3 * l)
            pr = b.af.alloc([128])
            b.tt(pr[:, 0:64], lraw[:, l, 0:64], lraw[:, l, 64:128], ALU.mult, [blr], [blr])
            b.tt(pr[:, 64:128], lraw[:, l, 128:192], lraw[:, l, 192:256], ALU.mult, [blr], [blr])
            S.emit("dve", lambda e, o=lam_sb[:, l, 0:1], i=pr[:, 0:64]: e.reduce_sum(out=o, in_=i, axis=mybir.AxisListType.X), [blr], [bc])
            S.emit("dve", lambda e, o=lam_sb[:, l, 1:2], i=pr[:, 64:128]: e.reduce_sum(out=o, in_=i, axis=mybir.AxisListType.X), [blr], [bc])
            b.act(lam_sb[:, l, 2:4], lam_sb[:, l, 0:2], AF.Exp, [bc], [bc])
            b.stt(lam_sb[:, l, 4:5], lam_sb[:, l, 3:4], -linit, lam_sb[:, l, 2:3], ALU.add, ALU.subtract, [bc], [bc])
            b.ts(vec_sb[:, l, 40:41], vec_sb[:, l, 40:41], 1.0 - linit, None, ALU.mult, None, [bc], [bc])
        posi = b.af.alloc([2048]).bitcast(I32)
        bpi = Buf("posi")
        u = b.af.alloc([2048]); kf = b.af.alloc([2048]); fr = b.af.alloc([2048])
        bu = Buf("u")
        for ch in range(4):
            sl = slice(ch * 2048, (ch + 1) * 2048)
            S.dma(posi[:, :], pos[0:1, sl].broadcast_to([128, 2048]), writes=[bpi], q="sp")
            b.copy(u, posi[:, :], [bpi], [bu])
            b.ts(u, u, freq, None, ALU.mult, None, [bu, bc], [bu])
            for which, tab in ((0, ropeS), (1, ropeC)):
                if which == 1:
                    b.ts(u, u, 0.25, None, ALU.add, None, [bu], [bu])
                b.copy(posi[:, :], u, [bu], [bpi])
                b.copy(kf, posi[:, :], [bpi], [bu])
                b.tt(fr, u, kf, ALU.subtract, [bu], [bu])
                b.act(fr, fr, AF.Sin, [bu], [bu], scale=TWO_PI)
                if which == 0:
                    b.ts(rtab[:, 0, :], fr, sgn, None, ALU.mult, None, [bu, bc], [brt])
                else:
                    b.copy(rtab[:, 1, :], fr, [bu], [brt])
                S.dma(tab[:, sl], rtab[:, which, :], reads=[brt], q="sp")

        def load_w(dst, src, k_chunks, q="pool"):
            for k in range(k_chunks):
                S.dma(dst[:, k, :], src[k * 128:(k + 1) * 128, :], writes=[bw], q=q)

        def rms_tile(xt, bxt, gcol, h, bh, psb, psbuf, scr, bscr, n=512, kch=8, dim=1024.0, bsq=None):
            sq = scr["sq"]
            if bsq is None:
                bsq = bscr
            for k in range(kch):
                b.act(sq[:, k, :], xt[:, k, :], AF.Square, [bxt], [bsq])
            for k in range(kch):
                b.mm(psb[:, 0:n], ones, sq[:, k, :], k == 0, k == kch - 1, [bsq, bc], [psbuf])
            b.act(scr["ln"], psb[:, 0:n], AF.Ln, [psbuf, bc], [bscr], scale=1.0 / dim, bias=epsc)
            b.act(scr["rstd"], scr["ln"], AF.Exp, [bscr], [bscr], scale=-0.5)
            for k in range(kch):
                b.stt(h[:, k, :], xt[:, k, :], gcol[:, k:k + 1], scr["rstd"], ALU.mult, ALU.mult,
                      [bxt, bscr, bc], [bh])

        bw = Buf("w")

        def phase_p1(l, tiles_full, tiles_kvc):
            nonlocal bw
            b.barrier()
            bw = Buf("w")
            W = b.abf.alloc([8, 4352])
            load_w(W, w_in[l][:, 0:4352], 8)
            xts = [b.af.alloc([8, 512]) for _ in range(2)]
            bxts = [Buf("xt") for _ in range(2)]
            scrs = [{"sq": b.abf.alloc([8, 512]), "ln": b.af.alloc([512]), "rstd": b.af.alloc([512])} for _ in range(2)]
            bscrs = [Buf("scr") for _ in range(2)]
            hs_ = [b.abf.alloc([8, 512]) for _ in range(2)]; bhs = [Buf("h") for _ in range(2)]
            qs = [b.abf.alloc([512]) for _ in range(3)]; bqs = [Buf("q") for _ in range(3)]
            t1 = [b.af.alloc([512]) for _ in range(3)]; bt1 = [Buf("t1") for _ in range(3)]
            qo = [b.abf.alloc([512]) for _ in range(3)]; bqo = [Buf("qo") for _ in range(3)]
            vsb = b.abf.alloc([4, 1280]); bvs = Buf("vsb")
            g1 = vec_sb[:, l, 0:8]
            rc = [b.abf.alloc([512]) for _ in range(2)]; rs_ = [b.abf.alloc([512]) for _ in range(2)]
            brc = [Buf("rc") for _ in range(2)]
            nt = 0
            alltiles = sorted(set(tiles_full) | set(tiles_kvc))
            it = 0
            for ti in alltiles:
                t0 = ti * 512
                full = ti in tiles_full
                rj = nt % 2; nt += 1
                xt = xts[rj]; bxt = bxts[rj]
                h = hs_[rj]; bh = bhs[rj]; scr = scrs[rj]; bscr = bscrs[rj]

                def p1_load(tix_, jj):
                    tt0 = tix_ * 512
                    S.dma(xts[jj], xs.rearrange("(k p) t -> p k t", p=128)[:, :, tt0:tt0 + 512], writes=[bxts[jj]], q="sp")
                    S.dma(rc[jj], ropeC[:, tt0:tt0 + 512], writes=[brc[jj]], q="sp")
                    S.dma(rs_[jj], ropeS[:, tt0:tt0 + 512], writes=[brc[jj]], q="sp")
                if nt == 1:
                    p1_load(ti, rj)
                if nt < len(alltiles):
                    p1_load(alltiles[nt], nt % 2)
                rms_tile(xt, bxt, g1, h, bh, ps[0], pb[0], scr, bscr)
                if full:
                    S.dma(hT.rearrange("(k p) t -> p k t", p=128)[:, :, t0:t0 + 512], h, reads=[bh], q="sp")
                chunks = []
                if full:
                    chunks += [(0 + i * 128, QAt, i * 128, True) for i in range(6)]
                chunks += [(768 + i * 128, KAt, i * 128, True) for i in range(6)]
                if full:
                    chunks += [(2304 + i * 128, QBt, i * 128, True) for i in range(4)]
                chunks += [(2816 + i * 128, KBt, i * 128, True) for i in range(4)]
                chunks += [(3840 + i * 128, Ct, i * 128, False) for i in range(4)]
                def p1_main(ci):
                    co, dst, dr, rope = chunks[ci]
                    pi = 1 + (ci % 3)
                    for k in range(8):
                        b.mm(ps[pi], W[:, k, co:co + 128], h[:, k, :], k == 0, k == 7, [bw, bh], [pb[pi]])
                    j = ci % 3
                    if not rope:
                        b.act(qo[j], ps[pi], AF.Copy, [pb[pi]], [bqo[j]])
                    else:
                        b.act(qs[j], ps[pi], AF.Copy, [pb[pi]], [bqs[j]])

                def p1_post(ci):
                    co, dst, dr, rope = chunks[ci]
                    j = ci % 3
                    if rope:
                        pr = 4 + (ci % 2)
                        b.mm(ps[pr], Rblk, qs[j], True, True, [bc, bqs[j]], [pb[pr]])
                        b.tt(t1[j], qs[j], rc[rj], ALU.mult, [bqs[j], brc[rj]], [bt1[j]], eng="pool")
                        b.tt(qo[j], ps[pr], rs_[rj], ALU.mult, [pb[pr], brc[rj]], [bqo[j]])
                        b.tt(qo[j], qo[j], t1[j], ALU.add, [bqo[j], bt1[j]], [bqo[j]])
                    S.dma(dst[dr:dr + 128, t0:t0 + 512], qo[j], reads=[bqo[j]], q="sp")

                for ci in range(len(chunks)):
                    p1_main(ci)
                    if ci >= 1:
                        p1_post(ci - 1)
                p1_post(len(chunks) - 1)
                it += 1
                for s in range(4):
                    for (co, n, vo) in ((1536, 512, 0), (2048, 256, 512), (3328, 512, 768)):
                        pi = 1 + (it % 3); it += 1
                        for k in range(8):
                            b.mm(ps[pi][:, 0:n], h[:, k, s * 128:(s + 1) * 128], W[:, k, co:co + n], k == 0, k == 7,
                                 [bw, bh], [pb[pi]])
                        b.act(vsb[:, s, vo:vo + n], ps[pi][:, 0:n], AF.Copy, [pb[pi]], [bvs])
                S.dma(VA[t0:t0 + 512, :].rearrange("(s p) c -> p s c", p=128), vsb[:, :, 0:768], reads=[bvs], q="sp")
                S.dma(VB[t0:t0 + 512, :].rearrange("(s p) c -> p s c", p=128), vsb[:, :, 768:1280], reads=[bvs], q="sp")

        bconv = [Buf(f"conv{i}") for i in range(9)]

        def emit_conv(idx, g_src, u_src, d_src):
            for gi in range(14):
                S.dma(Wg_b[idx, gi].rearrange("p (k j) -> p k j", j=256),
                      g_src[:, gi * 256:(gi + 1) * 256].rearrange("(k p) j -> p k j", p=128),
                      writes=[bconv[idx]], q="pool", bg=True)
                S.dma(Wu_b[idx, gi].rearrange("p (k j) -> p k j", j=256),
                      u_src[:, gi * 256:(gi + 1) * 256].rearrange("(k p) j -> p k j", p=128),
                      writes=[bconv[idx]], q="pool", bg=True)
            for c in range(8):
                S.dma(Wd_b[idx, c].rearrange("p (f j) -> p f j", j=128),
                      d_src[:, c * 128:(c + 1) * 128].rearrange("(f p) j -> p f j", p=128),
                      writes=[bconv[idx]], q="pool", bg=True)

        def phase_p2b(l, qtiles):
            b.barrier()
            if l == 0:
                emit_conv(0, ffn_g, ffn_u, ffn_d)
                if n_layers > 1:
                    for e in range(NE):
                        emit_conv(1 + e, moe_g[e], moe_u[e], moe_d[e])
            nkb_max = (max(qtiles) + 1) * 4
            kts = [b.abf.alloc([T]) for _ in range(1)]; bkt = [Buf("kt") for _ in range(1)]
            vts = [b.abf.alloc([64, 128]) for _ in range(1)]; bvt = [Buf("vt") for _ in range(1)]
            qzs = [[b.abf.alloc([512]) for _ in range(2)] for _ in range(2)]; bqt = [Buf("qt") for _ in range(2)]
            for par in range(2):
                for c in range(2):
                    b.memset(qzs[par][c], 0.0, [bqt[par]])
            pts = [b.abf.alloc([512]) for _ in range(4)]; bpt = [Buf("pt") for _ in range(4)]
            rd = b.af.alloc([512]); o1 = b.af.alloc([512]); o2 = b.af.alloc([512]); lnb = b.af.alloc([512])
            sqb = b.abf.alloc([512]); ob = b.abf.alloc([512])
            be = Buf("epi"); bob = Buf("ob")
            accD = [b.af.alloc([512]) for _ in range(2)]; bacc = [Buf("accD") for _ in range(2)]
            dh = b.abf.alloc([512]); dl = b.abf.alloc([512]); bdh = Buf("dh")
            pending = [None]
            neglam = lam_sb[:, l, 4:5]
            gsub = vec_sb[:, l, 40:41]
            nq = 0
            for hh in range(4):
                kt = kts[0]; vt = vts[0]
                S.dma(kt[:, 0:nkb_max * 128], KBt[hh * 128:(hh + 1) * 128, 0:nkb_max * 128], writes=[bkt[0]], q="sp")
                for c8 in range(0, nkb_max, 8):
                    S.dma(vt[:, c8:c8 + 8, :],
                          VB[c8 * 128:(c8 + 8) * 128, hh * 128:(hh + 1) * 128].rearrange("(n p) c -> p n c", p=128),
                          writes=[bvt[0]], q="sp")
                for qi in qtiles:
                    q0 = qi * 512
                    own = q0 >= TO
                    qz = qzs[nq % 2]; bq = bqt[nq % 2]; nq += 1
                    S.dma(qz[0][0:64, :], QBt[hh * 128:hh * 128 + 64, q0:q0 + 512], writes=[bq], q="sp")
                    S.dma(qz[1][64:128, :], QBt[hh * 128 + 64:(hh + 1) * 128, q0:q0 + 512], writes=[bq], q="sp")
                    nkb = (qi + 1) * 4
                    if own:
                        kbs = list(range(0, nkb))
                    else:
                        kbs = list(range(0, nkb))
                    items = [(c, kb) for kb in kbs for c in (0, 1)]

                    def qk(i):
                        c, kb = items[i]
                        sb_i = i % 4
                        diag = kb >= qi * 4
                        rows = slice(64 * c, 64 * c + 64)
                        b.mm(ps[sb_i], kt[:, kb * 128:(kb + 1) * 128], qz[c], True, not diag,
                             [bkt[0], bq], [pb[sb_i]])
                        if diag:
                            b.mm(ps[sb_i], ident, maskB[:, kb - qi * 4, :], False, True, [bc], [pb[sb_i]])

                    LA = 3
                    n_it = len(items)
                    for _w in range(WARM_P2B if qi != qtiles[0] else WARM_B):
                        b.mm(ps[7], ones, maskB[:, 0, :], True, True, [bc], [pb[7]])
                    for i0 in range(min(LA, n_it)):
                        qk(i0)
                    for i, (c, kb) in enumerate(items):
                        if i + LA < n_it:
                            qk(i + LA)
                        sb_i = i % 4
                        pt = pts[i % 4]; bp = bpt[i % 4]
                        pref = own and kb < TO // 128
                        b.act(pt, ps[sb_i], AF.Exp, [pb[sb_i], bc], [bp], scale=0.125, bias=pbias if pref else None)
                        first = kb == kbs[0]; last = kb == kbs[-1]
                        b.mm(ps[4 + c], vt[:, kb, :], pt, first, last, [bvt[0], bp], [pb[4 + c]])
                        if first:
                            b.copy(accD[c], pt, [bp], [bacc[c]])
                        else:
                            b.tt(accD[c], accD[c], pt, ALU.add, [bp, bacc[c]], [bacc[c]])
                        if i == 6 and pending[0] is not None:
                            pending[0]()
                            pending[0] = None
                    if pending[0] is not None:
                        pending[0]()
                        pending[0] = None
                    for c in (0, 1):
                        b.copy(dh, accD[c], [bacc[c]], [bdh])
                        b.tt(dl, accD[c], dh, ALU.subtract, [bacc[c], bdh], [bdh])
                        b.mm(ps[6 + c], ones, dh, True, False, [bc, bdh], [pb[6 + c]])
                        b.mm(ps[6 + c], ones, dl, False, True, [bc, bdh], [pb[6 + c]])
                        b.act(rd, ps[6 + c], AF.Ln, [pb[6 + c]], [be])
                        b.act(rd, rd, AF.Exp, [be], [be], scale=-1.0)
                        b.tt(o1 if c == 0 else o2, ps[4 + c], rd, ALU.mult, [pb[4 + c], be], [be])
                    b.stt(o1, o2, neglam, o1, ALU.mult, ALU.add, [be, bc], [be])

                    def part2(hh=hh, q0=q0):
                        b.act(sqb, o1, AF.Square, [be], [be])
                        b.mm(ps[6], ones, sqb, True, True, [bc, be], [pb[6]])
                        b.act(lnb, ps[6], AF.Ln, [pb[6], bc], [be], scale=1.0 / 128.0, bias=epsc)
                        b.act(lnb, lnb, AF.Exp, [be], [be], scale=-0.5)
                        b.stt(ob, o1, gsub, lnb, ALU.mult, ALU.mult, [be, bc], [bob])
                        S.dma(OBt[hh * 128:(hh + 1) * 128, q0:q0 + 512], ob, reads=[bob], q="sp")
                    pending[0] = part2
            if pending[0] is not None:
                pending[0]()
                pending[0] = None

        def phase_p2a(l, segs):
            b.barrier()
            ka = b.abf.alloc([2, 4096]); bka = Buf("ka")
            qaz = [b.abf.alloc([2, 2048]) for _ in range(2)]; bqa = Buf("qa")
            for hp_ in range(2):
                b.memset(qaz[hp_], 0.0, [bqa])
            vts = [b.abf.alloc([2, 256]) for _ in range(3)]; bvts = [Buf("va") for _ in range(3)]
            pts = [b.abf.alloc([256]) for _ in range(3)]; bpts = [Buf("pa") for _ in range(3)]
            accN = b.af.alloc([2, 2048]); accD = b.af.alloc([2, 2048]); bacc = Buf("acc")
            oa = b.abf.alloc([2, 2048]); boa = Buf("oa")
            vbc = [0]; gic = [0]
            for sg in segs:
                s0 = sg * 2048
                first_seg = (sg == 0)
                own_first = (s0 == TO)
                for g, dil in enumerate((1, 4, 16)):
                    for p in range(2):
                        r0 = g * 256 + p * 128
                        if first_seg:
                            S.dma(ka[:, p, 2048:4096], KAt[r0:r0 + 128, s0:s0 + 2048], writes=[bka], q="sp")
                        else:
                            S.dma(ka[:, p, :], KAt[r0:r0 + 128, s0 - 2048:s0 + 2048], writes=[bka], q="sp")
                        S.dma(qaz[0][0:64, p, :], QAt[r0:r0 + 64, s0:s0 + 2048], writes=[bqa], q="sp")
                        S.dma(qaz[1][64:128, p, :], QAt[r0 + 64:r0 + 128, s0:s0 + 2048], writes=[bqa], q="sp")
                    nblk = 16 // dil
                    blocks = [(r, n) for r in range(dil) for n in range(nblk)]
                    nb = len(blocks)
                    vb0 = vbc[0]; vbc[0] += nb
                    g0 = gic[0]; gic[0] += nb * 4

                    def binfo(k):
                        r, n = blocks[k]
                        cs = n * 128 * dil + r
                        has_prev = not (first_seg and n == 0)
                        return r, n, cs, has_prev, cs - 128 * dil

                    def loadV(k):
                        r, n, cs, has_prev, ps_ = binfo(k)
                        vi = (vb0 + k) % 3
                        vt = vts[vi]; bv = bvts[vi]
                        tok = s0 + cs
                        S.dma(vt[:, 0, :], VA[tok:tok + 127 * dil + 1:dil, g * 256:(g + 1) * 256], writes=[bv], q="sp")
                        if has_prev:
                            tokp = s0 + ps_
                            S.dma(vt[:, 1, :], VA[tokp:tokp + 127 * dil + 1:dil, g * 256:(g + 1) * 256], writes=[bv], q="sp")

                    def stA(i):
                        k, hg = divmod(i, 4)
                        r, n, cs, has_prev, ps_ = binfo(k)
                        p = hg // 2; hp = hg % 2
                        rows = slice(64 * hp, 64 * hp + 64)
                        si = (g0 + i) % 3
                        pst = ps[si]; psb_ = pb[si]
                        qsl = slice(cs, cs + 127 * dil + 1, dil)
                        b.mm(pst[:, 0:128], ka[:, p, 2048 + cs:2048 + cs + 127 * dil + 1:dil], qaz[hp][:, p, qsl],
                             True, False, [bka, bqa], [psb_])
                        b.mm(pst[:, 0:128], ident, maskA[:, 0:128], False, True, [bc], [psb_])
                        if has_prev:
                            b.mm(pst[:, 128:256], ka[:, p, 2048 + ps_:2048 + ps_ + 127 * dil + 1:dil],
                                 qaz[hp][:, p, qsl], True, False, [bka, bqa], [psb_])
                            b.mm(pst[:, 128:256], ident, maskA[:, 128:256], False, True, [bc], [psb_])

                    def stBCD(i):
                        k, hg = divmod(i, 4)
                        r, n, cs, has_prev, ps_ = binfo(k)
                        p = hg // 2; hp = hg % 2
                        rows = slice(64 * hp, 64 * hp + 64)
                        si = (g0 + i) % 3
                        pst = ps[si]; psb_ = pb[si]
                        qsl = slice(cs, cs + 127 * dil + 1, dil)
                        vi = (vb0 + k) % 3
                        vt = vts[vi]; bv = bvts[vi]
                        pt = pts[si]; bp = bpts[si]
                        b.act(pt[:, 0:128], pst[:, 0:128], AF.Exp, [psb_], [bp], scale=0.125)
                        if has_prev:
                            pref = own_first and n == 0
                            b.act(pt[:, 128:256], pst[:, 128:256], AF.Exp, [psb_, bc], [bp], scale=0.125,
                                  bias=pbias if pref else None)
                        po = 3 + ((g0 + i) % 3)
                        pso = ps[po]; pbo = pb[po]
                        b.mm(pso[:, 0:128], vt[:, 0, p * 128:(p + 1) * 128], pt[:, 0:128], True, not has_prev,
                             [bv, bp], [pbo])
                        if has_prev:
                            b.mm(pso[:, 0:128], vt[:, 1, p * 128:(p + 1) * 128], pt[:, 128:256], False, True,
                                 [bv, bp], [pbo])
                        b.mm(pso[:, 128:256], ones, pt[:, 0:128], True, not has_prev, [bc, bp], [pbo])
                        if has_prev:
                            b.mm(pso[:, 128:256], ones, pt[:, 128:256], False, True, [bc, bp], [pbo])
                        if g == 0:
                            b.act(accN[rows, p, qsl], pso[rows, 0:128], AF.Copy, [pbo], [bacc])
                            b.copy(accD[rows, p, qsl], pso[rows, 128:256], [pbo], [bacc])
                        else:
                            b.tt(accN[rows, p, qsl], accN[rows, p, qsl], pso[rows, 0:128], ALU.add,
                                 [pbo, bacc], [bacc])
                            b.tt(accD[rows, p, qsl], accD[rows, p, qsl], pso[rows, 128:256], ALU.add,
                                 [pbo, bacc], [bacc], eng="dve")

                    n_it = nb * 4
                    for _w in range(WARM_B):
                        b.mm(ps[6], ones, maskB[:, 0, :], True, True, [bc], [pb[6]])
                    loadV(0)
                    if nb > 1:
                        loadV(1)
                    stA(0); stA(1)
                    for i in range(n_it):
                        k, hg = divmod(i, 4)
                        if hg == 0 and k + 2 < nb:
                            loadV(k + 2)
                        if i + 2 < n_it:
                            stA(i + 2)
                        stBCD(i)
                for p in range(2):
                    b.recip(accD[:, p, :], accD[:, p, :], [bacc], [bacc])
                    b.tt(oa[:, p, :], accN[:, p, :], accD[:, p, :], ALU.mult, [bacc], [boa])
                    S.dma(OAt[p * 128:(p + 1) * 128, s0:s0 + 2048], oa[:, p, :], reads=[boa], q="sp")

        def phase_p2c(l, halves):
            nonlocal bw
            b.barrier()
            bw = Buf("w")
            PW = b.abf.alloc([4, 128])
            for g in range(4):
                S.dma(PW[:, g, :], pool_w[l, g], writes=[bw], q="pool")
            cb = b.af.alloc([16 + 2048]); s2 = b.af.alloc([16 + 2048]); s4 = b.af.alloc([16 + 2048])
            icn = b.af.alloc([2048])
            bcb = Buf("cb"); bs = Buf("s"); bic = Buf("icn")
            cbf = b.abf.alloc([16 + 2048]); bcbf = Buf("cbf")
            dd = b.abf.alloc([2048]); bdd = Buf("dd")
            oc = [b.abf.alloc([512]) for _ in range(2)]; boc = [Buf("oc") for _ in range(2)]
            it = 0
            for hf in halves:
                for q4 in range(2):
                    t0 = hf * TO + q4 * 2048
                    for g, w in enumerate((2, 4, 8, 16)):
                        S.dma(cbf[:, 16:], Ct[g * 128:(g + 1) * 128, t0:t0 + 2048], writes=[bcbf], q="sp")
                        if t0 == 0:
                            b.memset(cbf[:, 0:16], 0.0, [bcbf])
                        else:
                            S.dma(cbf[:, 0:16], Ct[g * 128:(g + 1) * 128, t0 - 16:t0], writes=[bcbf], q="sp")
                        S.dma(icn, invcnt[g:g + 1, t0:t0 + 2048].broadcast_to([128, 2048]), writes=[bic], q="sp")
                        b.copy(cb, cbf, [bcbf], [bcb])
                        if t0 == TO:
                            b.ts(cb[:, 0:16], cb[:, 0:16], pflag, None, ALU.mult, None, [bcb, bc], [bcb])
                        N = 16 + 2048
                        b.tt(s2[:, 1:N], cb[:, 1:N], cb[:, 0:N - 1], ALU.add, [bcb], [bs])
                        cur = s2
                        if w >= 4:
                            b.tt(s4[:, 3:N], s2[:, 3:N], s2[:, 1:N - 2], ALU.add, [bs], [bs])
                            cur = s4
                        if w >= 8:
                            b.tt(s2[:, 7:N], s4[:, 7:N], s4[:, 3:N - 4], ALU.add, [bs], [bs])
                            cur = s2
                        if w >= 16:
                            b.tt(s4[:, 15:N], s2[:, 15:N], s2[:, 7:N - 8], ALU.add, [bs], [bs])
                            cur = s4
                        b.tt(cur[:, 16:N], cur[:, 16:N], icn, ALU.mult, [bs, bic], [bs])
                        b.tt(dd, cur[:, 16:N], cb[:, 16:N], ALU.subtract, [bs, bcb], [bdd])
                        for s in range(4):
                            pi = it % 4; it += 1
                            b.mm(ps[pi], PW[:, g, :], dd[:, s * 512:(s + 1) * 512], True, True, [bw, bdd], [pb[pi]])
                            j = it % 2
                            b.act(oc[j], ps[pi], AF.Copy, [pb[pi], bc], [boc[j]], scale=vec_sb[:, l, 41 + g:42 + g])
                            S.dma(OCt[g * 128:(g + 1) * 128, t0 + s * 512:t0 + (s + 1) * 512], oc[j], reads=[boc[j]], q="sp")

        def phase_p3(l, tiles):
            nonlocal bw
            b.barrier()
            bw = Buf("w")
            WG = b.abf.alloc([8, 3072])
            load_w(WG, w_in[l][:, 4352:7424], 8)
            WP = b.abf.alloc([10, 1024])
            load_w(WP[:, 0:2, :], w_pa[l], 2); load_w(WP[:, 2:6, :], w_pb[l], 4); load_w(WP[:, 6:10, :], w_pc[l], 4)
            WO = b.abf.alloc([8, 1024])
            load_w(WO, w_out[l], 8)
            h = b.abf.alloc([8, 512]); bh = Buf("h")
            om = b.abf.alloc([10, 512]); bom = Buf("om")
            mixed = b.abf.alloc([8, 512]); bmx = Buf("mixed")
            xt = b.af.alloc([8, 512]); bxt = Buf("xt")
            sg = [b.af.alloc([512]) for _ in range(2)]; bsg = [Buf("sg") for _ in range(2)]
            macc = b.af.alloc([512]); bma = Buf("macc")
            it = 0
            for ti in tiles:
                t0 = ti * 512
                S.dma(h, hT.rearrange("(k p) t -> p k t", p=128)[:, :, t0:t0 + 512], writes=[bh], q="sp")
                S.dma(om[:, 0:2, :], OAt.rearrange("(k p) t -> p k t", p=128)[:, :, t0:t0 + 512], writes=[bom], q="sp")
                S.dma(om[:, 2:6, :], OBt.rearrange("(k p) t -> p k t", p=128)[:, :, t0:t0 + 512], writes=[bom], q="sp")
                S.dma(om[:, 6:10, :], OCt.rearrange("(k p) t -> p k t", p=128)[:, :, t0:t0 + 512], writes=[bom], q="sp")
                S.dma(xt, xs.rearrange("(k p) t -> p k t", p=128)[:, :, t0:t0 + 512], writes=[bxt], q="sp")
                for c in range(8):
                    for br, (k0, k1) in enumerate(((0, 2), (2, 6), (6, 10))):
                        pg = (it % 3) * 2; it += 1
                        for k in range(8):
                            b.mm(ps[pg], WG[:, k, br * 1024 + c * 128: br * 1024 + (c + 1) * 128], h[:, k, :],
                                 k == 0, k == 7, [bw, bh], [pb[pg]])
                        for k in range(k0, k1):
                            b.mm(ps[pg + 1], WP[:, k, c * 128:(c + 1) * 128], om[:, k, :], k == k0, k == k1 - 1,
                                 [bw, bom], [pb[pg + 1]])
                        j = it % 2
                        b.act(sg[j], ps[pg], AF.Sigmoid, [pb[pg], bc], [bsg[j]], bias=vec_sb[:, l, 16 + br * 8 + c:17 + br * 8 + c])
                        if br == 0:
                            b.tt(macc, sg[j], ps[pg + 1], ALU.mult, [bsg[j], pb[pg + 1]], [bma])
                        else:
                            b.tt(sg[j], sg[j], ps[pg + 1], ALU.mult, [bsg[j], pb[pg + 1]], [bsg[j]])
                            if br == 1:
                                b.tt(macc, macc, sg[j], ALU.add, [bma, bsg[j]], [bma], eng="pool")
                            else:
                                b.tt(mixed[:, c, :], macc, sg[j], ALU.add, [bma, bsg[j]], [bmx], eng="pool")
                for c in range(8):
                    po = 6 + (c % 2)
                    for k in range(8):
                        b.mm(ps[po], WO[:, k, c * 128:(c + 1) * 128], mixed[:, k, :], k == 0, k == 7, [bw, bmx], [pb[po]])
                    b.tt(xt[:, c, :], xt[:, c, :], ps[po], ALU.add, [bxt, pb[po]], [bxt])
                S.dma(xs.rearrange("(k p) t -> p k t", p=128)[:, :, t0:t0 + 512], xt, reads=[bxt], q="sp")

        def phase_p4(l, tiles2, final):
            b.barrier()
            moe = (l % 2 == 1)
            wg = [b.abf.alloc([8, 256]) for _ in range(2)]
            wu = [b.abf.alloc([8, 256]) for _ in range(2)]
            bwb = [Buf("wbuf") for _ in range(2)]
            wdh = [b.abf.alloc([NF, 128]) for _ in range(2)]
            bwd = [Buf("wd") for _ in range(2)]
            actT = b.abf.alloc([NF, 1024]); bact = Buf("act")
            h = b.abf.alloc([8, 1024]); bh = Buf("h")
            lnb = b.af.alloc([1024]); rstd = b.af.alloc([1024]); bscr = Buf("scr")
            xt = b.af.alloc([8, 1024]); bxt = Buf("xt")
            sl = [b.abf.alloc([512]) for _ in range(2)]; bsl = [Buf("sl") for _ in range(2)]
            g2 = vec_sb[:, l, 8:16]
            if moe:
                hf = b.af.alloc([8, 512]); bhf = Buf("hf")
                sq = actT[:, 0:8, :]
            else:
                hbufs = [h, b.abf.alloc([8, 1024])]; bhb = [bh, Buf("h2")]
                sqd = b.af.alloc([2048]).bitcast(BF16).rearrange("p (k t) -> p k t", t=512); bsqd = Buf("sqd")
                xc = [b.af.alloc([1024]) for _ in range(2)]; bxc = [Buf("xc") for _ in range(2)]

            def prep_dense(ti, hb):
                t0_ = ti * 1024
                S.dma(xt, xs.rearrange("(k p) t -> p k t", p=128)[:, :, t0_:t0_ + 1024], writes=[bxt], q="sp")
                for hv in range(2):
                    hs = slice(hv * 512, (hv + 1) * 512)
                    for k in range(8):
                        b.act(sqd[:, k, :], xt[:, k, hs], AF.Square, [bxt], [bsqd])
                    for k in range(8):
                        b.mm(ps[0], ones, sqd[:, k, :], k == 0, k == 7, [bsqd, bc], [pb[0]])
                    b.act(lnb[:, hs], ps[0], AF.Ln, [pb[0], bc], [bscr], scale=1.0 / 1024.0, bias=epsc)
                b.act(rstd, lnb, AF.Exp, [bscr], [bscr], scale=-0.5)
                for k in range(8):
                    b.stt(hbufs[hb][:, k, :], xt[:, k, :], g2[:, k:k + 1], rstd, ALU.mult, ALU.mult,
                          [bxt, bscr, bc], [bhb[hb]])
            if moe:
                rw = sbt("rw", [128, 8, NE], F32)
                brw = Buf("rw")
                S.dma(rw[:, :, :], router[:, :, :], writes=[brw], q="sp")
                lg = b.af.alloc([4, 8]); mx = b.af.alloc([4, 8]); ex = b.af.alloc([4, 8]); msk = b.af.alloc([4, 8])
                den = b.af.alloc([4]); gT = b.abf.alloc([512]); gbc = b.abf.alloc([NE, 1024])
                gate_bf = b.abf.alloc([4, 8])
                sel = b.abf.alloc([NE, 128])
                bg = Buf("gate"); bgb = Buf("gbc"); bsel = Buf("sel")
                ytmp = [b.af.alloc([512]) for _ in range(2)]; byt = [Buf("yt") for _ in range(2)]
                for e in range(NE):
                    b.copy(sel[0:8, e, :], ident[0:8, e:e + 1].broadcast_to([8, 128]), [bc], [bsel])
            nexp = NE if moe else 1
            wcount = 0
            dcount = 0
            it = 0
            if not moe:
                prep_dense(tiles2[0], 0)
            for tix, ti in enumerate(tiles2):
                t0 = ti * 1024
                if not moe:
                    h = hbufs[tix % 2]; bh = bhb[tix % 2]
                else:
                    S.dma(xt, xs.rearrange("(k p) t -> p k t", p=128)[:, :, t0:t0 + 1024], writes=[bxt], q="sp")
                    for k in range(8):
                        b.act(sq[:, k, :], xt[:, k, :], AF.Square, [bxt], [bact])
                    for hv in range(2):
                        hs = slice(hv * 512, (hv + 1) * 512)
                        for k in range(8):
                            b.mm(ps[0], ones, sq[:, k, hs], k == 0, k == 7, [bact, bc], [pb[0]])
                        b.act(lnb[:, hs], ps[0], AF.Ln, [pb[0], bc], [bscr], scale=1.0 / 1024.0, bias=epsc)
                    b.act(rstd, lnb, AF.Exp, [bscr], [bscr], scale=-0.5)
                    for k in range(8):
                        b.stt(h[:, k, :], xt[:, k, :], g2[:, k:k + 1], rstd, ALU.mult, ALU.mult, [bxt, bscr, bc], [bh])
                if moe:
                    for hv in range(2):
                        hs = slice(hv * 512, (hv + 1) * 512)
                        for k in range(8):
                            b.stt(hf[:, k, :], xt[:, k, hs], g2[:, k:k + 1], rstd[:, hs], ALU.mult, ALU.mult,
                                  [bxt, bscr, bc], [bhf])
                        for s in range(4):
                            for k in range(8):
                                b.mm(ps[1][:, s * 8:(s + 1) * 8], hf[:, k, s * 128:(s + 1) * 128], rw[:, k, :], k == 0, k == 7,
                                     [bhf, brw], [pb[1]])
                        b.copy(lg, ps[1][:, 0:32].rearrange("p (s e) -> p s e", e=8), [pb[1]], [bg])
                        for s in range(4):
                            S.emit("dve", lambda e_, o=mx[:, s, :], i=lg[:, s, :]: e_.max(out=o, in_=i), [bg], [bg])
                            b.ts(den[:, s:s + 1], mx[:, s, 0:1], -1.0, None, ALU.mult, None, [bg], [bg])
                            b.act(ex[:, s, :], lg[:, s, :], AF.Exp, [bg], [bg], bias=den[:, s:s + 1])
                            b.ts(msk[:, s, :], lg[:, s, :], mx[:, s, 1:2], None, ALU.is_ge, None, [bg], [bg])
                            b.tt(ex[:, s, :], ex[:, s, :], msk[:, s, :], ALU.mult, [bg], [bg])
                            S.emit("dve", lambda e_, o=den[:, s:s + 1], i=ex[:, s, :]: e_.reduce_sum(
                                out=o, in_=i, axis=mybir.AxisListType.X), [bg], [bg])
                            b.recip(den[:, s:s + 1], den[:, s:s + 1], [bg], [bg])
                            b.ts(gate_bf[:, s, :], ex[:, s, :], den[:, s:s + 1], None, ALU.mult, None, [bg], [bg])
                        for s in range(4):
                            b.mm(ps[2][0:8, s * 128:(s + 1) * 128], gate_bf[:, s, :], ident, True, True, [bg, bc], [pb[2]])
                        b.copy(gT[0:8, :], ps[2][0:8, :], [pb[2]], [bg])
                        for e in range(NE):
                            b.mm(ps[3], sel[0:8, e, :], gT[0:8, :], True, True, [bsel, bg], [pb[3]])
                            b.act(gbc[:, e, hs], ps[3], AF.Copy, [pb[3]], [bgb])
                for e in range(nexp):
                    widx = 1 + e if moe else 0
                    for gi in range(14):
                        wb = wcount % 2; wcount += 1
                        S.dma(wg[wb], Wg_b[widx, gi].rearrange("p (k j) -> p k j", j=256), reads=[bconv[widx]],
                              writes=[bwb[wb]], q="sp")
                        S.dma(wu[wb], Wu_b[widx, gi].rearrange("p (k j) -> p k j", j=256), reads=[bconv[widx]],
                              writes=[bwb[wb]], q="sp")
                        for fi in range(2):
                            f = gi * 2 + fi
                            for k in range(8):
                                for hv in range(2):
                                    b.mm(ps[4 + hv], wg[wb][:, k, fi * 128:(fi + 1) * 128], h[:, k, hv * 512:(hv + 1) * 512],
                                         k == 0, k == 7, [bwb[wb], bh], [pb[4 + hv]])
                            for k in range(8):
                                for hv in range(2):
                                    b.mm(ps[6 + hv], wu[wb][:, k, fi * 128:(fi + 1) * 128], h[:, k, hv * 512:(hv + 1) * 512],
                                         k == 0, k == 7, [bwb[wb], bh], [pb[6 + hv]])
                            for hv in range(2):
                                j = it % 2; it += 1
                                b.act(sl[j], ps[4 + hv], AF.Silu, [pb[4 + hv]], [bsl[j]])
                                b.tt(actT[:, f, hv * 512:(hv + 1) * 512], sl[j], ps[6 + hv], ALU.mult,
                                     [bsl[j], pb[6 + hv]], [bact])
                    if (not moe) and tix + 1 < len(tiles2):
                        prep_dense(tiles2[tix + 1], (tix + 1) % 2)
                    dbase = dcount; dcount += 8

                    def load_wd(c_):
                        db_ = (dbase + c_) % 2
                        S.dma(wdh[db_], Wd_b[widx, c_].rearrange("p (f j) -> p f j", j=128), reads=[bconv[widx]],
                              writes=[bwd[db_]], q="sp")
                    load_wd(0)
                    for c in range(8):
                        db = (dbase + c) % 2
                        pp = (1, 2) if c % 2 == 0 else (3, 5)
                        if not moe:
                            xj = c % 2
                            S.dma(xc[xj], xs[c * 128:(c + 1) * 128, t0:t0 + 1024], writes=[bxc[xj]], q="sp")
                        if c + 1 < 8:
                            load_wd(c + 1)
                        for f in range(NF):
                            for hv in range(2):
                                b.mm(ps[pp[hv]], wdh[db][:, f, :], actT[:, f, hv * 512:(hv + 1) * 512], f == 0, f == NF - 1,
                                     [bwd[db], bact], [pb[pp[hv]]])
                        for hv in range(2):
                            hs = slice(hv * 512, (hv + 1) * 512)
                            po = pp[hv]
                            if moe:
                                j = hv
                                b.tt(ytmp[j], ps[po], gbc[:, e, hs], ALU.mult, [pb[po], bgb], [byt[j]])
                                b.tt(xt[:, c, hs], xt[:, c, hs], ytmp[j], ALU.add, [bxt, byt[j]], [bxt], eng="pool")
                            else:
                                b.tt(xc[xj][:, hs], xc[xj][:, hs], ps[po], ALU.add, [bxc[xj], pb[po]], [bxc[xj]])
                        if not moe:
                            S.dma(xs[c * 128:(c + 1) * 128, t0:t0 + 1024], xc[xj], reads=[bxc[xj]], q="sp")
                if final:
                    for k in range(8):
                        b.act(sq[:, k, :], xt[:, k, :], AF.Square, [bxt], [bact])
                    for hv in range(2):
                        hs = slice(hv * 512, (hv + 1) * 512)
                        for k in range(8):
                            b.mm(ps[0], ones, sq[:, k, hs], k == 0, k == 7, [bact, bc], [pb[0]])
                        b.act(lnb[:, hs], ps[0], AF.Ln, [pb[0], bc], [bscr], scale=1.0 / 1024.0, bias=epsc)
                    b.act(rstd, lnb, AF.Exp, [bscr], [bscr], scale=-0.5)
                    for hv in range(2):
                        hs = slice(hv * 512, (hv + 1) * 512)
                        for k in range(8):
                            b.stt(hf[:, k, :], xt[:, k, hs], fing_sb[:, k:k + 1], rstd[:, hs], ALU.mult, ALU.mult,
                                  [bxt, bscr, bc], [bhf])
                        S.dma(outT.rearrange("(k p) t -> p k t", p=128)[:, :, t0 - TO + hv * 512:t0 - TO + (hv + 1) * 512],
                              hf, reads=[bhf], q="sp")
                elif moe:
                    S.dma(xs.rearrange("(k p) t -> p k t", p=128)[:, :, t0:t0 + 1024], xt, reads=[bxt], q="sp")

        all_tiles = list(range(16)); own_tiles = list(range(8, 16))
        for l in range(n_layers):
            last = (l == n_layers - 1)
            if not last:
                phase_p1(l, all_tiles, all_tiles)
                phase_p2b(l, all_tiles)
                phase_p2a(l, [0, 1, 2, 3])
                phase_p2c(l, [0, 1])
                phase_p3(l, all_tiles)
                phase_p4(l, list(range(8)), False)
            else:
                phase_p1(l, own_tiles, all_tiles)
                phase_p2b(l, own_tiles)
                phase_p2a(l, [2, 3])
                phase_p2c(l, [1])
                phase_p3(l, own_tiles)
                phase_p4(l, list(range(4, 8)), True)
        if debug:
            b.barrier()
            for k in range(8):
                S.dma(dbg[k * 128:(k + 1) * 128, :], xs[k * 128:(k + 1) * 128, :], q="sp")
        S.run(nc)
    return nc


def _consts():
    ident = np.eye(128, dtype=np.float32)
    ones = np.ones((128, 128), np.float32)
    R = np.zeros((128, 128), np.float32)
    for hb in (0, 64):
        for d in range(16):
            src = d + 8 if d < 8 else d - 8
            R[hb + src, hb + d] = 1.0
    k = np.arange(128)[:, None]
    q = np.arange(128)[None, :]
    cur = np.where(q >= k, 0.0, NEGM).astype(np.float32)
    prev = np.where(k >= q, 0.0, NEGM).astype(np.float32)
    maskA = np.concatenate([cur, prev], axis=1)
    q5 = np.arange(512)[None, :]
    maskB = np.concatenate([np.where(128 * i + k <= q5, 0.0, NEGM).astype(np.float32) for i in range(4)], axis=1)
    inv_freq = (500000.0 ** (-np.arange(0, 16, 2, dtype=np.float32) / 16.0)).astype(np.float32)
    freq = np.zeros((128, 1), np.float32)
    sgn = np.zeros((128, 1), np.float32)
    for p in range(128):
        d = p % 64
        if d < 16:
            freq[p, 0] = inv_freq[d % 8] / (2.0 * np.pi)
            sgn[p, 0] = -1.0 if d < 8 else 1.0
    return np.concatenate([ident, ones, R, maskA, maskB, freq, sgn], axis=1).astype(np.float32)


def _feat(v):
    v = np.asarray(v, np.float32)
    return np.ascontiguousarray(v.reshape(-1, 128).T)


_NC_CACHE = {}


def make_in_maps(x, positions, norm1_g, w_in, b_gate, diff_lambda, diff_subln_g, pool_w, pool_scale,
                 w_proj_a, w_proj_b, w_proj_c, w_out, norm2_g, ffn_w_gate, ffn_w_up, ffn_w_down,
                 moe_router, moe_w_gate, moe_w_up, moe_w_down, final_norm_g):
    f32 = lambda a: np.ascontiguousarray(np.asarray(a, dtype=np.float32))
    x = f32(x); positions = np.asarray(positions).astype(np.int32)
    consts = _consts()
    vecs = np.zeros((2, 128, 64), np.float32)
    for l in range(2):
        vecs[l, :, 0:8] = _feat(norm1_g[l])
        vecs[l, :, 8:16] = _feat(norm2_g[l])
        for i in range(3):
            vecs[l, :, 16 + i * 8:24 + i * 8] = _feat(b_gate[l][i])
        vecs[l, :, 40] = np.asarray(diff_subln_g[l], np.float32)
        vecs[l, :, 41:45] = _feat(pool_scale[l])
    lam_in = f32(diff_lambda).reshape(2, 1, 256)
    shared = {
        "consts": consts, "vecs": vecs, "lam_in": lam_in, "fin_g": _feat(final_norm_g),
        "w_in": f32(w_in), "pool_w": f32(pool_w), "w_pa": f32(w_proj_a), "w_pb": f32(w_proj_b),
        "w_pc": f32(w_proj_c), "w_out": f32(w_out), "ffn_g": f32(ffn_w_gate)[0], "ffn_u": f32(ffn_w_up)[0],
        "ffn_d": f32(ffn_w_down)[0],
        "router": np.ascontiguousarray(f32(moe_router)[0].reshape(8, 128, NE).transpose(1, 0, 2)),
        "moe_g": f32(moe_w_gate)[0], "moe_u": f32(moe_w_up)[0], "moe_d": f32(moe_w_down)[0],
    }
    in_maps = []
    tpos = np.arange(8192)
    for c in range(8):
        bi, j = c // 2, c % 2
        order = np.concatenate([np.arange(4096), np.arange(4096, 8192)]) if j == 1 else \
            np.concatenate([np.arange(4096, 8192), np.arange(4096)])
        m = dict(shared)
        m["xT"] = np.ascontiguousarray(x[bi][order].T)
        m["pos"] = np.ascontiguousarray(positions[bi][order][None, :])
        tp = tpos[order]
        m["invcnt"] = np.stack([1.0 / np.minimum(tp + 1, w) for w in (2, 4, 8, 16)]).astype(np.float32)
        pc = np.zeros((128, 2), np.float32)
        pc[:, 0] = 0.0 if j == 1 else NEGM
        pc[:, 1] = 1.0 if j == 1 else 0.0
        m["percore"] = pc
        in_maps.append(m)
    return in_maps


def kernel(**inputs):
    if "nc" not in _NC_CACHE:
        _NC_CACHE["nc"] = build_program()
    nc = _NC_CACHE["nc"]
    in_maps = make_in_maps(**inputs)
    res = run_bass_kernel_spmd(nc, in_maps, core_ids=list(range(8)))
    out = np.zeros((4, 8192, 1024), np.float32)
    for c in range(8):
        bi, j = c // 2, c % 2
        out[bi, j * 4096:(j + 1) * 4096, :] = res.results[c]["outT"].T
    return out
```
